# Optimizing a Trainium2 kernel written in Bass

```python
import math
import jax, jax.numpy as jnp
from jax import lax
import numpy as np

D_MODEL = 1024
BATCH = 16
SEQ = 4096
DEPTH = 2

GRID_W = 64
CTX_LEN = 256
EPS = 1e-6
N_MOD = 6
N_BRANCH = 3
CHUNK = 128
A_GROUPS = 8
A_GROUP_DIM = 64
A_WIDTH = A_GROUPS * A_GROUP_DIM
B_GROUPS = 8
B_WIDTH = 512
CONV_W = 3
C_HEADS = 8
C_HEAD_DIM = 64
C_V_DIM = 2 * C_HEAD_DIM
C_QK_WIDTH = C_HEADS * 2 * C_HEAD_DIM
C_V_WIDTH = C_HEADS * C_V_DIM
Q_BLOCK = 128
ROPE_BASE = 10000.0
OFF_AU = 0
OFF_AV = OFF_AU + A_WIDTH
OFF_BB = OFF_AV + A_WIDTH
OFF_BC = OFF_BB + B_WIDTH
OFF_BH = OFF_BC + B_WIDTH
OFF_CQ = OFF_BH + B_WIDTH
OFF_CK = OFF_CQ + C_QK_WIDTH
OFF_CV = OFF_CK + C_QK_WIDTH
OFF_GATE = OFF_CV + C_V_WIDTH
IN_COLS = OFF_GATE + N_BRANCH * D_MODEL
SPLITS = [OFF_AV, OFF_BB, OFF_BC, OFF_BH, OFF_CQ, OFF_CK, OFF_CV, OFF_GATE]
D_FF_DENSE = 2816
N_EXPERTS = 8
TOP_K = 2
D_FF_EXPERT = 3584
N_DENSE_LAYERS = (DEPTH + 1) // 2
N_MOE_LAYERS = DEPTH // 2

kernel_name = 'hybrid_gated_diffusion_block'


def rmsnorm(x, g):
    xf = x.astype(jnp.float32)
    y = xf * lax.rsqrt(jnp.mean(xf * xf, axis=-1, keepdims=True) + EPS)
    return (y * g).astype(x.dtype)


def layernorm(x, g, b):
    xf = x.astype(jnp.float32)
    mu = jnp.mean(xf, axis=-1, keepdims=True)
    var = jnp.mean(jnp.square(xf - mu), axis=-1, keepdims=True)
    return ((xf - mu) * lax.rsqrt(var + EPS) * g + b).astype(x.dtype)


def modulate(x, g, shift, scale):
    return rmsnorm(x, g) * (1 + scale) + shift


def axial_rope_tables(n):
    rows = n // GRID_W
    r = jnp.repeat(jnp.arange(rows, dtype=jnp.float32), GRID_W)
    col = jnp.tile(jnp.arange(GRID_W, dtype=jnp.float32), rows)
    quarter = C_HEAD_DIM // 4
    inv = ROPE_BASE ** (-jnp.arange(quarter, dtype=jnp.float32) / quarter)
    ar = r[:, None] * inv
    ac = col[:, None] * inv
    ang = jnp.concatenate([ar, ar, ac, ac], axis=-1)
    return jnp.cos(ang), jnp.sin(ang)


def apply_rope(t, cos, sin):
    ts = t.reshape(t.shape[:-1] + (2, 2, C_HEAD_DIM // 4))
    rot = jnp.stack([-ts[..., 1, :], ts[..., 0, :]], axis=-2).reshape(t.shape)
    cb = cos[None, :, None, None, :]
    sb = sin[None, :, None, None, :]
    return (t * cb + rot * sb).astype(t.dtype)


def qk_heads(t):
    b, n, _ = t.shape
    return t.reshape(b, n, C_HEADS, 2, C_HEAD_DIM)


def v_heads(t):
    b, n, _ = t.shape
    return t.reshape(b, n, C_HEADS, C_V_DIM)


def diff_attention(q, k, v, lam):
    s = jnp.einsum('bqhmd,bkhmd->bhmqk', q, k).astype(jnp.float32) * (C_HEAD_DIM ** -0.5)
    p = jax.nn.softmax(s, axis=-1)
    a = p[:, :, 0] - lam * p[:, :, 1]
    return jnp.einsum('bhqk,bkhe->bqhe', a.astype(v.dtype), v)


def diff_attention_latent(q, k_lat, v_lat, k_ctx, v_ctx, lam):
    k = jnp.concatenate([k_lat, k_ctx], axis=1)
    v = jnp.concatenate([v_lat, v_ctx], axis=1)
    b, n = q.shape[:2]
    qb = jnp.moveaxis(q.reshape((b, n // Q_BLOCK, Q_BLOCK) + q.shape[2:]), 1, 0)
    out = lax.map(lambda qblk: diff_attention(qblk, k, v, lam), qb)
    return jnp.moveaxis(out, 0, 1).reshape(b, n, C_HEADS, C_V_DIM)


def diff_head_out(o, g, lam_init):
    b, n = o.shape[:2]
    return (rmsnorm(o, g) * (1 - lam_init)).reshape(b, n, C_V_WIDTH)


def chunk_spatial_gate(u, v, ln_g, ln_b, ws, bs):
    b, n, _ = v.shape
    vc = layernorm(v, ln_g, ln_b).reshape(b, n // CHUNK, CHUNK, A_GROUPS, A_GROUP_DIM)
    mixed = jnp.einsum('gts,bcsge->bctge', ws, vc) + bs.T[:, :, None]
    return u * mixed.reshape(b, n, A_WIDTH)


def short_gated_conv(bg, cg, h, w):
    z = cg * h
    n = z.shape[1]
    pad = CONV_W // 2
    zp = jnp.pad(z, ((0, 0), (pad, pad), (0, 0)))
    conv = w[0] * zp[:, 0:n]
    for j in range(1, CONV_W):
        conv = conv + w[j] * zp[:, j:j + n]
    return bg * conv


def local_mixers(p, ln_g, ln_b, ws, bs, conv_w):
    ya = chunk_spatial_gate(jax.nn.gelu(p[0]), jax.nn.gelu(p[1]), ln_g, ln_b, ws, bs)
    yb = short_gated_conv(p[2], p[3], p[4], conv_w)
    return ya, yb


def merge_branches(ya, yb, yc, gate_pre, b_gate, w_a_out, w_b_out, w_c_out, w_o):
    ga, gb, gc = jnp.split(jax.nn.sigmoid(gate_pre + b_gate), N_BRANCH, axis=-1)
    y = ga * (ya @ w_a_out) + gb * (yb @ w_b_out) + gc * (yc @ w_c_out)
    return y @ w_o


def swiglu(h, wg, wu, wd):
    return (jax.nn.silu(h @ wg) * (h @ wu)) @ wd


def moe_swiglu(h, w_router, wg, wu, wd):
    logits = (h @ w_router).astype(jnp.float32)
    top_v, top_i = lax.top_k(logits, TOP_K)
    top_w = jax.nn.softmax(top_v, axis=-1)
    comb = jnp.sum(jax.nn.one_hot(top_i, N_EXPERTS, dtype=jnp.float32) * top_w[..., None], axis=-2)
    comb = comb.astype(h.dtype)
    out = jnp.zeros_like(h)
    for e in range(N_EXPERTS):
        out = out + comb[..., e:e + 1] * swiglu(h, wg[e], wu[e], wd[e])
    return out


def channel_mixer(h, l, ff_w_gate, ff_w_up, ff_w_down, moe_w_router, moe_w_gate, moe_w_up, moe_w_down):
    i = l // 2
    if l % 2 == 0:
        return swiglu(h, ff_w_gate[i], ff_w_up[i], ff_w_down[i])
    return moe_swiglu(h, moe_w_router[i], moe_w_gate[i], moe_w_up[i], moe_w_down[i])


def _normal(key, shape, scale):
    return jax.random.normal(key, shape, jnp.float32) * scale


def setup_inputs(seed: int = 0) -> dict:
    key = jax.random.key(seed)
    ks = iter(jax.random.split(key, 32))
    D = D_MODEL
    L = DEPTH
    return {
        'x': _normal(next(ks), (BATCH, SEQ, D), 1.0),
        'c': _normal(next(ks), (BATCH, D), 1.0),
        'ctx': _normal(next(ks), (BATCH, CTX_LEN, D), 1.0),
        'c_ctx': _normal(next(ks), (D,), 1.0),
        'w_ada': _normal(next(ks), (L, D, N_MOD * D), 0.5 * D ** -0.5),
        'b_ada': _normal(next(ks), (L, N_MOD * D), 0.02),
        'norm1_g': 1.0 + _normal(next(ks), (L, D), 0.02),
        'norm2_g': 1.0 + _normal(next(ks), (L, D), 0.02),
        'w_in': _normal(next(ks), (L, D, IN_COLS), D ** -0.5),
        'b_gate': _normal(next(ks), (L, N_BRANCH * D), 0.02),
        'a_ln_g': 1.0 + _normal(next(ks), (L, A_WIDTH), 0.02),
        'a_ln_b': _normal(next(ks), (L, A_WIDTH), 0.02),
        'a_ws': _normal(next(ks), (L, A_GROUPS, CHUNK, CHUNK), CHUNK ** -0.5),
        'a_bs': 1.0 + _normal(next(ks), (L, A_GROUPS, CHUNK), 0.02),
        'b_conv': _normal(next(ks), (L, CONV_W, B_WIDTH), CONV_W ** -0.5),
        'c_lambda': _normal(next(ks), (L, 4, C_HEAD_DIM), 0.1),
        'c_subln_g': 1.0 + _normal(next(ks), (L, C_V_DIM), 0.02),
        'w_a_out': _normal(next(ks), (L, A_WIDTH, D), A_WIDTH ** -0.5),
        'w_b_out': _normal(next(ks), (L, B_WIDTH, D), B_WIDTH ** -0.5),
        'w_c_out': _normal(next(ks), (L, C_V_WIDTH, D), C_V_WIDTH ** -0.5),
        'w_o': _normal(next(ks), (L, D, D), D ** -0.5),
        'ff_w_gate': _normal(next(ks), (N_DENSE_LAYERS, D, D_FF_DENSE), D ** -0.5),
        'ff_w_up': _normal(next(ks), (N_DENSE_LAYERS, D, D_FF_DENSE), D ** -0.5),
        'ff_w_down': _normal(next(ks), (N_DENSE_LAYERS, D_FF_DENSE, D), D_FF_DENSE ** -0.5),
        'moe_w_router': _normal(next(ks), (N_MOE_LAYERS, D, N_EXPERTS), D ** -0.5),
        'moe_w_gate': _normal(next(ks), (N_MOE_LAYERS, N_EXPERTS, D, D_FF_EXPERT), D ** -0.5),
        'moe_w_up': _normal(next(ks), (N_MOE_LAYERS, N_EXPERTS, D, D_FF_EXPERT), D ** -0.5),
        'moe_w_down': _normal(next(ks), (N_MOE_LAYERS, N_EXPERTS, D_FF_EXPERT, D), D_FF_EXPERT ** -0.5),
        'final_norm_g': 1.0 + _normal(next(ks), (D,), 0.02),
    }


def reference(x, c, ctx, c_ctx, w_ada, b_ada, norm1_g, norm2_g, w_in, b_gate, a_ln_g, a_ln_b,
              a_ws, a_bs, b_conv, c_lambda, c_subln_g, w_a_out, w_b_out, w_c_out, w_o,
              ff_w_gate, ff_w_up, ff_w_down, moe_w_router, moe_w_gate, moe_w_up, moe_w_down,
              final_norm_g):
    n = x.shape[1]
    cos, sin = axial_rope_tables(n)
    xc = ctx
    for l in range(DEPTH):
        last = l == DEPTH - 1
        mod = jax.nn.silu(c) @ w_ada[l] + b_ada[l]
        mod_c = jax.nn.silu(c_ctx) @ w_ada[l] + b_ada[l]
        sh1, sc1, g1, sh2, sc2, g2 = jnp.split(mod[:, None, :], N_MOD, axis=-1)
        csh1, csc1, cg1, csh2, csc2, cg2 = jnp.split(mod_c, N_MOD, axis=-1)
        lam_init = 0.8 - 0.6 * math.exp(-0.3 * l)
        lq1, lk1, lq2, lk2 = c_lambda[l]
        lam = jnp.exp(jnp.sum(lq1 * lk1)) - jnp.exp(jnp.sum(lq2 * lk2)) + lam_init

        h = modulate(x, norm1_g[l], sh1, sc1)
        hc = modulate(xc, norm1_g[l], csh1, csc1)
        p = jnp.split(h @ w_in[l], SPLITS, axis=-1)
        if last:
            kv_c = hc @ w_in[l][:, OFF_CK:OFF_GATE]
            kc_raw, vc_raw = jnp.split(kv_c, [C_QK_WIDTH], axis=-1)
        else:
            pc = jnp.split(hc @ w_in[l], SPLITS, axis=-1)
            kc_raw, vc_raw = pc[6], pc[7]
        k_ctx = qk_heads(kc_raw)
        v_ctx = v_heads(vc_raw)

        ya, yb = local_mixers(p, a_ln_g[l], a_ln_b[l], a_ws[l], a_bs[l], b_conv[l])
        q = apply_rope(qk_heads(p[5]), cos, sin)
        k = apply_rope(qk_heads(p[6]), cos, sin)
        yc = diff_attention_latent(q, k, v_heads(p[7]), k_ctx, v_ctx, lam)
        yc = diff_head_out(yc, c_subln_g[l], lam_init)
        x = x + g1 * merge_branches(ya, yb, yc, p[8], b_gate[l], w_a_out[l], w_b_out[l], w_c_out[l], w_o[l])

        x = x + g2 * channel_mixer(modulate(x, norm2_g[l], sh2, sc2), l, ff_w_gate, ff_w_up, ff_w_down,
                                   moe_w_router, moe_w_gate, moe_w_up, moe_w_down)

        if not last:
            ya_c, yb_c = local_mixers(pc, a_ln_g[l], a_ln_b[l], a_ws[l], a_bs[l], b_conv[l])
            yc_c = diff_head_out(diff_attention(qk_heads(pc[5]), k_ctx, v_ctx, lam), c_subln_g[l], lam_init)
            xc = xc + cg1 * merge_branches(ya_c, yb_c, yc_c, pc[8], b_gate[l], w_a_out[l], w_b_out[l],
                                           w_c_out[l], w_o[l])
            xc = xc + cg2 * channel_mixer(modulate(xc, norm2_g[l], csh2, csc2), l, ff_w_gate, ff_w_up,
                                          ff_w_down, moe_w_router, moe_w_gate, moe_w_up, moe_w_down)
    return rmsnorm(x, final_norm_g)
```

```python
import math
from contextlib import ExitStack
import numpy as np
import concourse.bass as bass
import concourse.mybir as mybir
from concourse.bass_utils import run_bass_kernel_spmd

F32 = mybir.dt.float32
BF16 = mybir.dt.bfloat16
AF = mybir.ActivationFunctionType
ALU = mybir.AluOpType
AX = mybir.AxisListType

D = 1024
NL = 4096
NCX = 256
NT = NL + NCX
L = 2
EPS = 1e-6
OFF_AU, OFF_AV, OFF_BB, OFF_BC, OFF_BH = 0, 512, 1024, 1536, 2048
OFF_CQ, OFF_CK, OFF_CV, OFF_GATE = 2560, 3584, 4608, 5632
IN_COLS = 8704
WCOLS = IN_COLS + 2048
OFF_QP, OFF_KP = IN_COLS, IN_COLS + 1024
DFF = 2816
NE = 8
DFE = 3584
NJ_FF = DFF // 128
NJ_E = DFE // 128
BLKS = [(i * 512, 512) for i in range(8)] + [(NL, NCX)]


class Eng:
    def __init__(self, name, e, sem):
        self.name, self.e, self.sem, self.cnt, self.seen = name, e, sem, 0, {}


class TB:
    def __init__(self, name):
        self.name, self.w, self.r, self.dsem = name, None, {}, None


class DSem:
    def __init__(self, sem):
        self.sem, self.cnt = sem, 0


class Prog:
    def __init__(self, nc, es):
        self.nc, self.es = nc, es
        self.nsem = 0
        self.pe = Eng("pe", nc.tensor, self.mksem("s_pe"))
        self.act = Eng("act", nc.scalar, self.mksem("s_act"))
        self.dve = Eng("dve", nc.vector, self.mksem("s_dve"))
        self.pool = Eng("pool", nc.gpsimd, self.mksem("s_pool"))
        self.sp = Eng("sp", nc.sync, self.mksem("s_sp"))
        self.engs = [self.pe, self.act, self.dve, self.pool, self.sp]
        self.bar = self.mksem("s_bar")
        self.barcnt = 0
        self.tbs = []
        self.free_dsems = []
        self.dsems = []
        self.keep = set()
        self.ninst = 0

    def mksem(self, name):
        self.nsem += 1
        return self.es.enter_context(self.nc.semaphore(name))

    def tb(self, name, keep=False):
        t = TB(name)
        self.tbs.append(t)
        if keep:
            self.keep.add(name)
        return t

    def _wait(self, E, ev, raw=False):
        sem, val = ev
        if sem is E.sem and not (raw and E.name in ("act", "dve", "pool")):
            return
        k = id(sem)
        if E.seen.get(k, 0) >= val:
            return
        E.e.wait_ge(sem, val)
        self.ninst += 1
        E.seen[k] = val

    def _deps(self, E, reads, writes):
        for b in reads:
            if b.w is not None:
                self._wait(E, b.w, raw=True)
        for b in writes:
            if b.w is not None:
                self._wait(E, b.w)
            for ev in b.r.values():
                self._wait(E, ev)

    def _post(self, ev, reads, writes):
        for b in reads:
            b.r[id(ev[0])] = ev
        for b in writes:
            b.w = ev
            b.r = {}

    def op(self, E, fn, reads=(), writes=(), inc=True):
        self._deps(E, reads, writes)
        ins = fn(E.e)
        self.ninst += 1
        if inc:
            E.cnt += 1
            ins.then_inc(E.sem, 1)
            ev = (E.sem, E.cnt)
        else:
            ev = (E.sem, E.cnt + 1)
        self._post(ev, reads, writes)

    def dma(self, out, in_, sb, load, Q=None):
        Q = Q or self.sp
        if sb.dsem is None:
            if self.free_dsems:
                sb.dsem = self.free_dsems.pop()
            else:
                sb.dsem = DSem(self.mksem("dsem%d" % len(self.dsems)))
                self.dsems.append(sb.dsem)
        ds = sb.dsem
        reads = [] if load else [sb]
        writes = [sb] if load else []
        self._deps(Q, reads, writes)
        Q.e.dma_start(out=out, in_=in_).then_inc(ds.sem, 16)
        self.ninst += 1
        ds.cnt += 16
        self._post((ds.sem, ds.cnt), reads, writes)

    def barrier(self):
        sp = self.sp
        for E in self.engs:
            if E is not sp and E.cnt > 0:
                self._wait(sp, (E.sem, E.cnt))
        for ds in self.dsems:
            if ds.cnt:
                self._wait(sp, (ds.sem, ds.cnt))
        self.barcnt += 1
        sp.e.sem_inc(self.bar, 1)
        for E in self.engs:
            if E is sp:
                continue
            E.e.wait_ge(self.bar, self.barcnt)
            for E2 in self.engs:
                if E2.cnt:
                    E.seen[id(E2.sem)] = E2.cnt
            for ds in self.dsems:
                E.seen[id(ds.sem)] = ds.cnt
        for t in self.tbs:
            t.w = None
            t.r = {}
            if t.dsem is not None:
                self.free_dsems.append(t.dsem)
                t.dsem = None
        self.tbs = [t for t in self.tbs if t.name in self.keep]


class _Stop(Exception):
    pass


def build_nc(seqs=(0, 1), layers=(0, 1), debug_dump=False, stop=None):
    nc = bass.Bass("TRN2", target_bir_lowering=False)

    _uid = [0]

    def sbt(name, shape, dt):
        _uid[0] += 1
        return nc.sbuf_tensor("%s_u%d" % (name, _uid[0]), list(shape), dt)

    def din(name, shape, dt=F32):
        return nc.dram_tensor(name, list(shape), dt, kind="ExternalInput").ap()

    def dscr(name, shape, dt):
        return nc.dram_tensor(name, list(shape), dt).ap()

    x2 = din("x2", [2, NL, D])
    ctx2 = din("ctx2", [2, NCX, D])
    cT = din("cT", [128, 8, 3])
    w_ada = din("w_ada", [L, D, 6 * D])
    b_adaT = din("b_adaT", [L, 128, 48])
    n1gT = din("n1gT", [L, 128, 8])
    n2gT = din("n2gT", [L, 128, 8])
    fgT = din("fgT", [128, 8])
    w_in = din("w_in", [L, D, IN_COLS])
    w_perm = din("w_perm", [L, D, 2048])
    b_gateT = din("b_gateT", [L, 128, 24])
    a_ln_g = din("a_ln_g", [L, 512])
    a_ln_b = din("a_ln_b", [L, 512])
    a_wsT = din("a_wsT", [L, 128, 8, 128])
    a_bsT = din("a_bsT", [L, 128, 8])
    b_convT = din("b_convT", [L, 128, 4, 3])
    c_lambda = din("c_lambda", [L, 256])
    c_subln_g = din("c_subln_g", [L, 128])
    w_a_out = din("w_a_out", [L, 512, D])
    w_b_out = din("w_b_out", [L, 512, D])
    w_c_out = din("w_c_out", [L, D, D])
    w_o = din("w_o", [L, D, D])
    ff_wg = din("ff_wg", [D, DFF])
    ff_wu = din("ff_wu", [D, DFF])
    ff_wd = din("ff_wd", [DFF, D])
    moe_wr = din("moe_wr", [128, 8, NE])
    moe_wg = din("moe_wg", [NE, D, DFE])
    moe_wu = din("moe_wu", [NE, D, DFE])
    moe_wd = din("moe_wd", [NE, DFE, D])
    cosT = din("cosT", [128, NT])
    sinT = din("sinT", [128, NT])
    ident_d = din("ident", [128, 128])
    out2 = nc.dram_tensor("out2", [2, NL, D], F32, kind="ExternalOutput").ap()

    win_bf = dscr("win_bf", [L, D, WCOLS], BF16)
    wa_bf = dscr("wa_bf", [L, 512, D], BF16)
    wb_bf = dscr("wb_bf", [L, 512, D], BF16)
    wc_bf = dscr("wc_bf", [L, D, D], BF16)
    wo_bf = dscr("wo_bf", [L, D, D], BF16)
    fg_bf = dscr("fg_bf", [D, DFF], BF16)
    fu_bf = dscr("fu_bf", [D, DFF], BF16)
    fd_bf = dscr("fd_bf", [DFF, D], BF16)
    mg_bf = dscr("mg_bf", [NE, D, DFE], BF16)
    mu_bf = dscr("mu_bf", [NE, D, DFE], BF16)
    md_bf = dscr("md_bf", [NE, DFE, D], BF16)
    kind = "ExternalOutput" if debug_dump else "Internal"
    xT_d = nc.dram_tensor("xT_d", [2, D, NT], F32, kind=kind).ap()
    yaT_d = nc.dram_tensor("yaT_d", [2, 512, NT], BF16, kind=kind).ap()
    ybT_d = nc.dram_tensor("ybT_d", [2, 512, NT], BF16, kind=kind).ap()
    ycT_d = nc.dram_tensor("ycT_d", [2, D, NT], BF16, kind=kind).ap()
    hT_dbg = nc.dram_tensor("hT_dbg", [128, 8, NT], BF16, kind=kind).ap()

    with ExitStack() as es:
        P = Prog(nc, es)
        pe, act, dve, sp = P.pe, P.act, P.dve, P.sp

        def sb(name, shape, dt):
            return es.enter_context(sbt(name, list(shape), dt))

        psT = [es.enter_context(nc.psum_tensor("ps%d" % i, [128, 1024], F32)) for i in range(4)]
        bank = [P.tb("bank%d" % i, True) for i in range(8)]

        def psb(i, n=512, off=0):
            t = psT[i // 2]
            o = (i % 2) * 512 + off
            return t[:, o:o + n]

        ident = sb("ident", [128, 128], F32); ident_t = P.tb("ident", True)
        ones_ms = sb("ones_ms", [128, 128], BF16)
        epsT = sb("epsT", [128, 1], F32)
        modT = sb("modT", [128, L, 48, 3], F32)
        der = sb("der", [128, L, 3, 6, 8], F32)
        neglam = sb("neglam", [128, L], F32)
        gsub = sb("gsub", [128, L, 128], F32)
        bgT = sb("bgT", [128, L, 24], F32)
        convT = sb("convT", [128, L, 4, 3], F32)
        fgs = sb("fgs", [128, 8], F32)
        consts = P.tb("consts", True)

        P.dma(ident[:], ident_d[:, :], ident_t, True)
        P.op(dve, lambda e: e.memset(ones_ms[:], 1.0 / D), writes=[consts])
        P.op(dve, lambda e: e.memset(epsT[:], EPS), writes=[consts])
        cst2 = P.tb("cst2", True)
        for l in range(L):
            P.dma(bgT[:, l, :], b_gateT[l, :, :], cst2, True)
            P.dma(convT[:, l, :, :], b_convT[l, :, :, :], cst2, True)
        P.dma(fgs[:], fgT[:, :], cst2, True)

        def cast_copy(i, out, in_, reads, writes):
            E = (act, dve)[i % 2]
            if E is act:
                P.op(act, lambda e: e.copy(out, in_), reads=reads, writes=writes)
            else:
                P.op(dve, lambda e: e.tensor_copy(out, in_), reads=reads, writes=writes)

        def phase0():
            with ExitStack() as ps:
                CH = 4096
                st32 = [ps.enter_context(sbt("st32_%d" % i, [128, CH], F32)) for i in range(3)]
                st16 = [ps.enter_context(sbt("st16_%d" % i, [128, CH], BF16)) for i in range(3)]
                t32 = [P.tb("st32_%d" % i) for i in range(3)]
                t16 = [P.tb("st16_%d" % i) for i in range(3)]
                k = [0]

                def conv2d(src, dst):
                    M = src.shape[1]
                    for o in range(0, M, CH):
                        n = min(CH, M - o)
                        i = k[0] % 3
                        k[0] += 1
                        P.dma(st32[i][:, 0:n], src[:, o:o + n], t32[i], True)
                        cast_copy(k[0], st16[i][:, 0:n], st32[i][:, 0:n], [t32[i]], [t16[i]])
                        P.dma(dst[:, o:o + n], st16[i][:, 0:n], t16[i], False)

                def flat(ap2):
                    return ap2.rearrange("(p r) c -> p (r c)", p=128)

                for l in layers:
                    for r0 in range(0, D, 128):
                        for c0 in range(0, IN_COLS, CH):
                            n = min(CH, IN_COLS - c0)
                            i = k[0] % 3
                            k[0] += 1
                            P.dma(st32[i][:, 0:n], w_in[l, r0:r0 + 128, c0:c0 + n], t32[i], True)
                            cast_copy(k[0], st16[i][:, 0:n], st32[i][:, 0:n], [t32[i]], [t16[i]])
                            P.dma(win_bf[l, r0:r0 + 128, c0:c0 + n], st16[i][:, 0:n], t16[i], False)
                        i = k[0] % 3
                        k[0] += 1
                        P.dma(st32[i][:, 0:2048], w_perm[l, r0:r0 + 128, :], t32[i], True)
                        cast_copy(k[0], st16[i][:, 0:2048], st32[i][:, 0:2048], [t32[i]], [t16[i]])
                        P.dma(win_bf[l, r0:r0 + 128, IN_COLS:WCOLS], st16[i][:, 0:2048], t16[i], False)
                    conv2d(flat(w_a_out[l]), flat(wa_bf[l]))
                    conv2d(flat(w_b_out[l]), flat(wb_bf[l]))
                    conv2d(flat(w_c_out[l]), flat(wc_bf[l]))
                    conv2d(flat(w_o[l]), flat(wo_bf[l]))
                if 0 in layers:
                    conv2d(flat(ff_wg), flat(fg_bf))
                    conv2d(flat(ff_wu), flat(fu_bf))
                    conv2d(flat(ff_wd), flat(fd_bf))
                if 1 in layers:
                    for e_ in range(NE):
                        conv2d(flat(moe_wg[e_]), flat(mg_bf[e_]))
                        conv2d(flat(moe_wu[e_]), flat(mu_bf[e_]))
                        conv2d(flat(moe_wd[e_]), flat(md_bf[e_]))
                P.barrier()

        def phaseM():
            with ExitStack() as ps:
                cTs = ps.enter_context(sbt("cTs", [128, 8, 3], F32))
                scT = ps.enter_context(sbt("scT", [128, 8, 3], F32))
                wad = [ps.enter_context(sbt("wad%d" % i, [128, 8, 512], F32)) for i in range(2)]
                twad = [P.tb("wad%d" % i) for i in range(2)]
                badT = ps.enter_context(sbt("badT", [128, L, 48], F32))
                ngT = ps.enter_context(sbt("ngT", [128, 2, L, 8], F32))
                lamt = ps.enter_context(sbt("lamt", [128, 256], F32))
                lprod = ps.enter_context(sbt("lprod", [128, 2, 64], F32))
                lsum = ps.enter_context(sbt("lsum", [128, 2], F32))
                lexp = ps.enter_context(sbt("lexp", [128, 2], F32))
                gsr = ps.enter_context(sbt("gsr", [128, 128], F32))
                tmp8 = ps.enter_context(sbt("tmp8", [128, 8], F32))
                tM = P.tb("tM"); tS = P.tb("tS")
                P.dma(cTs[:], cT[:, :, :], tM, True)
                P.op(act, lambda e: e.activation(out=scT[:], in_=cTs[:], func=AF.Silu), reads=[tM], writes=[tS])
                tb_bad = P.tb("badT")
                for l in range(L):
                    P.dma(badT[:, l, :], b_adaT[l, :, :], tb_bad, True)
                    P.dma(ngT[:, 0, l, :], n1gT[l, :, :], tb_bad, True)
                    P.dma(ngT[:, 1, l, :], n2gT[l, :, :], tb_bad, True)
                tmod = P.tb("modT")
                for l in range(L):
                    pb = bank[l]
                    for grp in range(12):
                        i = grp % 2
                        P.dma(wad[i][:], w_ada[l, :, grp * 512:(grp + 1) * 512].rearrange("(kc p) c -> p kc c", p=128), twad[i], True)
                        for cc in range(4):
                            col = (grp * 4 + cc) * 3
                            for kc in range(8):
                                P.op(pe, lambda e, i=i, cc=cc, kc=kc, col=col, l=l: e.matmul(
                                    psb(l, 3, col), wad[i][:, kc, cc * 128:(cc + 1) * 128], scT[:, kc, :],
                                    start=(kc == 0), stop=(kc == 7)),
                                    reads=[twad[i], tS], writes=[pb], inc=(kc == 7))
                    P.op(dve, lambda e, l=l: e.tensor_tensor(
                        modT[:, l, :, :], psb(l, 144).rearrange("p (c j) -> p c j", j=3),
                        badT[:, l, :].unsqueeze(2).to_broadcast([128, 48, 3]), ALU.add),
                        reads=[pb, tb_bad], writes=[tmod])
                    for j in range(3):
                        for half in range(2):
                            base = half * 24
                            P.op(dve, lambda e, l=l, j=j, base=base: e.tensor_scalar(
                                tmp8[:], modT[:, l, base + 8:base + 16, j], 1.0, None, ALU.add),
                                reads=[tmod], writes=[tM])
                            P.op(dve, lambda e, l=l, j=j, half=half: e.tensor_tensor(
                                der[:, l, j, half * 3 + 0, :], tmp8[:], ngT[:, half, l, :], ALU.mult),
                                reads=[tM, tb_bad], writes=[consts])
                            P.op(dve, lambda e, l=l, j=j, half=half, base=base: e.tensor_copy(
                                der[:, l, j, half * 3 + 1, :], modT[:, l, base:base + 8, j]),
                                reads=[tmod], writes=[consts])
                            P.op(dve, lambda e, l=l, j=j, half=half, base=base: e.tensor_copy(
                                der[:, l, j, half * 3 + 2, :], modT[:, l, base + 16:base + 24, j]),
                                reads=[tmod], writes=[consts])
                    lam_init = 0.8 - 0.6 * math.exp(-0.3 * l)
                    tl = P.tb("lam%d" % l)
                    P.dma(lamt[:], c_lambda[l, :].partition_broadcast(128), tl, True)
                    P.op(dve, lambda e: e.tensor_tensor(lprod[:, 0, :], lamt[:, 0:64], lamt[:, 64:128], ALU.mult), reads=[tl], writes=[tM])
                    P.op(dve, lambda e: e.tensor_tensor(lprod[:, 1, :], lamt[:, 128:192], lamt[:, 192:256], ALU.mult), reads=[tl], writes=[tM])
                    P.op(dve, lambda e: e.tensor_reduce(lsum[:], lprod[:], AX.X, ALU.add), reads=[tM], writes=[tM])
                    P.op(act, lambda e: e.activation(out=lexp[:], in_=lsum[:], func=AF.Exp), reads=[tM], writes=[tS])
                    P.op(dve, lambda e, l=l, li=lam_init: e.scalar_tensor_tensor(
                        neglam[:, l:l + 1], lexp[:, 1:2], -li, lexp[:, 0:1], ALU.add, ALU.subtract),
                        reads=[tS], writes=[consts])
                    tg = P.tb("gsr%d" % l)
                    P.dma(gsr[:], c_subln_g[l, :].partition_broadcast(128), tg, True)
                    P.op(act, lambda e, l=l, li=lam_init: e.mul(gsub[:, l, :], gsr[:], 1.0 - li), reads=[tg], writes=[consts])
                P.barrier()

        def norm_mod(xblk, txb, n, l, j, slot0, dst_fn, tdst, sq, tsq, rstd, trs, tmpf, ttmp, pbank, post=None):
            nsub = (n + 511) // 512
            for sub in range(nsub):
                c0 = sub * 512
                m = min(512, n - c0)
                P.op(act, lambda e: e.activation(out=sq[:, :, 0:m], in_=xblk[:, :, c0:c0 + m], func=AF.Square),
                     reads=[txb], writes=[tsq])
                for kc in range(8):
                    P.op(pe, lambda e, kc=kc: e.matmul(psb(pbank, m), ones_ms[:], sq[:, kc, 0:m], start=(kc == 0), stop=(kc == 7)),
                         reads=[tsq, consts], writes=[bank[pbank]], inc=(kc == 7))
                P.op(act, lambda e: e.activation(out=rstd[:, c0:c0 + m], in_=psb(pbank, m), func=AF.Sqrt, bias=epsT[:, 0:1]),
                     reads=[bank[pbank], consts], writes=[trs])
                P.op(dve, lambda e: e.reciprocal(rstd[:, c0:c0 + m], rstd[:, c0:c0 + m]), reads=[trs], writes=[trs])
            for kc in range(8):
                i = kc % 2
                P.op(dve, lambda e, kc=kc, i=i: e.scalar_tensor_tensor(
                    tmpf[i][:, 0:n], xblk[:, kc, 0:n], der[:, l, j, slot0, kc:kc + 1], rstd[:, 0:n], ALU.mult, ALU.mult),
                    reads=[txb, trs, consts], writes=[ttmp[i]])
                if post is not None:
                    post(kc, i)
                else:
                    P.op(act, lambda e, kc=kc, i=i: e.activation(
                        out=dst_fn(kc), in_=tmpf[i][:, 0:n], func=AF.Identity, bias=der[:, l, j, slot0 + 1, kc:kc + 1]),
                        reads=[ttmp[i], consts], writes=[tdst])

        def wcols(l, c0, ncol):
            return win_bf[l, :, c0:c0 + ncol].rearrange("(kc p) c -> p kc c", p=128)

        def seq_layer(s, l):
            last = (l == L - 1)
            nblk_full = 9
            nblk_lat = 8 if last else 9
            with ExitStack() as sl:
                hT = sl.enter_context(sbt("hT", [128, 8, NT], BF16))
                thT = [P.tb("hT%d" % b, True) for b in range(9)]

                with ExitStack() as ps:
                    xblk = [ps.enter_context(sbt("xblkA%d" % i, [128, 8, 512], F32)) for i in range(2)]
                    txb = [P.tb("xblkA%d" % i) for i in range(2)]
                    sq = ps.enter_context(sbt("sqA", [128, 8, 512], BF16)); tsq = P.tb("sqA")
                    rstd = ps.enter_context(sbt("rstdA", [128, 512], F32)); trs = P.tb("rstdA")
                    tmpf = [ps.enter_context(sbt("tmpA%d" % i, [128, 512], F32)) for i in range(2)]
                    ttmp = [P.tb("tmpA%d" % i) for i in range(2)]
                    if l == 0:
                        xtok = [ps.enter_context(sbt("xtok%d" % i, [128, D], F32)) for i in range(2)]
                        txt = [P.tb("xtok%d" % i) for i in range(2)]
                    tcount = 0
                    for b, (c0, n) in enumerate(BLKS):
                        i = b % 2
                        j = s if b < 8 else 2
                        if l == 0:
                            for tt in range(n // 128):
                                k = tcount % 2
                                tcount += 1
                                src = x2[s, c0 + tt * 128:c0 + (tt + 1) * 128, :] if b < 8 else ctx2[s, tt * 128:(tt + 1) * 128, :]
                                P.dma(xtok[k][:], src, txt[k], True)
                                pbk = 2 * (tcount % 2)
                                pst = psT[pbk // 2]
                                for kc in range(8):
                                    P.op(pe, lambda e, kc=kc, k=k, pst=pst: e.transpose(
                                        pst[:, kc * 128:(kc + 1) * 128], xtok[k][:, kc * 128:(kc + 1) * 128], ident[:]),
                                        reads=[txt[k], ident_t], writes=[bank[pbk + kc // 4]], inc=(kc % 4 == 3))
                                P.op(act, lambda e, pst=pst, i=i, tt=tt: e.copy(
                                    xblk[i][:, :, tt * 128:(tt + 1) * 128], pst[:, :].rearrange("p (k t) -> p k t", k=8)),
                                    reads=[bank[pbk], bank[pbk + 1]], writes=[txb[i]])
                            P.dma(xT_d[s, :, c0:c0 + n].rearrange("(kc p) t -> p kc t", p=128), xblk[i][:, :, 0:n], txb[i], False)
                        else:
                            P.dma(xblk[i][:, :, 0:n], xT_d[s, :, c0:c0 + n].rearrange("(kc p) t -> p kc t", p=128), txb[i], True)
                        norm_mod(xblk[i], txb[i], n, l, j, 0, lambda kc, c0=c0, n=n: hT[:, kc, c0:c0 + n], thT[b],
                                 sq, tsq, rstd, trs, tmpf, ttmp, 4 + (b % 2))
                    P.barrier()
                    if debug_dump:
                        tdbg = P.tb("hTdbg")
                        P.dma(hT_dbg[:, :, :], hT[:, :, :], tdbg, False)
                        P.barrier()
                if stop == "A":
                    return True

                with ExitStack() as ps:
                    WA = ps.enter_context(sbt("WA", [128, 8, 1024], BF16)); tWA = P.tb("WA")
                    ws32 = ps.enter_context(sbt("ws32", [128, 8, 128], F32)); tws32 = P.tb("ws32")
                    wsT = ps.enter_context(sbt("wsT", [128, 8, 128], BF16)); twsT = P.tb("wsT")
                    lng = ps.enter_context(sbt("lng", [128, 512], F32))
                    lnb = ps.enter_context(sbt("lnb", [128, 512], F32))
                    bsT = ps.enter_context(sbt("bsT", [128, 8], F32)); tln = P.tb("ln")
                    u_sb = [ps.enter_context(sbt("u_sb%d" % i, [128, 512], F32)) for i in range(2)]
                    v_sb = [ps.enter_context(sbt("v_sb%d" % i, [128, 512], F32)) for i in range(2)]
                    junk = ps.enter_context(sbt("junkB1", [128, 512], F32))
                    vc = [ps.enter_context(sbt("vc%d" % i, [128, 512], BF16)) for i in range(2)]
                    st = [ps.enter_context(sbt("stB1_%d" % i, [128, 8], F32)) for i in range(2)]
                    yaB = [ps.enter_context(sbt("yaB%d" % i, [128, 4, 512], BF16)) for i in range(2)]
                    tu = [P.tb("u%d" % i) for i in range(2)]; tv = [P.tb("v%d" % i) for i in range(2)]
                    tvc = [P.tb("vc%d" % i) for i in range(2)]; tst = [P.tb("st%d" % i) for i in range(2)]
                    tya = [P.tb("yaB%d" % i) for i in range(2)]; tjunk = P.tb("junkB1")
                    P.dma(WA[:, :, 0:512], wcols(l, 0, 512), tWA, True)
                    P.dma(WA[:, :, 512:1024], wcols(l, 512, 512), tWA, True)
                    P.dma(ws32[:], a_wsT[l, :, :, :], tws32, True)
                    P.op(act, lambda e: e.copy(wsT[:], ws32[:]), reads=[tws32], writes=[twsT])
                    P.dma(lng[:], a_ln_g[l, :].partition_broadcast(128), tln, True)
                    P.dma(lnb[:], a_ln_b[l, :].partition_broadcast(128), tln, True)
                    P.dma(bsT[:], a_bsT[l, :, :], tln, True)
                    ntile = nblk_lat * 4 if nblk_lat == 8 else 34
                    for tt in range(ntile):
                        i = tt % 2
                        b = min(tt // 4, 8)
                        bu, bv, bm, bt = (0, 1, 2, 3) if i == 0 else (4, 5, 6, 7)
                        tok = slice(tt * 128, (tt + 1) * 128)
                        for (bk, cs) in ((bu, 0), (bv, 512)):
                            for kc in range(8):
                                P.op(pe, lambda e, kc=kc, bk=bk, cs=cs: e.matmul(
                                    psb(bk), hT[:, kc, tok], WA[:, kc, cs:cs + 512], start=(kc == 0), stop=(kc == 7)),
                                    reads=[thT[b], tWA], writes=[bank[bk]], inc=(kc == 7))
                        P.op(dve, lambda e: e.memset(st[i][:, 0:2], 0.0), writes=[tst[i]])
                        P.op(act, lambda e: e.activation(out=u_sb[i][:], in_=psb(bu), func=AF.Gelu_apprx_tanh),
                             reads=[bank[bu]], writes=[tu[i]])
                        P.op(act, lambda e: e.activation(out=v_sb[i][:], in_=psb(bv), func=AF.Gelu_apprx_tanh, accum_out=st[i][:, 0:1]),
                             reads=[bank[bv]], writes=[tv[i], tst[i]])
                        P.op(act, lambda e: e.activation(out=junk[:], in_=v_sb[i][:], func=AF.Square, accum_out=st[i][:, 1:2]),
                             reads=[tv[i]], writes=[tjunk, tst[i]])
                        P.op(dve, lambda e: e.tensor_scalar(st[i][:, 2:4], st[i][:, 0:2], 1.0 / 512, None, ALU.mult), reads=[tst[i]], writes=[tst[i]])
                        P.op(dve, lambda e: e.tensor_tensor(st[i][:, 4:5], st[i][:, 2:3], st[i][:, 2:3], ALU.mult), reads=[tst[i]], writes=[tst[i]])
                        P.op(dve, lambda e: e.tensor_tensor(st[i][:, 5:6], st[i][:, 3:4], st[i][:, 4:5], ALU.subtract), reads=[tst[i]], writes=[tst[i]])
                        P.op(act, lambda e: e.activation(out=st[i][:, 6:7], in_=st[i][:, 5:6], func=AF.Sqrt, bias=epsT[:, 0:1]), reads=[tst[i], consts], writes=[tst[i]])
                        P.op(dve, lambda e: e.reciprocal(st[i][:, 7:8], st[i][:, 6:7]), reads=[tst[i]], writes=[tst[i]])
                        P.op(dve, lambda e: e.tensor_scalar(v_sb[i][:], v_sb[i][:], st[i][:, 2:3], st[i][:, 7:8], ALU.subtract, ALU.mult),
                             reads=[tst[i], tv[i]], writes=[tv[i]])
                        P.op(dve, lambda e: e.tensor_tensor(v_sb[i][:], v_sb[i][:], lng[:], ALU.mult), reads=[tv[i], tln], writes=[tv[i]])
                        P.op(dve, lambda e: e.tensor_tensor(vc[i][:], v_sb[i][:], lnb[:], ALU.add), reads=[tv[i], tln], writes=[tvc[i]])
                        for g in range(8):
                            P.op(pe, lambda e, g=g: e.matmul(psb(bm, 64, g * 64), wsT[:, g, :], vc[i][:, g * 64:(g + 1) * 64], start=True, stop=True),
                                 reads=[twsT, tvc[i]], writes=[bank[bm]], inc=(g == 7))
                        P.op(dve, lambda e: e.tensor_tensor(
                            v_sb[i][:].rearrange("p (g e) -> p g e", g=8), psb(bm).rearrange("p (g e) -> p g e", g=8),
                            bsT[:, :].unsqueeze(2).to_broadcast([128, 8, 64]), ALU.add),
                            reads=[bank[bm], tln], writes=[tv[i]])
                        P.op(dve, lambda e: e.tensor_tensor(u_sb[i][:], u_sb[i][:], v_sb[i][:], ALU.mult), reads=[tv[i], tu[i]], writes=[tu[i]])
                        for c in range(4):
                            P.op(pe, lambda e, c=c: e.transpose(psb(bt, 128, c * 128), u_sb[i][:, c * 128:(c + 1) * 128], ident[:]),
                                 reads=[tu[i], ident_t], writes=[bank[bt]], inc=(c == 3))
                        yb_i = (tt // 4) % 2
                        q = tt % 4
                        P.op(act, lambda e, yb_i=yb_i, q=q: e.copy(
                            yaB[yb_i][:, :, q * 128:(q + 1) * 128], psb(bt).rearrange("p (c t) -> p c t", c=4)),
                            reads=[bank[bt]], writes=[tya[yb_i]])
                        c0, n = BLKS[b]
                        if (tt + 1) * 128 == c0 + n:
                            P.dma(yaT_d[s, :, c0:c0 + n].rearrange("(c p) t -> p c t", p=128), yaB[yb_i][:, :, 0:n], tya[yb_i], False)
                    P.barrier()
                if stop == "B1":
                    return True

                with ExitStack() as ps:
                    WB = [ps.enter_context(sbt("WB%d" % i, [128, 3, 8, 128], BF16)) for i in range(2)]
                    tWB = [P.tb("WB%d" % i) for i in range(2)]
                    ZW = NT + 4
                    zf = ps.enter_context(sbt("zf", [128, ZW], F32)); tzf = P.tb("zf")
                    bgf = ps.enter_context(sbt("bgf", [128, NT], F32)); tbg = P.tb("bgf")
                    p4 = [ps.enter_context(sbt("p4_%d" % i, [128, 512], F32)) for i in range(2)]
                    tp4 = [P.tb("p4_%d" % i) for i in range(2)]
                    cv = [ps.enter_context(sbt("cv%d" % i, [128, 512], F32)) for i in range(2)]
                    tcv = [P.tb("cv%d" % i) for i in range(2)]
                    ybo = [ps.enter_context(sbt("ybo%d" % i, [128, 512], BF16)) for i in range(2)]
                    tybo = [P.tb("ybo%d" % i) for i in range(2)]
                    P.op(dve, lambda e: e.memset(zf[:], 0.0), writes=[tzf])

                    def zoff(b):
                        return 1 + BLKS[b][0] if b < 8 else NL + 2

                    def loadWB(fc):
                        i = fc % 2
                        for k3, off in enumerate((OFF_BB, OFF_BC, OFF_BH)):
                            P.dma(WB[i][:, k3, :, :], wcols(l, off + fc * 128, 128), tWB[i], True)
                    loadWB(0)
                    cnt = 0
                    for fc in range(4):
                        i = fc % 2
                        if fc + 1 < 4:
                            loadWB(fc + 1)
                        for b in range(nblk_lat):
                            c0, n = BLKS[b]
                            pbs = (0, 1, 2) if cnt % 2 == 0 else (3, 4, 5)
                            k2 = cnt % 2
                            cnt += 1
                            for k3 in range(3):
                                for kc in range(8):
                                    P.op(pe, lambda e, k3=k3, kc=kc: e.matmul(
                                        psb(pbs[k3], n), WB[i][:, k3, kc, :], hT[:, kc, c0:c0 + n], start=(kc == 0), stop=(kc == 7)),
                                        reads=[tWB[i], thT[b]], writes=[bank[pbs[k3]]], inc=(kc == 7))
                            P.op(act, lambda e: e.copy(p4[k2][:, 0:n], psb(pbs[2], n)), reads=[bank[pbs[2]]], writes=[tp4[k2]])
                            P.op(dve, lambda e: e.tensor_tensor(zf[:, zoff(b):zoff(b) + n], psb(pbs[1], n), p4[k2][:, 0:n], ALU.mult),
                                 reads=[bank[pbs[1]], tp4[k2]], writes=[tzf])
                            P.op(act, lambda e: e.copy(bgf[:, c0:c0 + n], psb(pbs[0], n)), reads=[bank[pbs[0]]], writes=[tbg])
                        for b in range(nblk_lat):
                            c0, n = BLKS[b]
                            k2 = b % 2
                            z0 = zoff(b)
                            P.op(dve, lambda e: e.tensor_scalar(cv[k2][:, 0:n], zf[:, z0 - 1:z0 - 1 + n], convT[:, l, fc, 0:1], None, ALU.mult),
                                 reads=[tzf, cst2], writes=[tcv[k2]])
                            P.op(dve, lambda e: e.scalar_tensor_tensor(cv[k2][:, 0:n], zf[:, z0:z0 + n], convT[:, l, fc, 1:2], cv[k2][:, 0:n], ALU.mult, ALU.add),
                                 reads=[tzf, cst2, tcv[k2]], writes=[tcv[k2]])
                            P.op(dve, lambda e: e.scalar_tensor_tensor(cv[k2][:, 0:n], zf[:, z0 + 1:z0 + 1 + n], convT[:, l, fc, 2:3], cv[k2][:, 0:n], ALU.mult, ALU.add),
                                 reads=[tzf, cst2, tcv[k2]], writes=[tcv[k2]])
                            P.op(dve, lambda e: e.tensor_tensor(ybo[k2][:, 0:n], cv[k2][:, 0:n], bgf[:, c0:c0 + n], ALU.mult),
                                 reads=[tcv[k2], tbg], writes=[tybo[k2]])
                            P.dma(ybT_d[s, fc * 128:(fc + 1) * 128, c0:c0 + n], ybo[k2][:, 0:n], tybo[k2], False)
                    P.barrier()
                if stop == "B2":
                    return True

                with ExitStack() as ps:
                    cosS = ps.enter_context(sbt("cosS", [128, NT], F32))
                    sinS = ps.enter_context(sbt("sinS", [128, NT], F32)); ttab = P.tb("tab")
                    P.dma(cosS[:], cosT[:, :], ttab, True)
                    P.dma(sinS[:], sinT[:, :], ttab, True)
                    W5 = [ps.enter_context(sbt("W5_%d" % i, [128, 5, 8, 128], BF16)) for i in range(2)]
                    tW5 = [P.tb("W5_%d" % i) for i in range(2)]
                    QT = ps.enter_context(sbt("QT", [128, NT], BF16)); tQT = P.tb("QT")
                    KT = ps.enter_context(sbt("KT", [128, NT], BF16)); tKT = P.tb("KT")
                    VH = ps.enter_context(sbt("VH", [128, 34, 130], BF16)); tVH = P.tb("VH")
                    ycH = ps.enter_context(sbt("ycH", [128, NT], BF16)); tycH = P.tb("ycH")
                    r1 = [ps.enter_context(sbt("r1_%d" % i, [128, 512], F32)) for i in range(2)]
                    r2 = [ps.enter_context(sbt("r2_%d" % i, [128, 512], F32)) for i in range(2)]
                    tr1 = [P.tb("r1_%d" % i) for i in range(2)]; tr2 = [P.tb("r2_%d" % i) for i in range(2)]
                    PT = [ps.enter_context(sbt("PT%d" % i, [128, 1024], BF16)) for i in range(3)]
                    tPT = [P.tb("PT%d" % i) for i in range(3)]
                    o_sb = [ps.enter_context(sbt("o_sb%d" % i, [128, 128], F32)) for i in range(2)]
                    to = [P.tb("o_sb%d" % i) for i in range(2)]
                    sm = [ps.enter_context(sbt("sm%d" % i, [128, 8], F32)) for i in range(2)]
                    tsm = [P.tb("sm%d" % i) for i in range(2)]
                    junk = ps.enter_context(sbt("junkB3", [128, 128], F32)); tjunk = P.tb("junkB3")
                    P.op(dve, lambda e: e.memset(VH[:, :, 128:130], 1.0), writes=[tVH])
                    P.op(dve, lambda e: e.memset(ycH[:], 0.0), writes=[tycH])

                    def loadW5(h):
                        i = h % 2
                        offs = (OFF_CQ + h * 128, OFF_QP + h * 128, OFF_CK + h * 128, OFF_KP + h * 128, OFF_CV + h * 128)
                        for k5, off in enumerate(offs):
                            P.dma(W5[i][:, k5, :, :], wcols(l, off, 128), tW5[i], True)

                    def oacc(a, ncol=129):
                        return psb(4 + a // 3, ncol, (a % 3) * 160), bank[4 + a // 3]

                    loadW5(0)
                    pcount = [0]
                    for h in range(8):
                        wi = h % 2
                        if h + 1 < 8:
                            loadW5(h + 1)
                        for b in range(nblk_full):
                            c0, n = BLKS[b]
                            need_q = not (last and b == 8)
                            for (kk, dstT, tdst) in ((0, QT, tQT), (2, KT, tKT)):
                                if kk == 0 and not need_q:
                                    continue
                                i2 = pcount[0] % 2
                                pcount[0] += 1
                                b0, b1 = (0, 1) if i2 == 0 else (2, 3)
                                for (bk, k5) in ((b0, kk), (b1, kk + 1)):
                                    for kc in range(8):
                                        P.op(pe, lambda e, kc=kc, bk=bk, k5=k5: e.matmul(
                                            psb(bk, n), W5[wi][:, k5, kc, :], hT[:, kc, c0:c0 + n], start=(kc == 0), stop=(kc == 7)),
                                            reads=[tW5[wi], thT[b]], writes=[bank[bk]], inc=(kc == 7))
                                P.op(dve, lambda e: e.tensor_tensor(r1[i2][:, 0:n], psb(b0, n), cosS[:, c0:c0 + n], ALU.mult),
                                     reads=[bank[b0], ttab], writes=[tr1[i2]])
                                P.op(dve, lambda e: e.tensor_tensor(r2[i2][:, 0:n], psb(b1, n), sinS[:, c0:c0 + n], ALU.mult),
                                     reads=[bank[b1], ttab], writes=[tr2[i2]])
                                P.op(dve, lambda e, dstT=dstT: e.tensor_tensor(dstT[:, c0:c0 + n], r1[i2][:, 0:n], r2[i2][:, 0:n], ALU.add),
                                     reads=[tr1[i2], tr2[i2]], writes=[tdst])
                            nt_ = n // 128
                            for q in range(nt_):
                                tt = c0 // 128 + q
                                for kc in range(8):
                                    P.op(pe, lambda e, kc=kc, q=q, tt=tt: e.matmul(
                                        psb(7, 128, q * 128), hT[:, kc, tt * 128:(tt + 1) * 128], W5[wi][:, 4, kc, :], start=(kc == 0), stop=(kc == 7)),
                                        reads=[tW5[wi], thT[b]], writes=[bank[7]], inc=(kc == 7))
                            P.op(act, lambda e: e.copy(VH[:, c0 // 128:c0 // 128 + nt_, 0:128], psb(7, nt_ * 128).rearrange("p (q d) -> p q d", q=nt_)),
                                 reads=[bank[7]], writes=[tVH])
                        qblocks = [(qb * 512, 512, list(range(34))) for qb in range(8)]
                        if not last:
                            qblocks.append((NL, NCX, [32, 33]))
                        scount = 0
                        for (q0, nq, kts) in qblocks:
                            nqi = nq // 128
                            for ki, kt in enumerate(kts):
                                sbuf_i = scount % 2
                                pt_i = scount % 3
                                scount += 1
                                sb0 = 2 * sbuf_i
                                S = psT[sbuf_i]
                                for m in range(2):
                                    P.op(pe, lambda e, m=m: e.matmul(
                                        S[:, m * 512:m * 512 + nq], KT[m * 64:(m + 1) * 64, kt * 128:(kt + 1) * 128],
                                        QT[m * 64:(m + 1) * 64, q0:q0 + nq], start=True, stop=True),
                                        reads=[tKT, tQT], writes=[bank[sb0 + m]], inc=(m == 1))
                                if nq == 512:
                                    P.op(act, lambda e: e.activation(out=PT[pt_i][:], in_=S[:, :], func=AF.Exp, scale=0.125),
                                         reads=[bank[sb0], bank[sb0 + 1]], writes=[tPT[pt_i]])
                                else:
                                    P.op(act, lambda e: e.activation(
                                        out=PT[pt_i][:, :].rearrange("p (m q) -> p m q", m=2)[:, :, 0:nq],
                                        in_=S[:, :].rearrange("p (m q) -> p m q", m=2)[:, :, 0:nq], func=AF.Exp, scale=0.125),
                                        reads=[bank[sb0], bank[sb0 + 1]], writes=[tPT[pt_i]])
                                for qi in range(nqi):
                                    for m in range(2):
                                        a = qi * 2 + m
                                        ov, ob = oacc(a)
                                        P.op(pe, lambda e, m=m, qi=qi, ov=ov: e.matmul(
                                            ov, PT[pt_i][:, m * 512 + qi * 128:m * 512 + (qi + 1) * 128], VH[:, kt, 0:129],
                                            start=(ki == 0 and a % 3 == 0), stop=(ki == len(kts) - 1)),
                                            reads=[tPT[pt_i], tVH], writes=[ob], inc=(qi == nqi - 1 and m == 1))
                            for qi in range(nqi):
                                k2 = qi % 2
                                O0, ob0 = oacc(qi * 2)
                                O1, ob1 = oacc(qi * 2 + 1)
                                P.op(dve, lambda e: e.reciprocal(sm[k2][:, 0:1], O0[:, 128:129]), reads=[ob0], writes=[tsm[k2]])
                                P.op(dve, lambda e: e.reciprocal(sm[k2][:, 1:2], O1[:, 128:129]), reads=[ob1], writes=[tsm[k2]])
                                P.op(dve, lambda e: e.tensor_tensor(sm[k2][:, 2:3], sm[k2][:, 1:2], neglam[:, l:l + 1], ALU.mult),
                                     reads=[tsm[k2], consts], writes=[tsm[k2]])
                                P.op(dve, lambda e: e.tensor_scalar(o_sb[k2][:], O0[:, 0:128], sm[k2][:, 0:1], None, ALU.mult),
                                     reads=[ob0, tsm[k2]], writes=[to[k2]])
                                P.op(dve, lambda e: e.scalar_tensor_tensor(o_sb[k2][:], O1[:, 0:128], sm[k2][:, 2:3], o_sb[k2][:], ALU.mult, ALU.add),
                                     reads=[ob1, tsm[k2], to[k2]], writes=[to[k2]])
                                P.op(dve, lambda e: e.memset(sm[k2][:, 3:4], 0.0), writes=[tsm[k2]])
                                P.op(act, lambda e: e.activation(out=junk[:], in_=o_sb[k2][:], func=AF.Square, accum_out=sm[k2][:, 3:4]),
                                     reads=[to[k2]], writes=[tjunk, tsm[k2]])
                                P.op(act, lambda e: e.activation(out=sm[k2][:, 4:5], in_=sm[k2][:, 3:4], func=AF.Sqrt, bias=epsT[:, 0:1], scale=1.0 / 128),
                                     reads=[tsm[k2], consts], writes=[tsm[k2]])
                                P.op(dve, lambda e: e.reciprocal(sm[k2][:, 5:6], sm[k2][:, 4:5]), reads=[tsm[k2]], writes=[tsm[k2]])
                                P.op(dve, lambda e: e.scalar_tensor_tensor(o_sb[k2][:], o_sb[k2][:], sm[k2][:, 5:6], gsub[:, l, :], ALU.mult, ALU.mult),
                                     reads=[tsm[k2], to[k2], consts], writes=[to[k2]])
                                P.op(pe, lambda e, qi=qi: e.transpose(psb(7, 128, qi * 128), o_sb[k2][:], ident[:]),
                                     reads=[to[k2], ident_t], writes=[bank[7]])
                            P.op(act, lambda e: e.copy(ycH[:, q0:q0 + nq], psb(7, nq)), reads=[bank[7]], writes=[tycH])
                        ncols = NT if not last else NL
                        P.dma(ycT_d[s, h * 128:(h + 1) * 128, 0:ncols], ycH[:, 0:ncols], tycH, False)
                    P.barrier()
                if stop == "B3":
                    return True

                with ExitStack() as ps:
                    WO = ps.enter_context(sbt("WO", [128, 8, D], BF16)); tWO = P.tb("WO")
                    P.dma(WO[:, :, 0:512], wo_bf[l, :, 0:512].rearrange("(kc p) c -> p kc c", p=128), tWO, True)
                    P.dma(WO[:, :, 512:1024], wo_bf[l, :, 512:1024].rearrange("(kc p) c -> p kc c", p=128), tWO, True)
                    WC = [ps.enter_context(sbt("WC%d" % i, [128, 40, 128], BF16)) for i in range(2)]
                    tWC = [P.tb("WC%d" % i) for i in range(2)]
                    yin = [ps.enter_context(sbt("yin%d" % i, [128, 16, 512], BF16)) for i in range(2)]
                    tyin = [P.tb("yin%d" % i) for i in range(2)]
                    xb = [ps.enter_context(sbt("xbC%d" % i, [128, 8, 512], F32)) for i in range(2)]
                    txb = [P.tb("xbC%d" % i) for i in range(2)]
                    gs = [ps.enter_context(sbt("gs%d" % i, [128, 3, 512], F32)) for i in range(2)]
                    tgs = [P.tb("gs%d" % i) for i in range(2)]
                    t1 = [ps.enter_context(sbt("t1_%d" % i, [128, 512], F32)) for i in range(2)]
                    t2 = [ps.enter_context(sbt("t2_%d" % i, [128, 512], F32)) for i in range(2)]
                    tt1 = [P.tb("t1_%d" % i) for i in range(2)]; tt2 = [P.tb("t2_%d" % i) for i in range(2)]
                    yT = ps.enter_context(sbt("yT", [128, 8, 512], BF16)); tyT = P.tb("yT")

                    def loadWC(idx):
                        c = idx % 8
                        i = idx % 2
                        for br in range(3):
                            P.dma(WC[i][:, br * 8:(br + 1) * 8, :], wcols(l, OFF_GATE + br * D + c * 128, 128), tWC[i], True)
                        P.dma(WC[i][:, 24:28, :], wa_bf[l, :, c * 128:(c + 1) * 128].rearrange("(kc p) c -> p kc c", p=128), tWC[i], True)
                        P.dma(WC[i][:, 28:32, :], wb_bf[l, :, c * 128:(c + 1) * 128].rearrange("(kc p) c -> p kc c", p=128), tWC[i], True)
                        P.dma(WC[i][:, 32:40, :], wc_bf[l, :, c * 128:(c + 1) * 128].rearrange("(kc p) c -> p kc c", p=128), tWC[i], True)

                    def loadblk(b):
                        c0, n = BLKS[b]
                        i = b % 2
                        P.dma(yin[i][:, 0:4, 0:n], yaT_d[s, :, c0:c0 + n].rearrange("(c p) t -> p c t", p=128), tyin[i], True)
                        P.dma(yin[i][:, 4:8, 0:n], ybT_d[s, :, c0:c0 + n].rearrange("(c p) t -> p c t", p=128), tyin[i], True)
                        P.dma(yin[i][:, 8:16, 0:n], ycT_d[s, :, c0:c0 + n].rearrange("(c p) t -> p c t", p=128), tyin[i], True)
                        P.dma(xb[i][:, :, 0:n], xT_d[s, :, c0:c0 + n].rearrange("(kc p) t -> p kc t", p=128), txb[i], True)

                    loadblk(0)
                    loadWC(0)
                    idx = 0
                    nb = nblk_lat
                    for b in range(nb):
                        c0, n = BLKS[b]
                        bi = b % 2
                        j = s if b < 8 else 2
                        if b + 1 < nb:
                            loadblk(b + 1)
                        for c in range(8):
                            wi = idx % 2
                            k2 = idx % 2
                            if idx + 1 < nb * 8:
                                loadWC(idx + 1)
                            idx += 1
                            for br in range(3):
                                for kc in range(8):
                                    P.op(pe, lambda e, br=br, kc=kc: e.matmul(
                                        psb(br, n), WC[wi][:, br * 8 + kc, :], hT[:, kc, c0:c0 + n], start=(kc == 0), stop=(kc == 7)),
                                        reads=[tWC[wi], thT[b]], writes=[bank[br]], inc=(kc == 7))
                            for (br, k0, nk, y0) in ((0, 24, 4, 0), (1, 28, 4, 4), (2, 32, 8, 8)):
                                for kc in range(nk):
                                    P.op(pe, lambda e, br=br, kc=kc, k0=k0, y0=y0: e.matmul(
                                        psb(3 + br, n), WC[wi][:, k0 + kc, :], yin[bi][:, y0 + kc, 0:n], start=(kc == 0), stop=(kc == nk - 1)),
                                        reads=[tWC[wi], tyin[bi]], writes=[bank[3 + br]], inc=(kc == nk - 1))
                            for br in range(3):
                                P.op(act, lambda e, br=br: e.activation(out=gs[k2][:, br, 0:n], in_=psb(br, n), func=AF.Sigmoid,
                                                                        bias=bgT[:, l, br * 8 + c:br * 8 + c + 1]),
                                     reads=[bank[br], cst2], writes=[tgs[k2]])
                            P.op(dve, lambda e: e.tensor_tensor(t1[k2][:, 0:n], psb(3, n), gs[k2][:, 0, 0:n], ALU.mult), reads=[bank[3], tgs[k2]], writes=[tt1[k2]])
                            P.op(dve, lambda e: e.tensor_tensor(t2[k2][:, 0:n], psb(4, n), gs[k2][:, 1, 0:n], ALU.mult), reads=[bank[4], tgs[k2]], writes=[tt2[k2]])
                            P.op(dve, lambda e: e.tensor_tensor(t1[k2][:, 0:n], t1[k2][:, 0:n], t2[k2][:, 0:n], ALU.add), reads=[tt1[k2], tt2[k2]], writes=[tt1[k2]])
                            P.op(dve, lambda e: e.tensor_tensor(t2[k2][:, 0:n], psb(5, n), gs[k2][:, 2, 0:n], ALU.mult), reads=[bank[5], tgs[k2]], writes=[tt2[k2]])
                            P.op(dve, lambda e, c=c: e.tensor_tensor(yT[:, c, 0:n], t1[k2][:, 0:n], t2[k2][:, 0:n], ALU.add), reads=[tt1[k2], tt2[k2]], writes=[tyT])
                        for c in range(8):
                            pb_ = 6 + (c % 2)
                            for kc in range(8):
                                P.op(pe, lambda e, c=c, kc=kc, pb_=pb_: e.matmul(
                                    psb(pb_, n), WO[:, kc, c * 128:(c + 1) * 128], yT[:, kc, 0:n], start=(kc == 0), stop=(kc == 7)),
                                    reads=[tWO, tyT], writes=[bank[pb_]], inc=(kc == 7))
                            P.op(dve, lambda e, c=c, pb_=pb_: e.scalar_tensor_tensor(
                                xb[bi][:, c, 0:n], psb(pb_, n), der[:, l, j, 2, c:c + 1], xb[bi][:, c, 0:n], ALU.mult, ALU.add),
                                reads=[bank[pb_], consts, txb[bi]], writes=[txb[bi]])
                        P.dma(xT_d[s, :, c0:c0 + n].rearrange("(kc p) t -> p kc t", p=128), xb[bi][:, :, 0:n], txb[bi], False)
                    P.barrier()
                if stop == "C1":
                    return True
            if l == 0:
                phaseC2_dense(s, l)
            else:
                phaseC2_moe(s, l)

        def phaseC2_dense(s, l):
            with ExitStack() as ps:
                xb = [ps.enter_context(sbt("xbD%d" % i, [128, 8, 512], F32)) for i in range(2)]
                txb = [P.tb("xbD%d" % i) for i in range(2)]
                sq = ps.enter_context(sbt("sqD", [128, 8, 512], BF16)); tsq = P.tb("sqD")
                rstd = ps.enter_context(sbt("rstdD", [128, 512], F32)); trs = P.tb("rstdD")
                tmpf = [ps.enter_context(sbt("tmpD%d" % i, [128, 512], F32)) for i in range(2)]
                ttmp = [P.tb("tmpD%d" % i) for i in range(2)]
                h2 = ps.enter_context(sbt("h2D", [128, 8, 512], BF16)); th2 = P.tb("h2D")
                hid = ps.enter_context(sbt("hidD", [128, NJ_FF, 512], BF16)); thid = P.tb("hidD")
                W1 = [ps.enter_context(sbt("W1D%d" % i, [128, 2, 8, 128], BF16)) for i in range(3)]
                tW1 = [P.tb("W1D%d" % i) for i in range(3)]
                W2 = [ps.enter_context(sbt("W2D%d" % i, [128, NJ_FF, 128], BF16)) for i in range(2)]
                tW2 = [P.tb("W2D%d" % i) for i in range(2)]
                ssb = [ps.enter_context(sbt("ssbD%d" % i, [128, 512], F32)) for i in range(2)]
                tss = [P.tb("ssbD%d" % i) for i in range(2)]

                def loadW1(idx):
                    jc = idx % NJ_FF
                    i = idx % 3
                    P.dma(W1[i][:, 0, :, :], fg_bf[:, jc * 128:(jc + 1) * 128].rearrange("(kc p) c -> p kc c", p=128), tW1[i], True)
                    P.dma(W1[i][:, 1, :, :], fu_bf[:, jc * 128:(jc + 1) * 128].rearrange("(kc p) c -> p kc c", p=128), tW1[i], True)

                def loadW2(idx):
                    c = idx % 8
                    i = idx % 2
                    P.dma(W2[i][:], fd_bf[:, c * 128:(c + 1) * 128].rearrange("(jc p) c -> p jc c", p=128), tW2[i], True)

                def loadx(b):
                    c0, n = BLKS[b]
                    P.dma(xb[b % 2][:, :, 0:n], xT_d[s, :, c0:c0 + n].rearrange("(kc p) t -> p kc t", p=128), txb[b % 2], True)

                nb = 9
                loadx(0)
                loadW1(0); loadW1(1)
                loadW2(0)
                i1 = 0
                i2 = 0
                for b in range(nb):
                    c0, n = BLKS[b]
                    bi = b % 2
                    j = s if b < 8 else 2
                    if b + 1 < nb:
                        loadx(b + 1)
                    norm_mod(xb[bi], txb[bi], n, l, j, 3, lambda kc, n=n: h2[:, kc, 0:n], th2, sq, tsq, rstd, trs, tmpf, ttmp, 6)
                    for jc in range(NJ_FF):
                        wi = i1 % 3
                        k2 = i1 % 2
                        if i1 + 2 < nb * NJ_FF:
                            loadW1(i1 + 2)
                        i1 += 1
                        bg_, bu_ = (0, 1) if k2 == 0 else (2, 3)
                        for (bk, gi) in ((bg_, 0), (bu_, 1)):
                            for kc in range(8):
                                P.op(pe, lambda e, bk=bk, gi=gi, kc=kc: e.matmul(
                                    psb(bk, n), W1[wi][:, gi, kc, :], h2[:, kc, 0:n], start=(kc == 0), stop=(kc == 7)),
                                    reads=[tW1[wi], th2], writes=[bank[bk]], inc=(kc == 7))
                        P.op(act, lambda e: e.activation(out=ssb[k2][:, 0:n], in_=psb(bg_, n), func=AF.Silu), reads=[bank[bg_]], writes=[tss[k2]])
                        P.op(dve, lambda e, jc=jc: e.tensor_tensor(hid[:, jc, 0:n], psb(bu_, n), ssb[k2][:, 0:n], ALU.mult),
                             reads=[bank[bu_], tss[k2]], writes=[thid])
                    for c in range(8):
                        wi = i2 % 2
                        if i2 + 1 < nb * 8:
                            loadW2(i2 + 1)
                        i2 += 1
                        pb_ = 4 + (c % 2)
                        for jc in range(NJ_FF):
                            P.op(pe, lambda e, jc=jc, pb_=pb_: e.matmul(
                                psb(pb_, n), W2[wi][:, jc, :], hid[:, jc, 0:n], start=(jc == 0), stop=(jc == NJ_FF - 1)),
                                reads=[tW2[wi], thid], writes=[bank[pb_]], inc=(jc == NJ_FF - 1))
                        P.op(dve, lambda e, c=c, pb_=pb_: e.scalar_tensor_tensor(
                            xb[bi][:, c, 0:n], psb(pb_, n), der[:, l, j, 5, c:c + 1], xb[bi][:, c, 0:n], ALU.mult, ALU.add),
                            reads=[bank[pb_], consts, txb[bi]], writes=[txb[bi]])
                    P.dma(xT_d[s, :, c0:c0 + n].rearrange("(kc p) t -> p kc t", p=128), xb[bi][:, :, 0:n], txb[bi], False)
                P.barrier()

        def phaseC2_moe(s, l):
            T = 1024
            with ExitStack() as ps:
                xb = ps.enter_context(sbt("xbM", [128, 8, T], F32)); txb = P.tb("xbM")
                rstd = ps.enter_context(sbt("rstdM", [128, T], F32)); trs = P.tb("rstdM")
                tmpf = [ps.enter_context(sbt("tmpM%d" % i, [128, T], F32)) for i in range(2)]
                ttmp = [P.tb("tmpM%d" % i) for i in range(2)]
                h2f = [ps.enter_context(sbt("h2f%d" % i, [128, T], F32)) for i in range(2)]
                th2f = [P.tb("h2f%d" % i) for i in range(2)]
                h2 = ps.enter_context(sbt("h2M", [128, 8, T], BF16)); th2 = P.tb("h2M")
                hid = ps.enter_context(sbt("hidM", [128, NJ_E, T], BF16)); thid = P.tb("hidM")
                sq = hid[:, 0:8, 0:512]; tsq = thid
                acc = ps.enter_context(sbt("accM", [128, 8, T], F32)); tacc = P.tb("accM")
                cbc = [ps.enter_context(sbt("cbc%d" % i, [128, T], F32)) for i in range(2)]
                tcbc = [P.tb("cbc%d" % i) for i in range(2)]
                W1 = [ps.enter_context(sbt("W1M%d" % i, [128, 2, 8, 128], BF16)) for i in range(3)]
                tW1 = [P.tb("W1M%d" % i) for i in range(3)]
                W2 = [ps.enter_context(sbt("W2M%d" % i, [128, NJ_E, 128], BF16)) for i in range(2)]
                tW2 = [P.tb("W2M%d" % i) for i in range(2)]
                wr = ps.enter_context(sbt("wrM", [128, 8, NE], F32)); twr = P.tb("wrM")
                lg = ps.enter_context(sbt("lgM", [128, 8, NE], F32))
                lg2 = ps.enter_context(sbt("lg2M", [128, 8, NE], F32))
                mk1 = ps.enter_context(sbt("mk1M", [128, 8, NE], F32))
                mk2 = ps.enter_context(sbt("mk2M", [128, 8, NE], F32))
                comb = ps.enter_context(sbt("combM", [128, 8, NE], F32))
                rs = ps.enter_context(sbt("rsM", [128, 6, 8], F32)); trt = P.tb("routeM")
                combT = ps.enter_context(sbt("combTM", [8, T], F32)); tcT = P.tb("combTM")
                sel = ps.enter_context(sbt("selM", [8, NE, 128], F32)); tsel = P.tb("selM")
                P.dma(wr[:], moe_wr[:, :, :], twr, True)
                for e_ in range(NE):
                    P.op(dve, lambda e, e_=e_: e.tensor_copy(sel[:, e_, :], ident[0:8, e_:e_ + 1].to_broadcast([8, 128])),
                         reads=[ident_t], writes=[tsel])

                def loadW1(idx):
                    e_ = (idx // NJ_E) % NE
                    jc = idx % NJ_E
                    i = idx % 3
                    P.dma(W1[i][:, 0, :, :], mg_bf[e_, :, jc * 128:(jc + 1) * 128].rearrange("(kc p) c -> p kc c", p=128), tW1[i], True)
                    P.dma(W1[i][:, 1, :, :], mu_bf[e_, :, jc * 128:(jc + 1) * 128].rearrange("(kc p) c -> p kc c", p=128), tW1[i], True)

                def loadW2(idx):
                    e_ = (idx // 8) % NE
                    c = idx % 8
                    i = idx % 2
                    P.dma(W2[i][:], md_bf[e_, :, c * 128:(c + 1) * 128].rearrange("(jc p) c -> p jc c", p=128), tW2[i], True)

                nb = NL // T
                loadW1(0); loadW1(1)
                loadW2(0)
                i1 = 0
                i2 = 0
                for b in range(nb):
                    c0 = b * T
                    P.dma(xb[:], xT_d[s, :, c0:c0 + T].rearrange("(kc p) t -> p kc t", p=128), txb, True)

                    def post(kc, i):
                        k2 = kc % 2
                        P.op(act, lambda e: e.activation(out=h2f[k2][:], in_=tmpf[i][:], func=AF.Identity, bias=der[:, l, s, 4, kc:kc + 1]),
                             reads=[ttmp[i], consts], writes=[th2f[k2]])
                        for tt in range(8):
                            P.op(pe, lambda e, tt=tt: e.matmul(psb(7, NE, tt * NE), h2f[k2][:, tt * 128:(tt + 1) * 128], wr[:, kc, :],
                                                               start=(kc == 0 and tt == 0), stop=(kc == 7)),
                                 reads=[th2f[k2], twr], writes=[bank[7]], inc=(tt == 7))
                        P.op(dve, lambda e: e.tensor_copy(h2[:, kc, :], h2f[k2][:]), reads=[th2f[k2]], writes=[th2])
                    norm_mod(xb, txb, T, l, s, 3, None, None, sq, tsq, rstd, trs, tmpf, ttmp, 6, post=post)
                    lgp = psb(7, 64).rearrange("p (t e) -> p t e", e=NE)
                    R = [trt]
                    P.op(dve, lambda e: e.tensor_copy(lg[:], lgp), reads=[bank[7]], writes=R)
                    P.op(dve, lambda e: e.tensor_reduce(rs[:, 0, :], lg[:], AX.X, ALU.max), reads=R, writes=R)
                    P.op(dve, lambda e: e.tensor_tensor(mk1[:], lg[:], rs[:, 0, :].unsqueeze(2).to_broadcast([128, 8, NE]), ALU.is_equal), reads=R, writes=R)
                    P.op(dve, lambda e: e.scalar_tensor_tensor(lg2[:], mk1[:], -1e30, lg[:], ALU.mult, ALU.add), reads=R, writes=R)
                    P.op(dve, lambda e: e.tensor_reduce(rs[:, 1, :], lg2[:], AX.X, ALU.max), reads=R, writes=R)
                    P.op(dve, lambda e: e.tensor_tensor(mk2[:], lg2[:], rs[:, 1, :].unsqueeze(2).to_broadcast([128, 8, NE]), ALU.is_equal), reads=R, writes=R)
                    P.op(dve, lambda e: e.tensor_tensor(rs[:, 2, :], rs[:, 1, :], rs[:, 0, :], ALU.subtract), reads=R, writes=R)
                    P.op(act, lambda e: e.activation(out=rs[:, 3, :], in_=rs[:, 2, :], func=AF.Exp), reads=R, writes=R)
                    P.op(dve, lambda e: e.tensor_scalar(rs[:, 4, :], rs[:, 3, :], 1.0, None, ALU.add), reads=R, writes=R)
                    P.op(dve, lambda e: e.reciprocal(rs[:, 4, :], rs[:, 4, :]), reads=R, writes=R)
                    P.op(dve, lambda e: e.tensor_tensor(rs[:, 5, :], rs[:, 3, :], rs[:, 4, :], ALU.mult), reads=R, writes=R)
                    P.op(dve, lambda e: e.tensor_tensor(mk1[:], mk1[:], rs[:, 4, :].unsqueeze(2).to_broadcast([128, 8, NE]), ALU.mult), reads=R, writes=R)
                    P.op(dve, lambda e: e.tensor_tensor(mk2[:], mk2[:], rs[:, 5, :].unsqueeze(2).to_broadcast([128, 8, NE]), ALU.mult), reads=R, writes=R)
                    P.op(dve, lambda e: e.tensor_tensor(comb[:], mk1[:], mk2[:], ALU.add), reads=R, writes=R)
                    for tt in range(8):
                        P.op(pe, lambda e, tt=tt: e.transpose(psT[3][0:8, tt * 128:(tt + 1) * 128], comb[:, tt, :], ident[:]),
                             reads=R + [ident_t], writes=[bank[6 + tt // 4]], inc=(tt % 4 == 3))
                    P.op(act, lambda e: e.copy(combT[:], psT[3][0:8, :]), reads=[bank[6], bank[7]], writes=[tcT])
                    for e_ in range(NE):
                        ci = e_ % 2
                        for sub in range(2):
                            P.op(pe, lambda e, sub=sub: e.matmul(psb(6 + sub), sel[:, e_, :], combT[:, sub * 512:(sub + 1) * 512], start=True, stop=True),
                                 reads=[tsel, tcT], writes=[bank[6 + sub]])
                        P.op(act, lambda e: e.copy(cbc[ci][:], psT[3][:, :]), reads=[bank[6], bank[7]], writes=[tcbc[ci]])
                        for jc in range(NJ_E):
                            wi = i1 % 3
                            k2 = i1 % 2
                            if i1 + 2 < nb * NE * NJ_E:
                                loadW1(i1 + 2)
                            i1 += 1
                            for gi in range(2):
                                for sub in range(2):
                                    for kc in range(8):
                                        P.op(pe, lambda e, gi=gi, sub=sub, kc=kc: e.matmul(
                                            psb(gi * 2 + sub), W1[wi][:, gi, kc, :], h2[:, kc, sub * 512:(sub + 1) * 512], start=(kc == 0), stop=(kc == 7)),
                                            reads=[tW1[wi], th2], writes=[bank[gi * 2 + sub]], inc=(kc == 7))
                            P.op(act, lambda e: e.activation(out=tmpf[k2][:], in_=psT[0][:, :], func=AF.Silu), reads=[bank[0], bank[1]], writes=[ttmp[k2]])
                            P.op(dve, lambda e, jc=jc: e.tensor_tensor(hid[:, jc, :], psT[1][:, :], tmpf[k2][:], ALU.mult),
                                 reads=[bank[2], bank[3], ttmp[k2]], writes=[thid])
                        for c in range(8):
                            wi = i2 % 2
                            if i2 + 1 < nb * NE * 8:
                                loadW2(i2 + 1)
                            i2 += 1
                            for sub in range(2):
                                for jc in range(NJ_E):
                                    P.op(pe, lambda e, jc=jc, sub=sub: e.matmul(
                                        psb(4 + sub), W2[wi][:, jc, :], hid[:, jc, sub * 512:(sub + 1) * 512], start=(jc == 0), stop=(jc == NJ_E - 1)),
                                        reads=[tW2[wi], thid], writes=[bank[4 + sub]], inc=(jc == NJ_E - 1))
                            if e_ == 0:
                                P.op(dve, lambda e, c=c: e.tensor_tensor(acc[:, c, :], psT[2][:, :], cbc[ci][:], ALU.mult),
                                     reads=[bank[4], bank[5], tcbc[ci]], writes=[tacc])
                            else:
                                k2 = c % 2
                                P.op(dve, lambda e: e.tensor_tensor(h2f[k2][:], psT[2][:, :], cbc[ci][:], ALU.mult),
                                     reads=[bank[4], bank[5], tcbc[ci]], writes=[th2f[k2]])
                                P.op(dve, lambda e, c=c: e.tensor_tensor(acc[:, c, :], acc[:, c, :], h2f[k2][:], ALU.add),
                                     reads=[th2f[k2], tacc], writes=[tacc])
                    for c in range(8):
                        P.op(dve, lambda e, c=c: e.scalar_tensor_tensor(xb[:, c, :], acc[:, c, :], der[:, l, s, 5, c:c + 1], xb[:, c, :], ALU.mult, ALU.add),
                             reads=[tacc, consts, txb], writes=[txb])
                    for sub in range(2):
                        P.op(act, lambda e, sub=sub: e.activation(out=sq[:, :, :], in_=xb[:, :, sub * 512:(sub + 1) * 512], func=AF.Square), reads=[txb], writes=[tsq])
                        for kc in range(8):
                            P.op(pe, lambda e, kc=kc: e.matmul(psb(6), ones_ms[:], sq[:, kc, :], start=(kc == 0), stop=(kc == 7)),
                                 reads=[tsq, consts], writes=[bank[6]], inc=(kc == 7))
                        P.op(act, lambda e, sub=sub: e.activation(out=rstd[:, sub * 512:(sub + 1) * 512], in_=psb(6), func=AF.Sqrt, bias=epsT[:, 0:1]),
                             reads=[bank[6], consts], writes=[trs])
                    P.op(dve, lambda e: e.reciprocal(rstd[:], rstd[:]), reads=[trs], writes=[trs])
                    for c in range(8):
                        P.op(dve, lambda e, c=c: e.scalar_tensor_tensor(xb[:, c, :], xb[:, c, :], fgs[:, c:c + 1], rstd[:], ALU.mult, ALU.mult),
                             reads=[txb, trs, cst2], writes=[txb])
                    for tt in range(8):
                        k2 = tt % 2
                        pst = psT[k2]
                        for c in range(8):
                            P.op(pe, lambda e, c=c, tt=tt, pst=pst: e.transpose(pst[:, c * 128:(c + 1) * 128], xb[:, c, tt * 128:(tt + 1) * 128], ident[:]),
                                 reads=[txb, ident_t], writes=[bank[2 * k2 + c // 4]], inc=(c % 4 == 3))
                        P.op(act, lambda e, pst=pst: e.copy(tmpf[k2][:], pst[:, :]), reads=[bank[2 * k2], bank[2 * k2 + 1]], writes=[ttmp[k2]])
                        P.dma(out2[s, c0 + tt * 128:c0 + (tt + 1) * 128, :], tmpf[k2][:], ttmp[k2], False)
                P.barrier()

        phase0()
        phaseM()
        stopped = False
        for s in seqs:
            for l in layers:
                if not stopped:
                    stopped = bool(seq_layer(s, l))
        P.barrier()
        print("instructions:", P.ninst, "sems:", P.nsem)
    return nc


def _rope_tables():
    t = np.arange(NL)
    row = (t // 64).astype(np.float64)
    col = (t % 64).astype(np.float64)
    inv = 10000.0 ** (-np.arange(16, dtype=np.float64) / 16)
    cosT = np.ones((128, NT), np.float32)
    sinT = np.zeros((128, NT), np.float32)
    for p in range(128):
        d = p % 64
        a, j, i = d // 32, (d // 16) % 2, d % 16
        pos = row if a == 0 else col
        ang = (pos.astype(np.float32) * np.float32(inv[i])).astype(np.float32)
        cosT[p, :NL] = np.cos(ang)
        sn = np.sin(ang)
        sinT[p, :NL] = -sn if j == 0 else sn
    return cosT, sinT


def _perm_cols():
    idx = np.arange(1024)
    d = idx % 64
    base = idx - d
    a, j, i = d // 32, (d // 16) % 2, d % 16
    return base + a * 32 + (1 - j) * 16 + i


def _prep_shared(inp):
    f = lambda a: np.ascontiguousarray(a, dtype=np.float32)
    T128 = lambda v: np.ascontiguousarray(v.reshape(-1, 128).T)
    perm = _perm_cols()
    w_in = inp["w_in"]
    w_perm = np.concatenate([w_in[:, :, OFF_CQ:OFF_CQ + 1024][:, :, perm], w_in[:, :, OFF_CK:OFF_CK + 1024][:, :, perm]], axis=2)
    cosT, sinT = _rope_tables()
    sh = {
        "w_ada": f(inp["w_ada"]),
        "b_adaT": f(np.stack([T128(inp["b_ada"][l]) for l in range(L)])),
        "n1gT": f(np.stack([T128(inp["norm1_g"][l]) for l in range(L)])),
        "n2gT": f(np.stack([T128(inp["norm2_g"][l]) for l in range(L)])),
        "fgT": f(T128(inp["final_norm_g"])),
        "w_in": f(w_in),
        "w_perm": f(w_perm),
        "b_gateT": f(np.stack([T128(inp["b_gate"][l]) for l in range(L)])),
        "a_ln_g": f(inp["a_ln_g"]),
        "a_ln_b": f(inp["a_ln_b"]),
        "a_wsT": f(np.transpose(inp["a_ws"], (0, 3, 1, 2))),
        "a_bsT": f(np.transpose(inp["a_bs"], (0, 2, 1))),
        "b_convT": f(np.stack([np.transpose(inp["b_conv"][l].reshape(3, 4, 128), (2, 1, 0)) for l in range(L)])),
        "c_lambda": f(inp["c_lambda"].reshape(L, 256)),
        "c_subln_g": f(inp["c_subln_g"]),
        "w_a_out": f(inp["w_a_out"]), "w_b_out": f(inp["w_b_out"]),
        "w_c_out": f(inp["w_c_out"]), "w_o": f(inp["w_o"]),
        "ff_wg": f(inp["ff_w_gate"][0]), "ff_wu": f(inp["ff_w_up"][0]), "ff_wd": f(inp["ff_w_down"][0]),
        "moe_wr": f(np.transpose(inp["moe_w_router"][0].reshape(8, 128, NE), (1, 0, 2))),
        "moe_wg": f(inp["moe_w_gate"][0]), "moe_wu": f(inp["moe_w_up"][0]), "moe_wd": f(inp["moe_w_down"][0]),
        "cosT": cosT, "sinT": sinT,
        "ident": np.eye(128, dtype=np.float32),
    }
    return sh


def _core_inputs(inp, shared, core):
    b0 = 2 * core
    c3 = np.stack([inp["c"][b0], inp["c"][b0 + 1], inp["c_ctx"]], axis=1)
    cT = np.ascontiguousarray(np.transpose(c3.reshape(8, 128, 3), (1, 0, 2)), dtype=np.float32)
    m = dict(shared)
    m["x2"] = np.ascontiguousarray(inp["x"][b0:b0 + 2], dtype=np.float32)
    m["ctx2"] = np.ascontiguousarray(inp["ctx"][b0:b0 + 2], dtype=np.float32)
    m["cT"] = cT
    return m


def kernel(**inputs):
    inp = {k: np.asarray(v) for k, v in inputs.items()}
    nc = build_nc()
    shared = _prep_shared(inp)
    in_maps = [_core_inputs(inp, shared, c) for c in range(8)]
    res = run_bass_kernel_spmd(nc, in_maps, core_ids=list(range(8)))
    out = np.concatenate([np.asarray(r["out2"]) for r in res.results], axis=0)
    return out.astype(np.float32)
```

```python
import math
from contextlib import ExitStack
import numpy as np
import concourse.bass as bass
import concourse.mybir as mybir
from concourse.bass_utils import run_bass_kernel_spmd

F32 = mybir.dt.float32
BF16 = mybir.dt.bfloat16
AF = mybir.ActivationFunctionType
ALU = mybir.AluOpType
AX = mybir.AxisListType

D = 1024
NL = 4096
NCX = 256
NT = NL + NCX
L = 2
EPS = 1e-6
OFF_AU, OFF_AV, OFF_BB, OFF_BC, OFF_BH = 0, 512, 1024, 1536, 2048
OFF_CQ, OFF_CK, OFF_CV, OFF_GATE = 2560, 3584, 4608, 5632
IN_COLS = 8704
WCOLS = IN_COLS + 2048
OFF_QP, OFF_KP = IN_COLS, IN_COLS + 1024
DFF = 2816
NE = 8
DFE = 3584
NJ_FF = DFF // 128
NJ_E = DFE // 128
BLKS = [(i * 512, 512) for i in range(8)] + [(NL, NCX)]


class Eng:
    def __init__(self, name, e, sem):
        self.name, self.e, self.sem, self.cnt, self.seen = name, e, sem, 0, {}


class TB:
    def __init__(self, name):
        self.name, self.w, self.r, self.dsem = name, None, {}, None


class DSem:
    def __init__(self, sem):
        self.sem, self.cnt = sem, 0


class Prog:
    def __init__(self, nc, es):
        self.nc, self.es = nc, es
        self.nsem = 0
        self.pe = Eng("pe", nc.tensor, self.mksem("s_pe"))
        self.act = Eng("act", nc.scalar, self.mksem("s_act"))
        self.dve = Eng("dve", nc.vector, self.mksem("s_dve"))
        self.pool = Eng("pool", nc.gpsimd, self.mksem("s_pool"))
        self.sp = Eng("sp", nc.sync, self.mksem("s_sp"))
        self.engs = [self.pe, self.act, self.dve, self.pool, self.sp]
        self.bar = self.mksem("s_bar")
        self.barcnt = 0
        self.tbs = []
        self.free_dsems = []
        self.dsems = []
        self.keep = set()
        self.ninst = 0

    def mksem(self, name):
        self.nsem += 1
        return self.es.enter_context(self.nc.semaphore(name))

    def tb(self, name, keep=False):
        t = TB(name)
        self.tbs.append(t)
        if keep:
            self.keep.add(name)
        return t

    def _wait(self, E, ev, raw=False):
        sem, val = ev
        if sem is E.sem and not (raw and E.name in ("act", "dve", "pool")):
            return
        k = id(sem)
        if E.seen.get(k, 0) >= val:
            return
        E.e.wait_ge(sem, val)
        self.ninst += 1
        E.seen[k] = val

    def _deps(self, E, reads, writes):
        for b in reads:
            if b.w is not None:
                self._wait(E, b.w, raw=True)
        for b in writes:
            if b.w is not None:
                self._wait(E, b.w)
            for ev in b.r.values():
                self._wait(E, ev)

    def _post(self, ev, reads, writes):
        for b in reads:
            b.r[id(ev[0])] = ev
        for b in writes:
            b.w = ev
            b.r = {}

    def op(self, E, fn, reads=(), writes=(), inc=True):
        self._deps(E, reads, writes)
        ins = fn(E.e)
        self.ninst += 1
        if inc:
            E.cnt += 1
            ins.then_inc(E.sem, 1)
            ev = (E.sem, E.cnt)
        else:
            ev = (E.sem, E.cnt + 1)
        self._post(ev, reads, writes)

    def dma(self, out, in_, sb, load, Q=None):
        Q = Q or self.sp
        if sb.dsem is None:
            if self.free_dsems:
                sb.dsem = self.free_dsems.pop()
            else:
                sb.dsem = DSem(self.mksem("dsem%d" % len(self.dsems)))
                self.dsems.append(sb.dsem)
        ds = sb.dsem
        reads = [] if load else [sb]
        writes = [sb] if load else []
        self._deps(Q, reads, writes)
        Q.e.dma_start(out=out, in_=in_).then_inc(ds.sem, 16)
        self.ninst += 1
        ds.cnt += 16
        self._post((ds.sem, ds.cnt), reads, writes)

    def barrier(self):
        sp = self.sp
        for E in self.engs:
            if E is not sp and E.cnt > 0:
                self._wait(sp, (E.sem, E.cnt))
        for ds in self.dsems:
            if ds.cnt:
                self._wait(sp, (ds.sem, ds.cnt))
        self.barcnt += 1
        sp.e.sem_inc(self.bar, 1)
        for E in self.engs:
            if E is sp:
                continue
            E.e.wait_ge(self.bar, self.barcnt)
            for E2 in self.engs:
                if E2.cnt:
                    E.seen[id(E2.sem)] = E2.cnt
            for ds in self.dsems:
                E.seen[id(ds.sem)] = ds.cnt
        for t in self.tbs:
            t.w = None
            t.r = {}
            if t.dsem is not None:
                self.free_dsems.append(t.dsem)
                t.dsem = None
        self.tbs = [t for t in self.tbs if t.name in self.keep]


class _Stop(Exception):
    pass


def build_nc(seqs=(0, 1), layers=(0, 1), debug_dump=False, stop=None):
    nc = bass.Bass("TRN2", target_bir_lowering=False)

    _uid = [0]

    def sbt(name, shape, dt):
        _uid[0] += 1
        return nc.sbuf_tensor("%s_u%d" % (name, _uid[0]), list(shape), dt)

    def din(name, shape, dt=F32):
        return nc.dram_tensor(name, list(shape), dt, kind="ExternalInput").ap()

    def dscr(name, shape, dt):
        return nc.dram_tensor(name, list(shape), dt).ap()

    x2 = din("x2", [2, NL, D])
    ctx2 = din("ctx2", [2, NCX, D])
    cT = din("cT", [128, 8, 3])
    w_ada = din("w_ada", [L, D, 6 * D])
    b_adaT = din("b_adaT", [L, 128, 48])
    n1gT = din("n1gT", [L, 128, 8])
    n2gT = din("n2gT", [L, 128, 8])
    fgT = din("fgT", [128, 8])
    w_in = din("w_in", [L, D, IN_COLS])
    w_perm = din("w_perm", [L, D, 2048])
    b_gateT = din("b_gateT", [L, 128, 24])
    a_ln_g = din("a_ln_g", [L, 512])
    a_ln_b = din("a_ln_b", [L, 512])
    a_wsT = din("a_wsT", [L, 128, 8, 128])
    a_bsT = din("a_bsT", [L, 128, 8])
    b_convT = din("b_convT", [L, 128, 4, 3])
    c_lambda = din("c_lambda", [L, 256])
    c_subln_g = din("c_subln_g", [L, 128])
    w_a_out = din("w_a_out", [L, 512, D])
    w_b_out = din("w_b_out", [L, 512, D])
    w_c_out = din("w_c_out", [L, D, D])
    w_o = din("w_o", [L, D, D])
    ff_wg = din("ff_wg", [D, DFF])
    ff_wu = din("ff_wu", [D, DFF])
    ff_wd = din("ff_wd", [DFF, D])
    moe_wr = din("moe_wr", [128, 8, NE])
    moe_wg = din("moe_wg", [NE, D, DFE])
    moe_wu = din("moe_wu", [NE, D, DFE])
    moe_wd = din("moe_wd", [NE, DFE, D])
    cosT = din("cosT", [128, NT])
    sinT = din("sinT", [128, NT])
    ident_d = din("ident", [128, 128])
    out2 = nc.dram_tensor("out2", [2, NL, D], F32, kind="ExternalOutput").ap()

    win_bf = dscr("win_bf", [L, D, WCOLS], BF16)
    wa_bf = dscr("wa_bf", [L, 512, D], BF16)
    wb_bf = dscr("wb_bf", [L, 512, D], BF16)
    wc_bf = dscr("wc_bf", [L, D, D], BF16)
    wo_bf = dscr("wo_bf", [L, D, D], BF16)
    fg_bf = dscr("fg_bf", [D, DFF], BF16)
    fu_bf = dscr("fu_bf", [D, DFF], BF16)
    fd_bf = dscr("fd_bf", [DFF, D], BF16)
    mg_bf = dscr("mg_bf", [NE, D, DFE], BF16)
    mu_bf = dscr("mu_bf", [NE, D, DFE], BF16)
    md_bf = dscr("md_bf", [NE, DFE, D], BF16)
    kind = "ExternalOutput" if debug_dump else "Internal"
    xT_d = nc.dram_tensor("xT_d", [2, D, NT], F32, kind=kind).ap()
    yaT_d = nc.dram_tensor("yaT_d", [2, 512, NT], BF16, kind=kind).ap()
    ybT_d = nc.dram_tensor("ybT_d", [2, 512, NT], BF16, kind=kind).ap()
    ycT_d = nc.dram_tensor("ycT_d", [2, D, NT], BF16, kind=kind).ap()
    hT_dbg = nc.dram_tensor("hT_dbg", [128, 8, NT], BF16, kind=kind).ap()

    with ExitStack() as es:
        P = Prog(nc, es)
        pe, act, dve, sp = P.pe, P.act, P.dve, P.sp

        def sb(name, shape, dt):
            return es.enter_context(sbt(name, list(shape), dt))

        psT = [es.enter_context(nc.psum_tensor("ps%d" % i, [128, 1024], F32)) for i in range(4)]
        bank = [P.tb("bank%d" % i, True) for i in range(8)]

        def psb(i, n=512, off=0):
            t = psT[i // 2]
            o = (i % 2) * 512 + off
            return t[:, o:o + n]

        ident = sb("ident", [128, 128], F32); ident_t = P.tb("ident", True)
        ones_ms = sb("ones_ms", [128, 128], BF16)
        epsT = sb("epsT", [128, 1], F32)
        modT = sb("modT", [128, L, 48, 3], F32)
        der = sb("der", [128, L, 3, 6, 8], F32)
        neglam = sb("neglam", [128, L], F32)
        gsub = sb("gsub", [128, L, 128], F32)
        bgT = sb("bgT", [128, L, 24], F32)
        convT = sb("convT", [128, L, 4, 3], F32)
        fgs = sb("fgs", [128, 8], F32)
        consts = P.tb("consts", True)

        P.dma(ident[:], ident_d[:, :], ident_t, True)
        P.op(dve, lambda e: e.memset(ones_ms[:], 1.0 / D), writes=[consts])
        P.op(dve, lambda e: e.memset(epsT[:], EPS), writes=[consts])
        cst2 = P.tb("cst2", True)
        for l in range(L):
            P.dma(bgT[:, l, :], b_gateT[l, :, :], cst2, True)
            P.dma(convT[:, l, :, :], b_convT[l, :, :, :], cst2, True)
        P.dma(fgs[:], fgT[:, :], cst2, True)

        def cast_copy(i, out, in_, reads, writes):
            E = (act, dve)[i % 2]
            if E is act:
                P.op(act, lambda e: e.copy(out, in_), reads=reads, writes=writes)
            else:
                P.op(dve, lambda e: e.tensor_copy(out, in_), reads=reads, writes=writes)

        def phase0():
            with ExitStack() as ps:
                CH = 4096
                st32 = [ps.enter_context(sbt("st32_%d" % i, [128, CH], F32)) for i in range(3)]
                st16 = [ps.enter_context(sbt("st16_%d" % i, [128, CH], BF16)) for i in range(3)]
                t32 = [P.tb("st32_%d" % i) for i in range(3)]
                t16 = [P.tb("st16_%d" % i) for i in range(3)]
                k = [0]

                def conv2d(src, dst):
                    M = src.shape[1]
                    for o in range(0, M, CH):
                        n = min(CH, M - o)
                        i = k[0] % 3
                        k[0] += 1
                        P.dma(st32[i][:, 0:n], src[:, o:o + n], t32[i], True)
                        cast_copy(k[0], st16[i][:, 0:n], st32[i][:, 0:n], [t32[i]], [t16[i]])
                        P.dma(dst[:, o:o + n], st16[i][:, 0:n], t16[i], False)

                def flat(ap2):
                    return ap2.rearrange("(p r) c -> p (r c)", p=128)

                for l in layers:
                    for r0 in range(0, D, 128):
                        for c0 in range(0, IN_COLS, CH):
                            n = min(CH, IN_COLS - c0)
                            i = k[0] % 3
                            k[0] += 1
                            P.dma(st32[i][:, 0:n], w_in[l, r0:r0 + 128, c0:c0 + n], t32[i], True)
                            cast_copy(k[0], st16[i][:, 0:n], st32[i][:, 0:n], [t32[i]], [t16[i]])
                            P.dma(win_bf[l, r0:r0 + 128, c0:c0 + n], st16[i][:, 0:n], t16[i], False)
                        i = k[0] % 3
                        k[0] += 1
                        P.dma(st32[i][:, 0:2048], w_perm[l, r0:r0 + 128, :], t32[i], True)
                        cast_copy(k[0], st16[i][:, 0:2048], st32[i][:, 0:2048], [t32[i]], [t16[i]])
                        P.dma(win_bf[l, r0:r0 + 128, IN_COLS:WCOLS], st16[i][:, 0:2048], t16[i], False)
                    conv2d(flat(w_a_out[l]), flat(wa_bf[l]))
                    conv2d(flat(w_b_out[l]), flat(wb_bf[l]))
                    conv2d(flat(w_c_out[l]), flat(wc_bf[l]))
                    conv2d(flat(w_o[l]), flat(wo_bf[l]))
                if 0 in layers:
                    conv2d(flat(ff_wg), flat(fg_bf))
                    conv2d(flat(ff_wu), flat(fu_bf))
                    conv2d(flat(ff_wd), flat(fd_bf))
                if 1 in layers:
                    for e_ in range(NE):
                        conv2d(flat(moe_wg[e_]), flat(mg_bf[e_]))
                        conv2d(flat(moe_wu[e_]), flat(mu_bf[e_]))
                        conv2d(flat(moe_wd[e_]), flat(md_bf[e_]))
                P.barrier()

        def phaseM():
            with ExitStack() as ps:
                cTs = ps.enter_context(sbt("cTs", [128, 8, 3], F32))
                scT = ps.enter_context(sbt("scT", [128, 8, 3], F32))
                wad = [ps.enter_context(sbt("wad%d" % i, [128, 8, 512], F32)) for i in range(2)]
                twad = [P.tb("wad%d" % i) for i in range(2)]
                badT = ps.enter_context(sbt("badT", [128, L, 48], F32))
                ngT = ps.enter_context(sbt("ngT", [128, 2, L, 8], F32))
                lamt = ps.enter_context(sbt("lamt", [128, 256], F32))
                lprod = ps.enter_context(sbt("lprod", [128, 2, 64], F32))
                lsum = ps.enter_context(sbt("lsum", [128, 2], F32))
                lexp = ps.enter_context(sbt("lexp", [128, 2], F32))
                gsr = ps.enter_context(sbt("gsr", [128, 128], F32))
                tmp8 = ps.enter_context(sbt("tmp8", [128, 8], F32))
                tM = P.tb("tM"); tS = P.tb("tS")
                P.dma(cTs[:], cT[:, :, :], tM, True)
                P.op(act, lambda e: e.activation(out=scT[:], in_=cTs[:], func=AF.Silu), reads=[tM], writes=[tS])
                tb_bad = P.tb("badT")
                for l in range(L):
                    P.dma(badT[:, l, :], b_adaT[l, :, :], tb_bad, True)
                    P.dma(ngT[:, 0, l, :], n1gT[l, :, :], tb_bad, True)
                    P.dma(ngT[:, 1, l, :], n2gT[l, :, :], tb_bad, True)
                tmod = P.tb("modT")
                for l in range(L):
                    pb = bank[l]
                    for grp in range(12):
                        i = grp % 2
                        P.dma(wad[i][:], w_ada[l, :, grp * 512:(grp + 1) * 512].rearrange("(kc p) c -> p kc c", p=128), twad[i], True)
                        for cc in range(4):
                            col = (grp * 4 + cc) * 3
                            for kc in range(8):
                                P.op(pe, lambda e, i=i, cc=cc, kc=kc, col=col, l=l: e.matmul(
                                    psb(l, 3, col), wad[i][:, kc, cc * 128:(cc + 1) * 128], scT[:, kc, :],
                                    start=(kc == 0), stop=(kc == 7)),
                                    reads=[twad[i], tS], writes=[pb], inc=(kc == 7))
                    P.op(dve, lambda e, l=l: e.tensor_tensor(
                        modT[:, l, :, :], psb(l, 144).rearrange("p (c j) -> p c j", j=3),
                        badT[:, l, :].unsqueeze(2).to_broadcast([128, 48, 3]), ALU.add),
                        reads=[pb, tb_bad], writes=[tmod])
                    for j in range(3):
                        for half in range(2):
                            base = half * 24
                            P.op(dve, lambda e, l=l, j=j, base=base: e.tensor_scalar(
                                tmp8[:], modT[:, l, base + 8:base + 16, j], 1.0, None, ALU.add),
                                reads=[tmod], writes=[tM])
                            P.op(dve, lambda e, l=l, j=j, half=half: e.tensor_tensor(
                                der[:, l, j, half * 3 + 0, :], tmp8[:], ngT[:, half, l, :], ALU.mult),
                                reads=[tM, tb_bad], writes=[consts])
                            P.op(dve, lambda e, l=l, j=j, half=half, base=base: e.tensor_copy(
                                der[:, l, j, half * 3 + 1, :], modT[:, l, base:base + 8, j]),
                                reads=[tmod], writes=[consts])
                            P.op(dve, lambda e, l=l, j=j, half=half, base=base: e.tensor_copy(
                                der[:, l, j, half * 3 + 2, :], modT[:, l, base + 16:base + 24, j]),
                                reads=[tmod], writes=[consts])
                    lam_init = 0.8 - 0.6 * math.exp(-0.3 * l)
                    tl = P.tb("lam%d" % l)
                    P.dma(lamt[:], c_lambda[l, :].partition_broadcast(128), tl, True)
                    P.op(dve, lambda e: e.tensor_tensor(lprod[:, 0, :], lamt[:, 0:64], lamt[:, 64:128], ALU.mult), reads=[tl], writes=[tM])
                    P.op(dve, lambda e: e.tensor_tensor(lprod[:, 1, :], lamt[:, 128:192], lamt[:, 192:256], ALU.mult), reads=[tl], writes=[tM])
                    P.op(dve, lambda e: e.tensor_reduce(lsum[:], lprod[:], AX.X, ALU.add), reads=[tM], writes=[tM])
                    P.op(act, lambda e: e.activation(out=lexp[:], in_=lsum[:], func=AF.Exp), reads=[tM], writes=[tS])
                    P.op(dve, lambda e, l=l, li=lam_init: e.scalar_tensor_tensor(
                        neglam[:, l:l + 1], lexp[:, 1:2], -li, lexp[:, 0:1], ALU.add, ALU.subtract),
                        reads=[tS], writes=[consts])
                    tg = P.tb("gsr%d" % l)
                    P.dma(gsr[:], c_subln_g[l, :].partition_broadcast(128), tg, True)
                    P.op(act, lambda e, l=l, li=lam_init: e.mul(gsub[:, l, :], gsr[:], 1.0 - li), reads=[tg], writes=[consts])
                P.barrier()

        def norm_mod(xblk, txb, n, l, j, slot0, dst_fn, tdst, sq, tsq, rstd, trs, tmpf, ttmp, pbank, post=None):
            nsub = (n + 511) // 512
            for sub in range(nsub):
                c0 = sub * 512
                m = min(512, n - c0)
                P.op(act, lambda e: e.activation(out=sq[:, :, 0:m], in_=xblk[:, :, c0:c0 + m], func=AF.Square),
                     reads=[txb], writes=[tsq])
                for kc in range(8):
                    P.op(pe, lambda e, kc=kc: e.matmul(psb(pbank, m), ones_ms[:], sq[:, kc, 0:m], start=(kc == 0), stop=(kc == 7)),
                         reads=[tsq, consts], writes=[bank[pbank]], inc=(kc == 7))
                P.op(act, lambda e: e.activation(out=rstd[:, c0:c0 + m], in_=psb(pbank, m), func=AF.Sqrt, bias=epsT[:, 0:1]),
                     reads=[bank[pbank], consts], writes=[trs])
                P.op(dve, lambda e: e.reciprocal(rstd[:, c0:c0 + m], rstd[:, c0:c0 + m]), reads=[trs], writes=[trs])
            for kc in range(8):
                i = kc % 2
                P.op(dve, lambda e, kc=kc, i=i: e.scalar_tensor_tensor(
                    tmpf[i][:, 0:n], xblk[:, kc, 0:n], der[:, l, j, slot0, kc:kc + 1], rstd[:, 0:n], ALU.mult, ALU.mult),
                    reads=[txb, trs, consts], writes=[ttmp[i]])
                if post is not None:
                    post(kc, i)
                else:
                    P.op(act, lambda e, kc=kc, i=i: e.activation(
                        out=dst_fn(kc), in_=tmpf[i][:, 0:n], func=AF.Identity, bias=der[:, l, j, slot0 + 1, kc:kc + 1]),
                        reads=[ttmp[i], consts], writes=[tdst])

        def wcols(l, c0, ncol):
            return win_bf[l, :, c0:c0 + ncol].rearrange("(kc p) c -> p kc c", p=128)

        def seq_layer(s, l):
            last = (l == L - 1)
            nblk_full = 9
            nblk_lat = 8 if last else 9
            with ExitStack() as sl:
                hT = sl.enter_context(sbt("hT", [128, 8, NT], BF16))
                thT = [P.tb("hT%d" % b, True) for b in range(9)]

                with ExitStack() as ps:
                    xblk = [ps.enter_context(sbt("xblkA%d" % i, [128, 8, 512], F32)) for i in range(2)]
                    txb = [P.tb("xblkA%d" % i) for i in range(2)]
                    sq = ps.enter_context(sbt("sqA", [128, 8, 512], BF16)); tsq = P.tb("sqA")
                    rstd = ps.enter_context(sbt("rstdA", [128, 512], F32)); trs = P.tb("rstdA")
                    tmpf = [ps.enter_context(sbt("tmpA%d" % i, [128, 512], F32)) for i in range(2)]
                    ttmp = [P.tb("tmpA%d" % i) for i in range(2)]
                    if l == 0:
                        xtok = [ps.enter_context(sbt("xtok%d" % i, [128, D], F32)) for i in range(2)]
                        txt = [P.tb("xtok%d" % i) for i in range(2)]
                    tcount = 0
                    for b, (c0, n) in enumerate(BLKS):
                        i = b % 2
                        j = s if b < 8 else 2
                        if l == 0:
                            for tt in range(n // 128):
                                k = tcount % 2
                                tcount += 1
                                src = x2[s, c0 + tt * 128:c0 + (tt + 1) * 128, :] if b < 8 else ctx2[s, tt * 128:(tt + 1) * 128, :]
                                P.dma(xtok[k][:], src, txt[k], True)
                                pbk = 2 * (tcount % 2)
                                pst = psT[pbk // 2]
                                for kc in range(8):
                                    P.op(pe, lambda e, kc=kc, k=k, pst=pst: e.transpose(
                                        pst[:, kc * 128:(kc + 1) * 128], xtok[k][:, kc * 128:(kc + 1) * 128], ident[:]),
                                        reads=[txt[k], ident_t], writes=[bank[pbk + kc // 4]], inc=(kc % 4 == 3))
                                P.op(act, lambda e, pst=pst, i=i, tt=tt: e.copy(
                                    xblk[i][:, :, tt * 128:(tt + 1) * 128], pst[:, :].rearrange("p (k t) -> p k t", k=8)),
                                    reads=[bank[pbk], bank[pbk + 1]], writes=[txb[i]])
                            P.dma(xT_d[s, :, c0:c0 + n].rearrange("(kc p) t -> p kc t", p=128), xblk[i][:, :, 0:n], txb[i], False)
                        else:
                            P.dma(xblk[i][:, :, 0:n], xT_d[s, :, c0:c0 + n].rearrange("(kc p) t -> p kc t", p=128), txb[i], True)
                        norm_mod(xblk[i], txb[i], n, l, j, 0, lambda kc, c0=c0, n=n: hT[:, kc, c0:c0 + n], thT[b],
                                 sq, tsq, rstd, trs, tmpf, ttmp, 4 + (b % 2))
                    P.barrier()
                    if debug_dump:
                        tdbg = P.tb("hTdbg")
                        P.dma(hT_dbg[:, :, :], hT[:, :, :], tdbg, False)
                        P.barrier()
                if stop == "A":
                    return True

                with ExitStack() as ps:
                    WA = ps.enter_context(sbt("WA", [128, 8, 1024], BF16)); tWA = P.tb("WA")
                    ws32 = ps.enter_context(sbt("ws32", [128, 8, 128], F32)); tws32 = P.tb("ws32")
                    wsT = ps.enter_context(sbt("wsT", [128, 8, 128], BF16)); twsT = P.tb("wsT")
                    lng = ps.enter_context(sbt("lng", [128, 512], F32))
                    lnb = ps.enter_context(sbt("lnb", [128, 512], F32))
                    bsT = ps.enter_context(sbt("bsT", [128, 8], F32)); tln = P.tb("ln")
                    u_sb = [ps.enter_context(sbt("u_sb%d" % i, [128, 512], F32)) for i in range(2)]
                    v_sb = [ps.enter_context(sbt("v_sb%d" % i, [128, 512], F32)) for i in range(2)]
                    junk = ps.enter_context(sbt("junkB1", [128, 512], F32))
                    vc = [ps.enter_context(sbt("vc%d" % i, [128, 512], BF16)) for i in range(2)]
                    st = [ps.enter_context(sbt("stB1_%d" % i, [128, 8], F32)) for i in range(2)]
                    yaB = [ps.enter_context(sbt("yaB%d" % i, [128, 4, 512], BF16)) for i in range(2)]
                    tu = [P.tb("u%d" % i) for i in range(2)]; tv = [P.tb("v%d" % i) for i in range(2)]
                    tvc = [P.tb("vc%d" % i) for i in range(2)]; tst = [P.tb("st%d" % i) for i in range(2)]
                    tya = [P.tb("yaB%d" % i) for i in range(2)]; tjunk = P.tb("junkB1")
                    P.dma(WA[:, :, 0:512], wcols(l, 0, 512), tWA, True)
                    P.dma(WA[:, :, 512:1024], wcols(l, 512, 512), tWA, True)
                    P.dma(ws32[:], a_wsT[l, :, :, :], tws32, True)
                    P.op(act, lambda e: e.copy(wsT[:], ws32[:]), reads=[tws32], writes=[twsT])
                    P.dma(lng[:], a_ln_g[l, :].partition_broadcast(128), tln, True)
                    P.dma(lnb[:], a_ln_b[l, :].partition_broadcast(128), tln, True)
                    P.dma(bsT[:], a_bsT[l, :, :], tln, True)
                    ntile = nblk_lat * 4 if nblk_lat == 8 else 34
                    for tt in range(ntile):
                        i = tt % 2
                        b = min(tt // 4, 8)
                        bu, bv, bm, bt = (0, 1, 2, 3) if i == 0 else (4, 5, 6, 7)
                        tok = slice(tt * 128, (tt + 1) * 128)
                        for (bk, cs) in ((bu, 0), (bv, 512)):
                            for kc in range(8):
                                P.op(pe, lambda e, kc=kc, bk=bk, cs=cs: e.matmul(
                                    psb(bk), hT[:, kc, tok], WA[:, kc, cs:cs + 512], start=(kc == 0), stop=(kc == 7)),
                                    reads=[thT[b], tWA], writes=[bank[bk]], inc=(kc == 7))
                        P.op(dve, lambda e: e.memset(st[i][:, 0:2], 0.0), writes=[tst[i]])
                        P.op(act, lambda e: e.activation(out=u_sb[i][:], in_=psb(bu), func=AF.Gelu_apprx_tanh),
                             reads=[bank[bu]], writes=[tu[i]])
                        P.op(act, lambda e: e.activation(out=v_sb[i][:], in_=psb(bv), func=AF.Gelu_apprx_tanh, accum_out=st[i][:, 0:1]),
                             reads=[bank[bv]], writes=[tv[i], tst[i]])
                        P.op(act, lambda e: e.activation(out=junk[:], in_=v_sb[i][:], func=AF.Square, accum_out=st[i][:, 1:2]),
                             reads=[tv[i]], writes=[tjunk, tst[i]])
                        P.op(dve, lambda e: e.tensor_scalar(st[i][:, 2:4], st[i][:, 0:2], 1.0 / 512, None, ALU.mult), reads=[tst[i]], writes=[tst[i]])
                        P.op(dve, lambda e: e.tensor_tensor(st[i][:, 4:5], st[i][:, 2:3], st[i][:, 2:3], ALU.mult), reads=[tst[i]], writes=[tst[i]])
                        P.op(dve, lambda e: e.tensor_tensor(st[i][:, 5:6], st[i][:, 3:4], st[i][:, 4:5], ALU.subtract), reads=[tst[i]], writes=[tst[i]])
                        P.op(act, lambda e: e.activation(out=st[i][:, 6:7], in_=st[i][:, 5:6], func=AF.Sqrt, bias=epsT[:, 0:1]), reads=[tst[i], consts], writes=[tst[i]])
                        P.op(dve, lambda e: e.reciprocal(st[i][:, 7:8], st[i][:, 6:7]), reads=[tst[i]], writes=[tst[i]])
                        P.op(dve, lambda e: e.tensor_scalar(v_sb[i][:], v_sb[i][:], st[i][:, 2:3], st[i][:, 7:8], ALU.subtract, ALU.mult),
                             reads=[tst[i], tv[i]], writes=[tv[i]])
                        P.op(dve, lambda e: e.tensor_tensor(v_sb[i][:], v_sb[i][:], lng[:], ALU.mult), reads=[tv[i], tln], writes=[tv[i]])
                        P.op(dve, lambda e: e.tensor_tensor(vc[i][:], v_sb[i][:], lnb[:], ALU.add), reads=[tv[i], tln], writes=[tvc[i]])
                        for g in range(8):
                            P.op(pe, lambda e, g=g: e.matmul(psb(bm, 64, g * 64), wsT[:, g, :], vc[i][:, g * 64:(g + 1) * 64], start=True, stop=True),
                                 reads=[twsT, tvc[i]], writes=[bank[bm]], inc=(g == 7))
                        P.op(dve, lambda e: e.tensor_tensor(
                            v_sb[i][:].rearrange("p (g e) -> p g e", g=8), psb(bm).rearrange("p (g e) -> p g e", g=8),
                            bsT[:, :].unsqueeze(2).to_broadcast([128, 8, 64]), ALU.add),
                            reads=[bank[bm], tln], writes=[tv[i]])
                        P.op(dve, lambda e: e.tensor_tensor(u_sb[i][:], u_sb[i][:], v_sb[i][:], ALU.mult), reads=[tv[i], tu[i]], writes=[tu[i]])
                        for c in range(4):
                            P.op(pe, lambda e, c=c: e.transpose(psb(bt, 128, c * 128), u_sb[i][:, c * 128:(c + 1) * 128], ident[:]),
                                 reads=[tu[i], ident_t], writes=[bank[bt]], inc=(c == 3))
                        yb_i = (tt // 4) % 2
                        q = tt % 4
                        P.op(act, lambda e, yb_i=yb_i, q=q: e.copy(
                            yaB[yb_i][:, :, q * 128:(q + 1) * 128], psb(bt).rearrange("p (c t) -> p c t", c=4)),
                            reads=[bank[bt]], writes=[tya[yb_i]])
                        c0, n = BLKS[b]
                        if (tt + 1) * 128 == c0 + n:
                            P.dma(yaT_d[s, :, c0:c0 + n].rearrange("(c p) t -> p c t", p=128), yaB[yb_i][:, :, 0:n], tya[yb_i], False)
                    P.barrier()
                if stop == "B1":
                    return True

                with ExitStack() as ps:
                    WB = [ps.enter_context(sbt("WB%d" % i, [128, 3, 8, 128], BF16)) for i in range(2)]
                    tWB = [P.tb("WB%d" % i) for i in range(2)]
                    ZW = NT + 4
                    zf = ps.enter_context(sbt("zf", [128, ZW], F32)); tzf = P.tb("zf")
                    bgf = ps.enter_context(sbt("bgf", [128, NT], F32)); tbg = P.tb("bgf")
                    p4 = [ps.enter_context(sbt("p4_%d" % i, [128, 512], F32)) for i in range(2)]
                    tp4 = [P.tb("p4_%d" % i) for i in range(2)]
                    cv = [ps.enter_context(sbt("cv%d" % i, [128, 512], F32)) for i in range(2)]
                    tcv = [P.tb("cv%d" % i) for i in range(2)]
                    ybo = [ps.enter_context(sbt("ybo%d" % i, [128, 512], BF16)) for i in range(2)]
                    tybo = [P.tb("ybo%d" % i) for i in range(2)]
                    P.op(dve, lambda e: e.memset(zf[:], 0.0), writes=[tzf])

                    def zoff(b):
                        return 1 + BLKS[b][0] if b < 8 else NL + 2

                    def loadWB(fc):
                        i = fc % 2
                        for k3, off in enumerate((OFF_BB, OFF_BC, OFF_BH)):
                            P.dma(WB[i][:, k3, :, :], wcols(l, off + fc * 128, 128), tWB[i], True)
                    loadWB(0)
                    cnt = 0
                    for fc in range(4):
                        i = fc % 2
                        if fc + 1 < 4:
                            loadWB(fc + 1)
                        for b in range(nblk_lat):
                            c0, n = BLKS[b]
                            pbs = (0, 1, 2) if cnt % 2 == 0 else (3, 4, 5)
                            k2 = cnt % 2
                            cnt += 1
                            for k3 in range(3):
                                for kc in range(8):
                                    P.op(pe, lambda e, k3=k3, kc=kc: e.matmul(
                                        psb(pbs[k3], n), WB[i][:, k3, kc, :], hT[:, kc, c0:c0 + n], start=(kc == 0), stop=(kc == 7)),
                                        reads=[tWB[i], thT[b]], writes=[bank[pbs[k3]]], inc=(kc == 7))
                            P.op(act, lambda e: e.copy(p4[k2][:, 0:n], psb(pbs[2], n)), reads=[bank[pbs[2]]], writes=[tp4[k2]])
                            P.op(dve, lambda e: e.tensor_tensor(zf[:, zoff(b):zoff(b) + n], psb(pbs[1], n), p4[k2][:, 0:n], ALU.mult),
                                 reads=[bank[pbs[1]], tp4[k2]], writes=[tzf])
                            P.op(act, lambda e: e.copy(bgf[:, c0:c0 + n], psb(pbs[0], n)), reads=[bank[pbs[0]]], writes=[tbg])
                        for b in range(nblk_lat):
                            c0, n = BLKS[b]
                            k2 = b % 2
                            z0 = zoff(b)
                            P.op(dve, lambda e: e.tensor_scalar(cv[k2][:, 0:n], zf[:, z0 - 1:z0 - 1 + n], convT[:, l, fc, 0:1], None, ALU.mult),
                                 reads=[tzf, cst2], writes=[tcv[k2]])
                            P.op(dve, lambda e: e.scalar_tensor_tensor(cv[k2][:, 0:n], zf[:, z0:z0 + n], convT[:, l, fc, 1:2], cv[k2][:, 0:n], ALU.mult, ALU.add),
                                 reads=[tzf, cst2, tcv[k2]], writes=[tcv[k2]])
                            P.op(dve, lambda e: e.scalar_tensor_tensor(cv[k2][:, 0:n], zf[:, z0 + 1:z0 + 1 + n], convT[:, l, fc, 2:3], cv[k2][:, 0:n], ALU.mult, ALU.add),
                                 reads=[tzf, cst2, tcv[k2]], writes=[tcv[k2]])
                            P.op(dve, lambda e: e.tensor_tensor(ybo[k2][:, 0:n], cv[k2][:, 0:n], bgf[:, c0:c0 + n], ALU.mult),
                                 reads=[tcv[k2], tbg], writes=[tybo[k2]])
                            P.dma(ybT_d[s, fc * 128:(fc + 1) * 128, c0:c0 + n], ybo[k2][:, 0:n], tybo[k2], False)
                    P.barrier()
                if stop == "B2":
                    return True

                with ExitStack() as ps:
                    cosS = ps.enter_context(sbt("cosS", [128, NT], F32))
                    sinS = ps.enter_context(sbt("sinS", [128, NT], F32)); ttab = P.tb("tab")
                    P.dma(cosS[:], cosT[:, :], ttab, True)
                    P.dma(sinS[:], sinT[:, :], ttab, True)
                    W5 = [ps.enter_context(sbt("W5_%d" % i, [128, 5, 8, 128], BF16)) for i in range(2)]
                    tW5 = [P.tb("W5_%d" % i) for i in range(2)]
                    QT = ps.enter_context(sbt("QT", [128, NT], BF16)); tQT = P.tb("QT")
                    KT = ps.enter_context(sbt("KT", [128, NT], BF16)); tKT = P.tb("KT")
                    VH = ps.enter_context(sbt("VH", [128, 34, 130], BF16)); tVH = P.tb("VH")
                    ycH = ps.enter_context(sbt("ycH", [128, NT], BF16)); tycH = P.tb("ycH")
                    r1 = [ps.enter_context(sbt("r1_%d" % i, [128, 512], F32)) for i in range(2)]
                    r2 = [ps.enter_context(sbt("r2_%d" % i, [128, 512], F32)) for i in range(2)]
                    tr1 = [P.tb("r1_%d" % i) for i in range(2)]; tr2 = [P.tb("r2_%d" % i) for i in range(2)]
                    PT = [ps.enter_context(sbt("PT%d" % i, [128, 1024], BF16)) for i in range(3)]
                    tPT = [P.tb("PT%d" % i) for i in range(3)]
                    o_sb = [ps.enter_context(sbt("o_sb%d" % i, [128, 128], F32)) for i in range(2)]
                    to = [P.tb("o_sb%d" % i) for i in range(2)]
                    sm = [ps.enter_context(sbt("sm%d" % i, [128, 8], F32)) for i in range(2)]
                    tsm = [P.tb("sm%d" % i) for i in range(2)]
                    junk = ps.enter_context(sbt("junkB3", [128, 128], F32)); tjunk = P.tb("junkB3")
                    osb = [ps.enter_context(sbt("osb%d" % i, [128, 3, 512], F32)) for i in range(2)]
                    tosb = [P.tb("osb%d" % i) for i in range(2)]
                    P.op(dve, lambda e: e.memset(VH[:, :, 128:130], 1.0), writes=[tVH])
                    P.op(dve, lambda e: e.memset(ycH[:], 0.0), writes=[tycH])

                    def loadW5(h):
                        i = h % 2
                        offs = (OFF_CQ + h * 128, OFF_QP + h * 128, OFF_CK + h * 128, OFF_KP + h * 128, OFF_CV + h * 128)
                        for k5, off in enumerate(offs):
                            P.dma(W5[i][:, k5, :, :], wcols(l, off, 128), tW5[i], True)

                    def oacc(a, ncol=129):
                        return psb(4 + a // 3, ncol, (a % 3) * 160), bank[4 + a // 3]

                    loadW5(0)
                    pcount = [0]
                    for h in range(8):
                        wi = h % 2
                        if h + 1 < 8:
                            loadW5(h + 1)
                        for b in range(nblk_full):
                            c0, n = BLKS[b]
                            need_q = not (last and b == 8)
                            for (kk, dstT, tdst) in ((0, QT, tQT), (2, KT, tKT)):
                                if kk == 0 and not need_q:
                                    continue
                                i2 = pcount[0] % 2
                                pcount[0] += 1
                                b0, b1 = (0, 1) if i2 == 0 else (2, 3)
                                for (bk, k5) in ((b0, kk), (b1, kk + 1)):
                                    for kc in range(8):
                                        P.op(pe, lambda e, kc=kc, bk=bk, k5=k5: e.matmul(
                                            psb(bk, n), W5[wi][:, k5, kc, :], hT[:, kc, c0:c0 + n], start=(kc == 0), stop=(kc == 7)),
                                            reads=[tW5[wi], thT[b]], writes=[bank[bk]], inc=(kc == 7))
                                P.op(dve, lambda e: e.tensor_tensor(r1[i2][:, 0:n], psb(b0, n), cosS[:, c0:c0 + n], ALU.mult),
                                     reads=[bank[b0], ttab], writes=[tr1[i2]])
                                P.op(dve, lambda e: e.tensor_tensor(r2[i2][:, 0:n], psb(b1, n), sinS[:, c0:c0 + n], ALU.mult),
                                     reads=[bank[b1], ttab], writes=[tr2[i2]])
                                P.op(dve, lambda e, dstT=dstT: e.tensor_tensor(dstT[:, c0:c0 + n], r1[i2][:, 0:n], r2[i2][:, 0:n], ALU.add),
                                     reads=[tr1[i2], tr2[i2]], writes=[tdst])
                            nt_ = n // 128
                            for q in range(nt_):
                                tt = c0 // 128 + q
                                for kc in range(8):
                                    P.op(pe, lambda e, kc=kc, q=q, tt=tt: e.matmul(
                                        psb(7, 128, q * 128), hT[:, kc, tt * 128:(tt + 1) * 128], W5[wi][:, 4, kc, :], start=(kc == 0), stop=(kc == 7)),
                                        reads=[tW5[wi], thT[b]], writes=[bank[7]], inc=(kc == 7))
                            P.op(act, lambda e: e.copy(VH[:, c0 // 128:c0 // 128 + nt_, 0:128], psb(7, nt_ * 128).rearrange("p (q d) -> p q d", q=nt_)),
                                 reads=[bank[7]], writes=[tVH])
                        qblocks = [(qb * 512, 512, list(range(34))) for qb in range(8)]
                        if not last:
                            qblocks.append((NL, NCX, [32, 33]))
                        steps = []
                        for bi_, (q0, nq, kts) in enumerate(qblocks):
                            for ki, kt in enumerate(kts):
                                steps.append((bi_, q0, nq, kt, ki, len(kts)))
                        nst = len(steps)

                        def QK(i):
                            bi_, q0, nq, kt, ki, nk = steps[i]
                            sbuf_i = i % 2
                            S = psT[sbuf_i]
                            for m in range(2):
                                P.op(pe, lambda e, m=m: e.matmul(
                                    S[:, m * 512:m * 512 + nq], KT[m * 64:(m + 1) * 64, kt * 128:(kt + 1) * 128],
                                    QT[m * 64:(m + 1) * 64, q0:q0 + nq], start=True, stop=True),
                                    reads=[tKT, tQT], writes=[bank[2 * sbuf_i + m]], inc=(m == 1))

                        def EXP(i):
                            bi_, q0, nq, kt, ki, nk = steps[i]
                            sbuf_i = i % 2
                            pt_i = i % 3
                            S = psT[sbuf_i]
                            sb0 = 2 * sbuf_i
                            if nq == 512:
                                P.op(act, lambda e: e.activation(out=PT[pt_i][:], in_=S[:, :], func=AF.Exp, scale=0.125),
                                     reads=[bank[sb0], bank[sb0 + 1]], writes=[tPT[pt_i]])
                            else:
                                P.op(act, lambda e: e.activation(
                                    out=PT[pt_i][:, :].rearrange("p (m q) -> p m q", m=2)[:, :, 0:nq],
                                    in_=S[:, :].rearrange("p (m q) -> p m q", m=2)[:, :, 0:nq], func=AF.Exp, scale=0.125),
                                    reads=[bank[sb0], bank[sb0 + 1]], writes=[tPT[pt_i]])

                        def AV(i):
                            bi_, q0, nq, kt, ki, nk = steps[i]
                            pt_i = i % 3
                            nqi = nq // 128
                            for qi in range(nqi):
                                for m in range(2):
                                    a = qi * 2 + m
                                    ov, ob = oacc(a)
                                    P.op(pe, lambda e, m=m, qi=qi, ov=ov, a=a: e.matmul(
                                        ov, PT[pt_i][:, m * 512 + qi * 128:m * 512 + (qi + 1) * 128], VH[:, kt, 0:129],
                                        start=(ki == 0 and a % 3 == 0), stop=(ki == nk - 1)),
                                        reads=[tPT[pt_i], tVH], writes=[ob], inc=(qi == nqi - 1 and m == 1))

                        def EPI_evac(bi_):
                            q0, nq, kts = qblocks[bi_]
                            k3 = bi_ % 2
                            nb_ = 3 if nq == 512 else 2
                            for j3 in range(nb_):
                                P.op(dve, lambda e, j3=j3: e.tensor_copy(osb[k3][:, j3, 0:449], psb(4 + j3, 449)),
                                     reads=[bank[4 + j3]], writes=[tosb[k3]])

                        def EPI_rest(bi_):
                            q0, nq, kts = qblocks[bi_]
                            k3 = bi_ % 2
                            nqi = nq // 128
                            for qi in range(nqi):
                                k2 = qi % 2
                                a0, a1 = qi * 2, qi * 2 + 1
                                O0 = osb[k3][:, a0 // 3, (a0 % 3) * 160:(a0 % 3) * 160 + 129]
                                O1 = osb[k3][:, a1 // 3, (a1 % 3) * 160:(a1 % 3) * 160 + 129]
                                rd = [tosb[k3]]
                                P.op(dve, lambda e: e.reciprocal(sm[k2][:, 0:1], O0[:, 128:129]), reads=rd, writes=[tsm[k2]])
                                P.op(dve, lambda e: e.reciprocal(sm[k2][:, 1:2], O1[:, 128:129]), reads=rd, writes=[tsm[k2]])
                                P.op(dve, lambda e: e.tensor_tensor(sm[k2][:, 2:3], sm[k2][:, 1:2], neglam[:, l:l + 1], ALU.mult),
                                     reads=[tsm[k2], consts], writes=[tsm[k2]])
                                P.op(dve, lambda e: e.tensor_scalar(o_sb[k2][:], O0[:, 0:128], sm[k2][:, 0:1], None, ALU.mult),
                                     reads=rd + [tsm[k2]], writes=[to[k2]])
                                P.op(dve, lambda e: e.scalar_tensor_tensor(o_sb[k2][:], O1[:, 0:128], sm[k2][:, 2:3], o_sb[k2][:], ALU.mult, ALU.add),
                                     reads=rd + [tsm[k2], to[k2]], writes=[to[k2]])
                                P.op(dve, lambda e: e.memset(sm[k2][:, 3:4], 0.0), writes=[tsm[k2]])
                                P.op(dve, lambda e: e.tensor_tensor_scan if False else e.tensor_tensor(junk[:], o_sb[k2][:], o_sb[k2][:], ALU.mult),
                                     reads=[to[k2]], writes=[tjunk])
                                P.op(dve, lambda e: e.tensor_reduce(sm[k2][:, 3:4], junk[:], AX.X, ALU.add), reads=[tjunk], writes=[tsm[k2]])
                                P.op(act, lambda e: e.activation(out=sm[k2][:, 4:5], in_=sm[k2][:, 3:4], func=AF.Sqrt, bias=epsT[:, 0:1], scale=1.0 / 128),
                                     reads=[tsm[k2], consts], writes=[tsm[k2]])
                                P.op(dve, lambda e: e.reciprocal(sm[k2][:, 5:6], sm[k2][:, 4:5]), reads=[tsm[k2]], writes=[tsm[k2]])
                                P.op(dve, lambda e: e.scalar_tensor_tensor(o_sb[k2][:], o_sb[k2][:], sm[k2][:, 5:6], gsub[:, l, :], ALU.mult, ALU.mult),
                                     reads=[tsm[k2], to[k2], consts], writes=[to[k2]])
                                P.op(pe, lambda e, qi=qi: e.transpose(psb(7, 128, qi * 128), o_sb[k2][:], ident[:]),
                                     reads=[to[k2], ident_t], writes=[bank[7]])
                            P.op(dve, lambda e: e.tensor_copy(ycH[:, q0:q0 + nq], psb(7, nq)), reads=[bank[7]], writes=[tycH])

                        pending = []
                        QK(0)
                        for i in range(nst):
                            if i + 1 < nst:
                                QK(i + 1)
                            EXP(i)
                            AV(i)
                            bi_, q0, nq, kt, ki, nk = steps[i]
                            if ki == nk - 1:
                                EPI_evac(bi_)
                                pending.append((i + 6, bi_))
                            while pending and (pending[0][0] <= i or i == nst - 1):
                                EPI_rest(pending.pop(0)[1])
                        ncols = NT if not last else NL
                        P.dma(ycT_d[s, h * 128:(h + 1) * 128, 0:ncols], ycH[:, 0:ncols], tycH, False)
                    P.barrier()
                if stop == "B3":
                    return True

                with ExitStack() as ps:
                    WO = ps.enter_context(sbt("WO", [128, 8, D], BF16)); tWO = P.tb("WO")
                    P.dma(WO[:, :, 0:512], wo_bf[l, :, 0:512].rearrange("(kc p) c -> p kc c", p=128), tWO, True)
                    P.dma(WO[:, :, 512:1024], wo_bf[l, :, 512:1024].rearrange("(kc p) c -> p kc c", p=128), tWO, True)
                    WC = [ps.enter_context(sbt("WC%d" % i, [128, 40, 128], BF16)) for i in range(2)]
                    tWC = [P.tb("WC%d" % i) for i in range(2)]
                    yin = [ps.enter_context(sbt("yin%d" % i, [128, 16, 512], BF16)) for i in range(2)]
                    tyin = [P.tb("yin%d" % i) for i in range(2)]
                    xb = [ps.enter_context(sbt("xbC%d" % i, [128, 8, 512], F32)) for i in range(2)]
                    txb = [P.tb("xbC%d" % i) for i in range(2)]
                    gs = [ps.enter_context(sbt("gs%d" % i, [128, 3, 512], F32)) for i in range(2)]
                    tgs = [P.tb("gs%d" % i) for i in range(2)]
                    t1 = [ps.enter_context(sbt("t1_%d" % i, [128, 512], F32)) for i in range(2)]
                    t2 = [ps.enter_context(sbt("t2_%d" % i, [128, 512], F32)) for i in range(2)]
                    tt1 = [P.tb("t1_%d" % i) for i in range(2)]; tt2 = [P.tb("t2_%d" % i) for i in range(2)]
                    yT = ps.enter_context(sbt("yT", [128, 8, 512], BF16)); tyT = P.tb("yT")

                    def loadWC(idx):
                        c = idx % 8
                        i = idx % 2
                        for br in range(3):
                            P.dma(WC[i][:, br * 8:(br + 1) * 8, :], wcols(l, OFF_GATE + br * D + c * 128, 128), tWC[i], True)
                        P.dma(WC[i][:, 24:28, :], wa_bf[l, :, c * 128:(c + 1) * 128].rearrange("(kc p) c -> p kc c", p=128), tWC[i], True)
                        P.dma(WC[i][:, 28:32, :], wb_bf[l, :, c * 128:(c + 1) * 128].rearrange("(kc p) c -> p kc c", p=128), tWC[i], True)
                        P.dma(WC[i][:, 32:40, :], wc_bf[l, :, c * 128:(c + 1) * 128].rearrange("(kc p) c -> p kc c", p=128), tWC[i], True)

                    def loadblk(b):
                        c0, n = BLKS[b]
                        i = b % 2
                        P.dma(yin[i][:, 0:4, 0:n], yaT_d[s, :, c0:c0 + n].rearrange("(c p) t -> p c t", p=128), tyin[i], True)
                        P.dma(yin[i][:, 4:8, 0:n], ybT_d[s, :, c0:c0 + n].rearrange("(c p) t -> p c t", p=128), tyin[i], True)
                        P.dma(yin[i][:, 8:16, 0:n], ycT_d[s, :, c0:c0 + n].rearrange("(c p) t -> p c t", p=128), tyin[i], True)
                        P.dma(xb[i][:, :, 0:n], xT_d[s, :, c0:c0 + n].rearrange("(kc p) t -> p kc t", p=128), txb[i], True)

                    loadblk(0)
                    loadWC(0)
                    idx = 0
                    nb = nblk_lat
                    for b in range(nb):
                        c0, n = BLKS[b]
                        bi = b % 2
                        j = s if b < 8 else 2
                        if b + 1 < nb:
                            loadblk(b + 1)
                        for c in range(8):
                            wi = idx % 2
                            k2 = idx % 2
                            if idx + 1 < nb * 8:
                                loadWC(idx + 1)
                            idx += 1
                            for br in range(3):
                                for kc in range(8):
                                    P.op(pe, lambda e, br=br, kc=kc: e.matmul(
                                        psb(br, n), WC[wi][:, br * 8 + kc, :], hT[:, kc, c0:c0 + n], start=(kc == 0), stop=(kc == 7)),
                                        reads=[tWC[wi], thT[b]], writes=[bank[br]], inc=(kc == 7))
                            for (br, k0, nk, y0) in ((0, 24, 4, 0), (1, 28, 4, 4), (2, 32, 8, 8)):
                                for kc in range(nk):
                                    P.op(pe, lambda e, br=br, kc=kc, k0=k0, y0=y0: e.matmul(
                                        psb(3 + br, n), WC[wi][:, k0 + kc, :], yin[bi][:, y0 + kc, 0:n], start=(kc == 0), stop=(kc == nk - 1)),
                                        reads=[tWC[wi], tyin[bi]], writes=[bank[3 + br]], inc=(kc == nk - 1))
                            for br in range(3):
                                P.op(act, lambda e, br=br: e.activation(out=gs[k2][:, br, 0:n], in_=psb(br, n), func=AF.Sigmoid,
                                                                        bias=bgT[:, l, br * 8 + c:br * 8 + c + 1]),
                                     reads=[bank[br], cst2], writes=[tgs[k2]])
                            P.op(dve, lambda e: e.tensor_tensor(t1[k2][:, 0:n], psb(3, n), gs[k2][:, 0, 0:n], ALU.mult), reads=[bank[3], tgs[k2]], writes=[tt1[k2]])
                            P.op(dve, lambda e: e.tensor_tensor(t2[k2][:, 0:n], psb(4, n), gs[k2][:, 1, 0:n], ALU.mult), reads=[bank[4], tgs[k2]], writes=[tt2[k2]])
                            P.op(dve, lambda e: e.tensor_tensor(t1[k2][:, 0:n], t1[k2][:, 0:n], t2[k2][:, 0:n], ALU.add), reads=[tt1[k2], tt2[k2]], writes=[tt1[k2]])
                            P.op(dve, lambda e: e.tensor_tensor(t2[k2][:, 0:n], psb(5, n), gs[k2][:, 2, 0:n], ALU.mult), reads=[bank[5], tgs[k2]], writes=[tt2[k2]])
                            P.op(dve, lambda e, c=c: e.tensor_tensor(yT[:, c, 0:n], t1[k2][:, 0:n], t2[k2][:, 0:n], ALU.add), reads=[tt1[k2], tt2[k2]], writes=[tyT])
                        for c in range(8):
                            pb_ = 6 + (c % 2)
                            for kc in range(8):
                                P.op(pe, lambda e, c=c, kc=kc, pb_=pb_: e.matmul(
                                    psb(pb_, n), WO[:, kc, c * 128:(c + 1) * 128], yT[:, kc, 0:n], start=(kc == 0), stop=(kc == 7)),
                                    reads=[tWO, tyT], writes=[bank[pb_]], inc=(kc == 7))
                            P.op(dve, lambda e, c=c, pb_=pb_: e.scalar_tensor_tensor(
                                xb[bi][:, c, 0:n], psb(pb_, n), der[:, l, j, 2, c:c + 1], xb[bi][:, c, 0:n], ALU.mult, ALU.add),
                                reads=[bank[pb_], consts, txb[bi]], writes=[txb[bi]])
                        P.dma(xT_d[s, :, c0:c0 + n].rearrange("(kc p) t -> p kc t", p=128), xb[bi][:, :, 0:n], txb[bi], False)
                    P.barrier()
                if stop == "C1":
                    return True
            if l == 0:
                phaseC2_dense(s, l)
            else:
                phaseC2_moe(s, l)

        def phaseC2_dense(s, l):
            with ExitStack() as ps:
                xb = [ps.enter_context(sbt("xbD%d" % i, [128, 8, 512], F32)) for i in range(2)]
                txb = [P.tb("xbD%d" % i) for i in range(2)]
                sq = ps.enter_context(sbt("sqD", [128, 8, 512], BF16)); tsq = P.tb("sqD")
                rstd = ps.enter_context(sbt("rstdD", [128, 512], F32)); trs = P.tb("rstdD")
                tmpf = [ps.enter_context(sbt("tmpD%d" % i, [128, 512], F32)) for i in range(2)]
                ttmp = [P.tb("tmpD%d" % i) for i in range(2)]
                h2 = ps.enter_context(sbt("h2D", [128, 8, 512], BF16)); th2 = P.tb("h2D")
                hid = ps.enter_context(sbt("hidD", [128, NJ_FF, 512], BF16)); thid = P.tb("hidD")
                W1 = [ps.enter_context(sbt("W1D%d" % i, [128, 2, 8, 128], BF16)) for i in range(3)]
                tW1 = [P.tb("W1D%d" % i) for i in range(3)]
                W2 = [ps.enter_context(sbt("W2D%d" % i, [128, NJ_FF, 128], BF16)) for i in range(2)]
                tW2 = [P.tb("W2D%d" % i) for i in range(2)]
                ssb = [ps.enter_context(sbt("ssbD%d" % i, [128, 512], F32)) for i in range(2)]
                tss = [P.tb("ssbD%d" % i) for i in range(2)]

                def loadW1(idx):
                    jc = idx % NJ_FF
                    i = idx % 3
                    P.dma(W1[i][:, 0, :, :], fg_bf[:, jc * 128:(jc + 1) * 128].rearrange("(kc p) c -> p kc c", p=128), tW1[i], True)
                    P.dma(W1[i][:, 1, :, :], fu_bf[:, jc * 128:(jc + 1) * 128].rearrange("(kc p) c -> p kc c", p=128), tW1[i], True)

                def loadW2(idx):
                    c = idx % 8
                    i = idx % 2
                    P.dma(W2[i][:], fd_bf[:, c * 128:(c + 1) * 128].rearrange("(jc p) c -> p jc c", p=128), tW2[i], True)

                def loadx(b):
                    c0, n = BLKS[b]
                    P.dma(xb[b % 2][:, :, 0:n], xT_d[s, :, c0:c0 + n].rearrange("(kc p) t -> p kc t", p=128), txb[b % 2], True)

                nb = 9
                loadx(0)
                loadW1(0); loadW1(1)
                loadW2(0)
                i1 = 0
                i2 = 0
                for b in range(nb):
                    c0, n = BLKS[b]
                    bi = b % 2
                    j = s if b < 8 else 2
                    if b + 1 < nb:
                        loadx(b + 1)
                    norm_mod(xb[bi], txb[bi], n, l, j, 3, lambda kc, n=n: h2[:, kc, 0:n], th2, sq, tsq, rstd, trs, tmpf, ttmp, 6)
                    for jc in range(NJ_FF):
                        wi = i1 % 3
                        k2 = i1 % 2
                        if i1 + 2 < nb * NJ_FF:
                            loadW1(i1 + 2)
                        i1 += 1
                        bg_, bu_ = (0, 1) if k2 == 0 else (2, 3)
                        for (bk, gi) in ((bg_, 0), (bu_, 1)):
                            for kc in range(8):
                                P.op(pe, lambda e, bk=bk, gi=gi, kc=kc: e.matmul(
                                    psb(bk, n), W1[wi][:, gi, kc, :], h2[:, kc, 0:n], start=(kc == 0), stop=(kc == 7)),
                                    reads=[tW1[wi], th2], writes=[bank[bk]], inc=(kc == 7))
                        P.op(act, lambda e: e.activation(out=ssb[k2][:, 0:n], in_=psb(bg_, n), func=AF.Silu), reads=[bank[bg_]], writes=[tss[k2]])
                        P.op(dve, lambda e, jc=jc: e.tensor_tensor(hid[:, jc, 0:n], psb(bu_, n), ssb[k2][:, 0:n], ALU.mult),
                             reads=[bank[bu_], tss[k2]], writes=[thid])
                    for c in range(8):
                        wi = i2 % 2
                        if i2 + 1 < nb * 8:
                            loadW2(i2 + 1)
                        i2 += 1
                        pb_ = 4 + (c % 2)
                        for jc in range(NJ_FF):
                            P.op(pe, lambda e, jc=jc, pb_=pb_: e.matmul(
                                psb(pb_, n), W2[wi][:, jc, :], hid[:, jc, 0:n], start=(jc == 0), stop=(jc == NJ_FF - 1)),
                                reads=[tW2[wi], thid], writes=[bank[pb_]], inc=(jc == NJ_FF - 1))
                        P.op(dve, lambda e, c=c, pb_=pb_: e.scalar_tensor_tensor(
                            xb[bi][:, c, 0:n], psb(pb_, n), der[:, l, j, 5, c:c + 1], xb[bi][:, c, 0:n], ALU.mult, ALU.add),
                            reads=[bank[pb_], consts, txb[bi]], writes=[txb[bi]])
                    P.dma(xT_d[s, :, c0:c0 + n].rearrange("(kc p) t -> p kc t", p=128), xb[bi][:, :, 0:n], txb[bi], False)
                P.barrier()

        def phaseC2_moe(s, l):
            T = 1024
            with ExitStack() as ps:
                xb = ps.enter_context(sbt("xbM", [128, 8, T], F32)); txb = P.tb("xbM")
                rstd = ps.enter_context(sbt("rstdM", [128, T], F32)); trs = P.tb("rstdM")
                tmpf = [ps.enter_context(sbt("tmpM%d" % i, [128, T], F32)) for i in range(2)]
                ttmp = [P.tb("tmpM%d" % i) for i in range(2)]
                h2f = [ps.enter_context(sbt("h2f%d" % i, [128, T], F32)) for i in range(2)]
                th2f = [P.tb("h2f%d" % i) for i in range(2)]
                h2 = ps.enter_context(sbt("h2M", [128, 8, T], BF16)); th2 = P.tb("h2M")
                hid = ps.enter_context(sbt("hidM", [128, NJ_E, T], BF16)); thid = P.tb("hidM")
                sq = hid[:, 0:8, 0:512]; tsq = thid
                acc = ps.enter_context(sbt("accM", [128, 8, T], F32)); tacc = P.tb("accM")
                cbc = [ps.enter_context(sbt("cbc%d" % i, [128, T], F32)) for i in range(2)]
                tcbc = [P.tb("cbc%d" % i) for i in range(2)]
                W1 = [ps.enter_context(sbt("W1M%d" % i, [128, 2, 8, 128], BF16)) for i in range(3)]
                tW1 = [P.tb("W1M%d" % i) for i in range(3)]
                W2 = [ps.enter_context(sbt("W2M%d" % i, [128, NJ_E, 128], BF16)) for i in range(2)]
                tW2 = [P.tb("W2M%d" % i) for i in range(2)]
                wr = ps.enter_context(sbt("wrM", [128, 8, NE], F32)); twr = P.tb("wrM")
                lg = ps.enter_context(sbt("lgM", [128, 8, NE], F32))
                lg2 = ps.enter_context(sbt("lg2M", [128, 8, NE], F32))
                mk1 = ps.enter_context(sbt("mk1M", [128, 8, NE], F32))
                mk2 = ps.enter_context(sbt("mk2M", [128, 8, NE], F32))
                comb = ps.enter_context(sbt("combM", [128, 8, NE], F32))
                rs = ps.enter_context(sbt("rsM", [128, 6, 8], F32)); trt = P.tb("routeM")
                combT = ps.enter_context(sbt("combTM", [8, T], F32)); tcT = P.tb("combTM")
                sel = ps.enter_context(sbt("selM", [8, NE, 128], F32)); tsel = P.tb("selM")
                P.dma(wr[:], moe_wr[:, :, :], twr, True)
                for e_ in range(NE):
                    P.op(dve, lambda e, e_=e_: e.tensor_copy(sel[:, e_, :], ident[0:8, e_:e_ + 1].to_broadcast([8, 128])),
                         reads=[ident_t], writes=[tsel])

                def loadW1(idx):
                    e_ = (idx // NJ_E) % NE
                    jc = idx % NJ_E
                    i = idx % 3
                    P.dma(W1[i][:, 0, :, :], mg_bf[e_, :, jc * 128:(jc + 1) * 128].rearrange("(kc p) c -> p kc c", p=128), tW1[i], True)
                    P.dma(W1[i][:, 1, :, :], mu_bf[e_, :, jc * 128:(jc + 1) * 128].rearrange("(kc p) c -> p kc c", p=128), tW1[i], True)

                def loadW2(idx):
                    e_ = (idx // 8) % NE
                    c = idx % 8
                    i = idx % 2
                    P.dma(W2[i][:], md_bf[e_, :, c * 128:(c + 1) * 128].rearrange("(jc p) c -> p jc c", p=128), tW2[i], True)

                nb = NL // T
                loadW1(0); loadW1(1)
                loadW2(0)
                i1 = 0
                i2 = 0
                for b in range(nb):
                    c0 = b * T
                    P.dma(xb[:], xT_d[s, :, c0:c0 + T].rearrange("(kc p) t -> p kc t", p=128), txb, True)

                    def post(kc, i):
                        k2 = kc % 2
                        P.op(act, lambda e: e.activation(out=h2f[k2][:], in_=tmpf[i][:], func=AF.Identity, bias=der[:, l, s, 4, kc:kc + 1]),
                             reads=[ttmp[i], consts], writes=[th2f[k2]])
                        for tt in range(8):
                            P.op(pe, lambda e, tt=tt: e.matmul(psb(7, NE, tt * NE), h2f[k2][:, tt * 128:(tt + 1) * 128], wr[:, kc, :],
                                                               start=(kc == 0 and tt == 0), stop=(kc == 7)),
                                 reads=[th2f[k2], twr], writes=[bank[7]], inc=(tt == 7))
                        P.op(dve, lambda e: e.tensor_copy(h2[:, kc, :], h2f[k2][:]), reads=[th2f[k2]], writes=[th2])
                    norm_mod(xb, txb, T, l, s, 3, None, None, sq, tsq, rstd, trs, tmpf, ttmp, 6, post=post)
                    lgp = psb(7, 64).rearrange("p (t e) -> p t e", e=NE)
                    R = [trt]
                    P.op(dve, lambda e: e.tensor_copy(lg[:], lgp), reads=[bank[7]], writes=R)
                    P.op(dve, lambda e: e.tensor_reduce(rs[:, 0, :], lg[:], AX.X, ALU.max), reads=R, writes=R)
                    P.op(dve, lambda e: e.tensor_tensor(mk1[:], lg[:], rs[:, 0, :].unsqueeze(2).to_broadcast([128, 8, NE]), ALU.is_equal), reads=R, writes=R)
                    P.op(dve, lambda e: e.scalar_tensor_tensor(lg2[:], mk1[:], -1e30, lg[:], ALU.mult, ALU.add), reads=R, writes=R)
                    P.op(dve, lambda e: e.tensor_reduce(rs[:, 1, :], lg2[:], AX.X, ALU.max), reads=R, writes=R)
                    P.op(dve, lambda e: e.tensor_tensor(mk2[:], lg2[:], rs[:, 1, :].unsqueeze(2).to_broadcast([128, 8, NE]), ALU.is_equal), reads=R, writes=R)
                    P.op(dve, lambda e: e.tensor_tensor(rs[:, 2, :], rs[:, 1, :], rs[:, 0, :], ALU.subtract), reads=R, writes=R)
                    P.op(act, lambda e: e.activation(out=rs[:, 3, :], in_=rs[:, 2, :], func=AF.Exp), reads=R, writes=R)
                    P.op(dve, lambda e: e.tensor_scalar(rs[:, 4, :], rs[:, 3, :], 1.0, None, ALU.add), reads=R, writes=R)
                    P.op(dve, lambda e: e.reciprocal(rs[:, 4, :], rs[:, 4, :]), reads=R, writes=R)
                    P.op(dve, lambda e: e.tensor_tensor(rs[:, 5, :], rs[:, 3, :], rs[:, 4, :], ALU.mult), reads=R, writes=R)
                    P.op(dve, lambda e: e.tensor_tensor(mk1[:], mk1[:], rs[:, 4, :].unsqueeze(2).to_broadcast([128, 8, NE]), ALU.mult), reads=R, writes=R)
                    P.op(dve, lambda e: e.tensor_tensor(mk2[:], mk2[:], rs[:, 5, :].unsqueeze(2).to_broadcast([128, 8, NE]), ALU.mult), reads=R, writes=R)
                    P.op(dve, lambda e: e.tensor_tensor(comb[:], mk1[:], mk2[:], ALU.add), reads=R, writes=R)
                    for tt in range(8):
                        P.op(pe, lambda e, tt=tt: e.transpose(psT[3][0:8, tt * 128:(tt + 1) * 128], comb[:, tt, :], ident[:]),
                             reads=R + [ident_t], writes=[bank[6 + tt // 4]], inc=(tt % 4 == 3))
                    P.op(act, lambda e: e.copy(combT[:], psT[3][0:8, :]), reads=[bank[6], bank[7]], writes=[tcT])
                    for e_ in range(NE):
                        ci = e_ % 2
                        for sub in range(2):
                            P.op(pe, lambda e, sub=sub: e.matmul(psb(6 + sub), sel[:, e_, :], combT[:, sub * 512:(sub + 1) * 512], start=True, stop=True),
                                 reads=[tsel, tcT], writes=[bank[6 + sub]])
                        P.op(act, lambda e: e.copy(cbc[ci][:], psT[3][:, :]), reads=[bank[6], bank[7]], writes=[tcbc[ci]])
                        for jc in range(NJ_E):
                            wi = i1 % 3
                            k2 = i1 % 2
                            if i1 + 2 < nb * NE * NJ_E:
                                loadW1(i1 + 2)
                            i1 += 1
                            for gi in range(2):
                                for sub in range(2):
                                    for kc in range(8):
                                        P.op(pe, lambda e, gi=gi, sub=sub, kc=kc: e.matmul(
                                            psb(gi * 2 + sub), W1[wi][:, gi, kc, :], h2[:, kc, sub * 512:(sub + 1) * 512], start=(kc == 0), stop=(kc == 7)),
                                            reads=[tW1[wi], th2], writes=[bank[gi * 2 + sub]], inc=(kc == 7))
                            P.op(act, lambda e: e.activation(out=tmpf[k2][:], in_=psT[0][:, :], func=AF.Silu), reads=[bank[0], bank[1]], writes=[ttmp[k2]])
                            P.op(dve, lambda e, jc=jc: e.tensor_tensor(hid[:, jc, :], psT[1][:, :], tmpf[k2][:], ALU.mult),
                                 reads=[bank[2], bank[3], ttmp[k2]], writes=[thid])
                        for c in range(8):
                            wi = i2 % 2
                            if i2 + 1 < nb * NE * 8:
                                loadW2(i2 + 1)
                            i2 += 1
                            for sub in range(2):
                                for jc in range(NJ_E):
                                    P.op(pe, lambda e, jc=jc, sub=sub: e.matmul(
                                        psb(4 + sub), W2[wi][:, jc, :], hid[:, jc, sub * 512:(sub + 1) * 512], start=(jc == 0), stop=(jc == NJ_E - 1)),
                                        reads=[tW2[wi], thid], writes=[bank[4 + sub]], inc=(jc == NJ_E - 1))
                            if e_ == 0:
                                P.op(dve, lambda e, c=c: e.tensor_tensor(acc[:, c, :], psT[2][:, :], cbc[ci][:], ALU.mult),
                                     reads=[bank[4], bank[5], tcbc[ci]], writes=[tacc])
                            else:
                                k2 = c % 2
                                P.op(dve, lambda e: e.tensor_tensor(h2f[k2][:], psT[2][:, :], cbc[ci][:], ALU.mult),
                                     reads=[bank[4], bank[5], tcbc[ci]], writes=[th2f[k2]])
                                P.op(dve, lambda e, c=c: e.tensor_tensor(acc[:, c, :], acc[:, c, :], h2f[k2][:], ALU.add),
                                     reads=[th2f[k2], tacc], writes=[tacc])
                    for c in range(8):
                        P.op(dve, lambda e, c=c: e.scalar_tensor_tensor(xb[:, c, :], acc[:, c, :], der[:, l, s, 5, c:c + 1], xb[:, c, :], ALU.mult, ALU.add),
                             reads=[tacc, consts, txb], writes=[txb])
                    for sub in range(2):
                        P.op(act, lambda e, sub=sub: e.activation(out=sq[:, :, :], in_=xb[:, :, sub * 512:(sub + 1) * 512], func=AF.Square), reads=[txb], writes=[tsq])
                        for kc in range(8):
                            P.op(pe, lambda e, kc=kc: e.matmul(psb(6), ones_ms[:], sq[:, kc, :], start=(kc == 0), stop=(kc == 7)),
                                 reads=[tsq, consts], writes=[bank[6]], inc=(kc == 7))
                        P.op(act, lambda e, sub=sub: e.activation(out=rstd[:, sub * 512:(sub + 1) * 512], in_=psb(6), func=AF.Sqrt, bias=epsT[:, 0:1]),
                             reads=[bank[6], consts], writes=[trs])
                    P.op(dve, lambda e: e.reciprocal(rstd[:], rstd[:]), reads=[trs], writes=[trs])
                    for c in range(8):
                        P.op(dve, lambda e, c=c: e.scalar_tensor_tensor(xb[:, c, :], xb[:, c, :], fgs[:, c:c + 1], rstd[:], ALU.mult, ALU.mult),
                             reads=[txb, trs, cst2], writes=[txb])
                    for tt in range(8):
                        k2 = tt % 2
                        pst = psT[k2]
                        for c in range(8):
                            P.op(pe, lambda e, c=c, tt=tt, pst=pst: e.transpose(pst[:, c * 128:(c + 1) * 128], xb[:, c, tt * 128:(tt + 1) * 128], ident[:]),
                                 reads=[txb, ident_t], writes=[bank[2 * k2 + c // 4]], inc=(c % 4 == 3))
                        P.op(act, lambda e, pst=pst: e.copy(tmpf[k2][:], pst[:, :]), reads=[bank[2 * k2], bank[2 * k2 + 1]], writes=[ttmp[k2]])
                        P.dma(out2[s, c0 + tt * 128:c0 + (tt + 1) * 128, :], tmpf[k2][:], ttmp[k2], False)
                P.barrier()

        phase0()
        phaseM()
        stopped = False
        for s in seqs:
            for l in layers:
                if not stopped:
                    stopped = bool(seq_layer(s, l))
        P.barrier()
        print("instructions:", P.ninst, "sems:", P.nsem)
    return nc


def _rope_tables():
    t = np.arange(NL)
    row = (t // 64).astype(np.float64)
    col = (t % 64).astype(np.float64)
    inv = 10000.0 ** (-np.arange(16, dtype=np.float64) / 16)
    cosT = np.ones((128, NT), np.float32)
    sinT = np.zeros((128, NT), np.float32)
    for p in range(128):
        d = p % 64
        a, j, i = d // 32, (d // 16) % 2, d % 16
        pos = row if a == 0 else col
        ang = (pos.astype(np.float32) * np.float32(inv[i])).astype(np.float32)
        cosT[p, :NL] = np.cos(ang)
        sn = np.sin(ang)
        sinT[p, :NL] = -sn if j == 0 else sn
    return cosT, sinT


def _perm_cols():
    idx = np.arange(1024)
    d = idx % 64
    base = idx - d
    a, j, i = d // 32, (d // 16) % 2, d % 16
    return base + a * 32 + (1 - j) * 16 + i


def _prep_shared(inp):
    f = lambda a: np.ascontiguousarray(a, dtype=np.float32)
    T128 = lambda v: np.ascontiguousarray(v.reshape(-1, 128).T)
    perm = _perm_cols()
    w_in = inp["w_in"]
    w_perm = np.concatenate([w_in[:, :, OFF_CQ:OFF_CQ + 1024][:, :, perm], w_in[:, :, OFF_CK:OFF_CK + 1024][:, :, perm]], axis=2)
    cosT, sinT = _rope_tables()
    sh = {
        "w_ada": f(inp["w_ada"]),
        "b_adaT": f(np.stack([T128(inp["b_ada"][l]) for l in range(L)])),
        "n1gT": f(np.stack([T128(inp["norm1_g"][l]) for l in range(L)])),
        "n2gT": f(np.stack([T128(inp["norm2_g"][l]) for l in range(L)])),
        "fgT": f(T128(inp["final_norm_g"])),
        "w_in": f(w_in),
        "w_perm": f(w_perm),
        "b_gateT": f(np.stack([T128(inp["b_gate"][l]) for l in range(L)])),
        "a_ln_g": f(inp["a_ln_g"]),
        "a_ln_b": f(inp["a_ln_b"]),
        "a_wsT": f(np.transpose(inp["a_ws"], (0, 3, 1, 2))),
        "a_bsT": f(np.transpose(inp["a_bs"], (0, 2, 1))),
        "b_convT": f(np.stack([np.transpose(inp["b_conv"][l].reshape(3, 4, 128), (2, 1, 0)) for l in range(L)])),
        "c_lambda": f(inp["c_lambda"].reshape(L, 256)),
        "c_subln_g": f(inp["c_subln_g"]),
        "w_a_out": f(inp["w_a_out"]), "w_b_out": f(inp["w_b_out"]),
        "w_c_out": f(inp["w_c_out"]), "w_o": f(inp["w_o"]),
        "ff_wg": f(inp["ff_w_gate"][0]), "ff_wu": f(inp["ff_w_up"][0]), "ff_wd": f(inp["ff_w_down"][0]),
        "moe_wr": f(np.transpose(inp["moe_w_router"][0].reshape(8, 128, NE), (1, 0, 2))),
        "moe_wg": f(inp["moe_w_gate"][0]), "moe_wu": f(inp["moe_w_up"][0]), "moe_wd": f(inp["moe_w_down"][0]),
        "cosT": cosT, "sinT": sinT,
        "ident": np.eye(128, dtype=np.float32),
    }
    return sh


def _core_inputs(inp, shared, core):
    b0 = 2 * core
    c3 = np.stack([inp["c"][b0], inp["c"][b0 + 1], inp["c_ctx"]], axis=1)
    cT = np.ascontiguousarray(np.transpose(c3.reshape(8, 128, 3), (1, 0, 2)), dtype=np.float32)
    m = dict(shared)
    m["x2"] = np.ascontiguousarray(inp["x"][b0:b0 + 2], dtype=np.float32)
    m["ctx2"] = np.ascontiguousarray(inp["ctx"][b0:b0 + 2], dtype=np.float32)
    m["cT"] = cT
    return m


def kernel(**inputs):
    inp = {k: np.asarray(v) for k, v in inputs.items()}
    nc = build_nc()
    shared = _prep_shared(inp)
    in_maps = [_core_inputs(inp, shared, c) for c in range(8)]
    res = run_bass_kernel_spmd(nc, in_maps, core_ids=list(range(8)))
    out = np.concatenate([np.asarray(r["out2"]) for r in res.results], axis=0)
    return out.astype(np.float32)
```

```python
import math
from contextlib import ExitStack
import numpy as np
import concourse.bass as bass
import concourse.mybir as mybir
from concourse.bass_utils import run_bass_kernel_spmd

F32 = mybir.dt.float32
BF16 = mybir.dt.bfloat16
AF = mybir.ActivationFunctionType
ALU = mybir.AluOpType
AX = mybir.AxisListType

D = 1024
NL = 4096
NCX = 256
NT = NL + NCX
L = 2
EPS = 1e-6
OFF_AU, OFF_AV, OFF_BB, OFF_BC, OFF_BH = 0, 512, 1024, 1536, 2048
OFF_CQ, OFF_CK, OFF_CV, OFF_GATE = 2560, 3584, 4608, 5632
IN_COLS = 8704
WCOLS = IN_COLS + 2048
OFF_QP, OFF_KP = IN_COLS, IN_COLS + 1024
DFF = 2816
NE = 8
DFE = 3584
NJ_FF = DFF // 128
NJ_E = DFE // 128
BLKS = [(i * 512, 512) for i in range(8)] + [(NL, NCX)]


class Eng:
    def __init__(self, name, e, sem):
        self.name, self.e, self.sem, self.cnt, self.seen = name, e, sem, 0, {}


class TB:
    def __init__(self, name):
        self.name, self.w, self.r, self.dsem = name, None, {}, None


class DSem:
    def __init__(self, sem):
        self.sem, self.cnt = sem, 0


class Prog:
    def __init__(self, nc, es):
        self.nc, self.es = nc, es
        self.nsem = 0
        self.pe = Eng("pe", nc.tensor, self.mksem("s_pe"))
        self.act = Eng("act", nc.scalar, self.mksem("s_act"))
        self.dve = Eng("dve", nc.vector, self.mksem("s_dve"))
        self.pool = Eng("pool", nc.gpsimd, self.mksem("s_pool"))
        self.sp = Eng("sp", nc.sync, self.mksem("s_sp"))
        self.engs = [self.pe, self.act, self.dve, self.pool, self.sp]
        self.bar = self.mksem("s_bar")
        self.barcnt = 0
        self.tbs = []
        self.free_dsems = []
        self.dsems = []
        self.keep = set()
        self.ninst = 0

    def mksem(self, name):
        self.nsem += 1
        return self.es.enter_context(self.nc.semaphore(name))

    def tb(self, name, keep=False):
        t = TB(name)
        self.tbs.append(t)
        if keep:
            self.keep.add(name)
        return t

    def _wait(self, E, ev, raw=False):
        sem, val = ev
        if sem is E.sem and not (raw and E.name in ("act", "dve", "pool")):
            return
        k = id(sem)
        if E.seen.get(k, 0) >= val:
            return
        E.e.wait_ge(sem, val)
        self.ninst += 1
        E.seen[k] = val

    def _deps(self, E, reads, writes):
        for b in reads:
            if b.w is not None:
                self._wait(E, b.w, raw=True)
        for b in writes:
            if b.w is not None:
                self._wait(E, b.w)
            for ev in b.r.values():
                self._wait(E, ev)

    def _post(self, ev, reads, writes):
        for b in reads:
            b.r[id(ev[0])] = ev
        for b in writes:
            b.w = ev
            b.r = {}

    def op(self, E, fn, reads=(), writes=(), inc=True):
        self._deps(E, reads, writes)
        ins = fn(E.e)
        self.ninst += 1
        if inc:
            E.cnt += 1
            ins.then_inc(E.sem, 1)
            ev = (E.sem, E.cnt)
        else:
            ev = (E.sem, E.cnt + 1)
        self._post(ev, reads, writes)

    def dma(self, out, in_, sb, load, Q=None):
        Q = Q or self.sp
        if sb.dsem is None:
            if self.free_dsems:
                sb.dsem = self.free_dsems.pop()
            else:
                sb.dsem = DSem(self.mksem("dsem%d" % len(self.dsems)))
                self.dsems.append(sb.dsem)
        ds = sb.dsem
        reads = [] if load else [sb]
        writes = [sb] if load else []
        self._deps(Q, reads, writes)
        Q.e.dma_start(out=out, in_=in_).then_inc(ds.sem, 16)
        self.ninst += 1
        ds.cnt += 16
        self._post((ds.sem, ds.cnt), reads, writes)

    def barrier(self):
        sp = self.sp
        for E in self.engs:
            if E is not sp and E.cnt > 0:
                self._wait(sp, (E.sem, E.cnt))
        for ds in self.dsems:
            if ds.cnt:
                self._wait(sp, (ds.sem, ds.cnt))
        self.barcnt += 1
        sp.e.sem_inc(self.bar, 1)
        for E in self.engs:
            if E is sp:
                continue
            E.e.wait_ge(self.bar, self.barcnt)
            for E2 in self.engs:
                if E2.cnt:
                    E.seen[id(E2.sem)] = E2.cnt
            for ds in self.dsems:
                E.seen[id(ds.sem)] = ds.cnt
        for t in self.tbs:
            t.w = None
            t.r = {}
            if t.dsem is not None:
                self.free_dsems.append(t.dsem)
                t.dsem = None
        self.tbs = [t for t in self.tbs if t.name in self.keep]


class _Stop(Exception):
    pass


def build_nc(seqs=(0, 1), layers=(0, 1), debug_dump=False, stop=None):
    nc = bass.Bass("TRN2", target_bir_lowering=False)

    _uid = [0]

    def sbt(name, shape, dt):
        _uid[0] += 1
        return nc.sbuf_tensor("%s_u%d" % (name, _uid[0]), list(shape), dt)

    def din(name, shape, dt=F32):
        return nc.dram_tensor(name, list(shape), dt, kind="ExternalInput").ap()

    def dscr(name, shape, dt):
        return nc.dram_tensor(name, list(shape), dt).ap()

    x2 = din("x2", [2, NL, D])
    ctx2 = din("ctx2", [2, NCX, D])
    cT = din("cT", [128, 8, 3])
    w_ada = din("w_ada", [L, D, 6 * D])
    b_adaT = din("b_adaT", [L, 128, 48])
    n1gT = din("n1gT", [L, 128, 8])
    n2gT = din("n2gT", [L, 128, 8])
    fgT = din("fgT", [128, 8])
    w_in = din("w_in", [L, D, IN_COLS])
    w_perm = din("w_perm", [L, D, 2048])
    b_gateT = din("b_gateT", [L, 128, 24])
    a_ln_g = din("a_ln_g", [L, 512])
    a_ln_b = din("a_ln_b", [L, 512])
    a_wsT = din("a_wsT", [L, 128, 8, 128])
    a_bsT = din("a_bsT", [L, 128, 8])
    b_convT = din("b_convT", [L, 128, 4, 3])
    c_lambda = din("c_lambda", [L, 256])
    c_subln_g = din("c_subln_g", [L, 128])
    w_a_out = din("w_a_out", [L, 512, D])
    w_b_out = din("w_b_out", [L, 512, D])
    w_c_out = din("w_c_out", [L, D, D])
    w_o = din("w_o", [L, D, D])
    ff_wg = din("ff_wg", [D, DFF])
    ff_wu = din("ff_wu", [D, DFF])
    ff_wd = din("ff_wd", [DFF, D])
    moe_wr = din("moe_wr", [128, 8, NE])
    moe_wg = din("moe_wg", [NE, D, DFE])
    moe_wu = din("moe_wu", [NE, D, DFE])
    moe_wd = din("moe_wd", [NE, DFE, D])
    cosT = din("cosT", [128, NT])
    sinT = din("sinT", [128, NT])
    ident_d = din("ident", [128, 128])
    out2 = nc.dram_tensor("out2", [2, NL, D], F32, kind="ExternalOutput").ap()

    NCH = WCOLS // 128
    win_c = dscr("win_c", [L, NCH, 128, D], BF16)
    win_rm = dscr("win_rm", [L, D, 1024], BF16)
    wa_c = dscr("wa_c", [L, 8, 128, 512], BF16)
    wb_c = dscr("wb_c", [L, 8, 128, 512], BF16)
    wc_c = dscr("wc_c", [L, 8, 128, D], BF16)
    wo_c = dscr("wo_c", [L, 8, 128, D], BF16)
    fg_c = dscr("fg_c", [NJ_FF, 128, D], BF16)
    fu_c = dscr("fu_c", [NJ_FF, 128, D], BF16)
    fd_c = dscr("fd_c", [8, 128, DFF], BF16)
    mg_c = dscr("mg_c", [NE, NJ_E, 128, D], BF16)
    mu_c = dscr("mu_c", [NE, NJ_E, 128, D], BF16)
    md_c = dscr("md_c", [NE, 8, 128, DFE], BF16)
    kind = "ExternalOutput" if debug_dump else "Internal"
    xT_d = nc.dram_tensor("xT_d", [2, D, NT], F32, kind=kind).ap()
    yaT_d = nc.dram_tensor("yaT_d", [2, 512, NT], BF16, kind=kind).ap()
    ybT_d = nc.dram_tensor("ybT_d", [2, 512, NT], BF16, kind=kind).ap()
    ycT_d = nc.dram_tensor("ycT_d", [2, D, NT], BF16, kind=kind).ap()
    hT_dbg = nc.dram_tensor("hT_dbg", [128, 8, NT], BF16, kind=kind).ap()

    with ExitStack() as es:
        P = Prog(nc, es)
        pe, act, dve, sp = P.pe, P.act, P.dve, P.sp

        def sb(name, shape, dt):
            return es.enter_context(sbt(name, list(shape), dt))

        psT = [es.enter_context(nc.psum_tensor("ps%d" % i, [128, 1024], F32)) for i in range(4)]
        bank = [P.tb("bank%d" % i, True) for i in range(8)]

        def psb(i, n=512, off=0):
            t = psT[i // 2]
            o = (i % 2) * 512 + off
            return t[:, o:o + n]

        ident = sb("ident", [128, 128], F32); ident_t = P.tb("ident", True)
        ones_ms = sb("ones_ms", [128, 128], BF16)
        epsT = sb("epsT", [128, 1], F32)
        modT = sb("modT", [128, L, 48, 3], F32)
        der = sb("der", [128, L, 3, 6, 8], F32)
        neglam = sb("neglam", [128, L], F32)
        gsub = sb("gsub", [128, L, 128], F32)
        bgT = sb("bgT", [128, L, 24], F32)
        convT = sb("convT", [128, L, 4, 3], F32)
        fgs = sb("fgs", [128, 8], F32)
        consts = P.tb("consts", True)

        P.dma(ident[:], ident_d[:, :], ident_t, True)
        P.op(dve, lambda e: e.memset(ones_ms[:], 1.0 / D), writes=[consts])
        P.op(dve, lambda e: e.memset(epsT[:], EPS), writes=[consts])
        cst2 = P.tb("cst2", True)
        for l in range(L):
            P.dma(bgT[:, l, :], b_gateT[l, :, :], cst2, True)
            P.dma(convT[:, l, :, :], b_convT[l, :, :, :], cst2, True)
        P.dma(fgs[:], fgT[:, :], cst2, True)

        def cast_copy(i, out, in_, reads, writes):
            E = (act, dve)[i % 2]
            if E is act:
                P.op(act, lambda e: e.copy(out, in_), reads=reads, writes=writes)
            else:
                P.op(dve, lambda e: e.tensor_copy(out, in_), reads=reads, writes=writes)

        def phase0():
            with ExitStack() as ps:
                CH = 8192
                st32 = [ps.enter_context(sbt("st32_%d" % i, [128, CH], F32)) for i in range(2)]
                st16 = [ps.enter_context(sbt("st16_%d" % i, [128, CH], BF16)) for i in range(2)]
                t32 = [P.tb("st32_%d" % i) for i in range(2)]
                t16 = [P.tb("st16_%d" % i) for i in range(2)]
                k = [0]

                def conv_chunked(src, dst, ch0=0):
                    K_, N_ = src.shape
                    nk = K_ // 128
                    g = max(1, min(N_ // 128, CH // (nk * 128)))
                    for c0 in range(0, N_, g * 128):
                        gg = min(g, (N_ - c0) // 128)
                        gw = gg * 128
                        i = k[0] % 2
                        k[0] += 1
                        n = nk * gw
                        P.dma(st32[i][:, 0:n].rearrange("p (k c) -> p k c", k=nk),
                              src[:, c0:c0 + gw].rearrange("(k p) c -> p k c", p=128), t32[i], True)
                        cast_copy(k[0], st16[i][:, 0:n].rearrange("p (g k c) -> p g k c", g=gg, k=nk),
                                  st32[i][:, 0:n].rearrange("p (k g c) -> p g k c", k=nk, g=gg), [t32[i]], [t16[i]])
                        c_ = ch0 + c0 // 128
                        P.dma(dst[c_:c_ + gg, :, :].rearrange("g p x -> p g x"),
                              st16[i][:, 0:n].rearrange("p (g x) -> p g x", g=gg), t16[i], False)

                for l in layers:
                    conv_chunked(w_in[l], win_c[l], 0)
                    conv_chunked(w_perm[l], win_c[l], IN_COLS // 128)
                    for r0 in range(0, D, 128):
                        i = k[0] % 2
                        k[0] += 1
                        P.dma(st32[i][:, 0:1024], w_in[l, r0:r0 + 128, 0:1024], t32[i], True)
                        cast_copy(k[0], st16[i][:, 0:1024], st32[i][:, 0:1024], [t32[i]], [t16[i]])
                        P.dma(win_rm[l, r0:r0 + 128, :], st16[i][:, 0:1024], t16[i], False)
                    conv_chunked(w_a_out[l], wa_c[l])
                    conv_chunked(w_b_out[l], wb_c[l])
                    conv_chunked(w_c_out[l], wc_c[l])
                    conv_chunked(w_o[l], wo_c[l])
                if 0 in layers:
                    conv_chunked(ff_wg, fg_c)
                    conv_chunked(ff_wu, fu_c)
                    conv_chunked(ff_wd, fd_c)
                if 1 in layers:
                    for e_ in range(NE):
                        conv_chunked(moe_wg[e_], mg_c[e_])
                        conv_chunked(moe_wu[e_], mu_c[e_])
                        conv_chunked(moe_wd[e_], md_c[e_])
                P.barrier()

        def phaseM():
            with ExitStack() as ps:
                cTs = ps.enter_context(sbt("cTs", [128, 8, 3], F32))
                scT = ps.enter_context(sbt("scT", [128, 8, 3], F32))
                wad = [ps.enter_context(sbt("wad%d" % i, [128, 8, 512], F32)) for i in range(2)]
                twad = [P.tb("wad%d" % i) for i in range(2)]
                badT = ps.enter_context(sbt("badT", [128, L, 48], F32))
                ngT = ps.enter_context(sbt("ngT", [128, 2, L, 8], F32))
                lamt = ps.enter_context(sbt("lamt", [128, 256], F32))
                lprod = ps.enter_context(sbt("lprod", [128, 2, 64], F32))
                lsum = ps.enter_context(sbt("lsum", [128, 2], F32))
                lexp = ps.enter_context(sbt("lexp", [128, 2], F32))
                gsr = ps.enter_context(sbt("gsr", [128, 128], F32))
                tmp8 = ps.enter_context(sbt("tmp8", [128, 8], F32))
                tM = P.tb("tM"); tS = P.tb("tS")
                P.dma(cTs[:], cT[:, :, :], tM, True)
                P.op(act, lambda e: e.activation(out=scT[:], in_=cTs[:], func=AF.Silu), reads=[tM], writes=[tS])
                tb_bad = P.tb("badT")
                for l in range(L):
                    P.dma(badT[:, l, :], b_adaT[l, :, :], tb_bad, True)
                    P.dma(ngT[:, 0, l, :], n1gT[l, :, :], tb_bad, True)
                    P.dma(ngT[:, 1, l, :], n2gT[l, :, :], tb_bad, True)
                tmod = P.tb("modT")
                for l in range(L):
                    pb = bank[l]
                    for grp in range(12):
                        i = grp % 2
                        P.dma(wad[i][:], w_ada[l, :, grp * 512:(grp + 1) * 512].rearrange("(kc p) c -> p kc c", p=128), twad[i], True)
                        for cc in range(4):
                            col = (grp * 4 + cc) * 3
                            for kc in range(8):
                                P.op(pe, lambda e, i=i, cc=cc, kc=kc, col=col, l=l: e.matmul(
                                    psb(l, 3, col), wad[i][:, kc, cc * 128:(cc + 1) * 128], scT[:, kc, :],
                                    start=(kc == 0), stop=(kc == 7)),
                                    reads=[twad[i], tS], writes=[pb], inc=(kc == 7))
                    P.op(dve, lambda e, l=l: e.tensor_tensor(
                        modT[:, l, :, :], psb(l, 144).rearrange("p (c j) -> p c j", j=3),
                        badT[:, l, :].unsqueeze(2).to_broadcast([128, 48, 3]), ALU.add),
                        reads=[pb, tb_bad], writes=[tmod])
                    for j in range(3):
                        for half in range(2):
                            base = half * 24
                            P.op(dve, lambda e, l=l, j=j, base=base: e.tensor_scalar(
                                tmp8[:], modT[:, l, base + 8:base + 16, j], 1.0, None, ALU.add),
                                reads=[tmod], writes=[tM])
                            P.op(dve, lambda e, l=l, j=j, half=half: e.tensor_tensor(
                                der[:, l, j, half * 3 + 0, :], tmp8[:], ngT[:, half, l, :], ALU.mult),
                                reads=[tM, tb_bad], writes=[consts])
                            P.op(dve, lambda e, l=l, j=j, half=half, base=base: e.tensor_copy(
                                der[:, l, j, half * 3 + 1, :], modT[:, l, base:base + 8, j]),
                                reads=[tmod], writes=[consts])
                            P.op(dve, lambda e, l=l, j=j, half=half, base=base: e.tensor_copy(
                                der[:, l, j, half * 3 + 2, :], modT[:, l, base + 16:base + 24, j]),
                                reads=[tmod], writes=[consts])
                    lam_init = 0.8 - 0.6 * math.exp(-0.3 * l)
                    tl = P.tb("lam%d" % l)
                    P.dma(lamt[:], c_lambda[l, :].partition_broadcast(128), tl, True)
                    P.op(dve, lambda e: e.tensor_tensor(lprod[:, 0, :], lamt[:, 0:64], lamt[:, 64:128], ALU.mult), reads=[tl], writes=[tM])
                    P.op(dve, lambda e: e.tensor_tensor(lprod[:, 1, :], lamt[:, 128:192], lamt[:, 192:256], ALU.mult), reads=[tl], writes=[tM])
                    P.op(dve, lambda e: e.tensor_reduce(lsum[:], lprod[:], AX.X, ALU.add), reads=[tM], writes=[tM])
                    P.op(act, lambda e: e.activation(out=lexp[:], in_=lsum[:], func=AF.Exp), reads=[tM], writes=[tS])
                    P.op(dve, lambda e, l=l, li=lam_init: e.scalar_tensor_tensor(
                        neglam[:, l:l + 1], lexp[:, 1:2], -li, lexp[:, 0:1], ALU.add, ALU.subtract),
                        reads=[tS], writes=[consts])
                    tg = P.tb("gsr%d" % l)
                    P.dma(gsr[:], c_subln_g[l, :].partition_broadcast(128), tg, True)
                    P.op(act, lambda e, l=l, li=lam_init: e.mul(gsub[:, l, :], gsr[:], 1.0 - li), reads=[tg], writes=[consts])
                P.barrier()

        def norm_mod(xblk, txb, n, l, j, slot0, dst_fn, tdst, sq, tsq, rstd, trs, tmpf, ttmp, pbank, post=None):
            nsub = (n + 511) // 512
            for sub in range(nsub):
                c0 = sub * 512
                m = min(512, n - c0)
                P.op(act, lambda e: e.activation(out=sq[:, :, 0:m], in_=xblk[:, :, c0:c0 + m], func=AF.Square),
                     reads=[txb], writes=[tsq])
                for kc in range(8):
                    P.op(pe, lambda e, kc=kc: e.matmul(psb(pbank, m), ones_ms[:], sq[:, kc, 0:m], start=(kc == 0), stop=(kc == 7)),
                         reads=[tsq, consts], writes=[bank[pbank]], inc=(kc == 7))
                P.op(act, lambda e: e.activation(out=rstd[:, c0:c0 + m], in_=psb(pbank, m), func=AF.Sqrt, bias=epsT[:, 0:1]),
                     reads=[bank[pbank], consts], writes=[trs])
                P.op(dve, lambda e: e.reciprocal(rstd[:, c0:c0 + m], rstd[:, c0:c0 + m]), reads=[trs], writes=[trs])
            for kc in range(8):
                i = kc % 2
                P.op(dve, lambda e, kc=kc, i=i: e.scalar_tensor_tensor(
                    tmpf[i][:, 0:n], xblk[:, kc, 0:n], der[:, l, j, slot0, kc:kc + 1], rstd[:, 0:n], ALU.mult, ALU.mult),
                    reads=[txb, trs, consts], writes=[ttmp[i]])
                if post is not None:
                    post(kc, i)
                else:
                    P.op(act, lambda e, kc=kc, i=i: e.activation(
                        out=dst_fn(kc), in_=tmpf[i][:, 0:n], func=AF.Identity, bias=der[:, l, j, slot0 + 1, kc:kc + 1]),
                        reads=[ttmp[i], consts], writes=[tdst])

        def wchunk(l, c0):
            return win_c[l, c0 // 128, :, :]

        def flat2(ap):
            return ap.rearrange("p a b -> p (a b)")

        def seq_layer(s, l):
            last = (l == L - 1)
            nblk_full = 9
            nblk_lat = 8 if last else 9
            with ExitStack() as sl:
                hT = sl.enter_context(sbt("hT", [128, 8, NT], BF16))
                thT = [P.tb("hT%d" % b, True) for b in range(9)]

                with ExitStack() as ps:
                    xblk = [ps.enter_context(sbt("xblkA%d" % i, [128, 8, 512], F32)) for i in range(2)]
                    txb = [P.tb("xblkA%d" % i) for i in range(2)]
                    sq = ps.enter_context(sbt("sqA", [128, 8, 512], BF16)); tsq = P.tb("sqA")
                    rstd = ps.enter_context(sbt("rstdA", [128, 512], F32)); trs = P.tb("rstdA")
                    tmpf = [ps.enter_context(sbt("tmpA%d" % i, [128, 512], F32)) for i in range(2)]
                    ttmp = [P.tb("tmpA%d" % i) for i in range(2)]
                    if l == 0:
                        xtok = [ps.enter_context(sbt("xtok%d" % i, [128, D], F32)) for i in range(2)]
                        txt = [P.tb("xtok%d" % i) for i in range(2)]
                    tcount = 0
                    for b, (c0, n) in enumerate(BLKS):
                        i = b % 2
                        j = s if b < 8 else 2
                        if l == 0:
                            for tt in range(n // 128):
                                k = tcount % 2
                                tcount += 1
                                src = x2[s, c0 + tt * 128:c0 + (tt + 1) * 128, :] if b < 8 else ctx2[s, tt * 128:(tt + 1) * 128, :]
                                P.dma(xtok[k][:], src, txt[k], True)
                                pbk = 2 * (tcount % 2)
                                pst = psT[pbk // 2]
                                for kc in range(8):
                                    P.op(pe, lambda e, kc=kc, k=k, pst=pst: e.transpose(
                                        pst[:, kc * 128:(kc + 1) * 128], xtok[k][:, kc * 128:(kc + 1) * 128], ident[:]),
                                        reads=[txt[k], ident_t], writes=[bank[pbk + kc // 4]], inc=(kc % 4 == 3))
                                P.op(act, lambda e, pst=pst, i=i, tt=tt: e.copy(
                                    xblk[i][:, :, tt * 128:(tt + 1) * 128], pst[:, :].rearrange("p (k t) -> p k t", k=8)),
                                    reads=[bank[pbk], bank[pbk + 1]], writes=[txb[i]])
                            P.dma(xT_d[s, :, c0:c0 + n].rearrange("(kc p) t -> p kc t", p=128), xblk[i][:, :, 0:n], txb[i], False)
                        else:
                            P.dma(xblk[i][:, :, 0:n], xT_d[s, :, c0:c0 + n].rearrange("(kc p) t -> p kc t", p=128), txb[i], True)
                        norm_mod(xblk[i], txb[i], n, l, j, 0, lambda kc, c0=c0, n=n: hT[:, kc, c0:c0 + n], thT[b],
                                 sq, tsq, rstd, trs, tmpf, ttmp, 4 + (b % 2))
                    P.barrier()
                    if debug_dump:
                        tdbg = P.tb("hTdbg")
                        P.dma(hT_dbg[:, :, :], hT[:, :, :], tdbg, False)
                        P.barrier()
                if stop == "A":
                    return True

                with ExitStack() as ps:
                    WA = ps.enter_context(sbt("WA", [128, 8, 1024], BF16)); tWA = P.tb("WA")
                    ws32 = ps.enter_context(sbt("ws32", [128, 8, 128], F32)); tws32 = P.tb("ws32")
                    wsT = ps.enter_context(sbt("wsT", [128, 8, 128], BF16)); twsT = P.tb("wsT")
                    lng = ps.enter_context(sbt("lng", [128, 512], F32))
                    lnb = ps.enter_context(sbt("lnb", [128, 512], F32))
                    bsT = ps.enter_context(sbt("bsT", [128, 8], F32)); tln = P.tb("ln")
                    u_sb = [ps.enter_context(sbt("u_sb%d" % i, [128, 512], F32)) for i in range(2)]
                    v_sb = [ps.enter_context(sbt("v_sb%d" % i, [128, 512], F32)) for i in range(2)]
                    junk = ps.enter_context(sbt("junkB1", [128, 512], F32))
                    vc = [ps.enter_context(sbt("vc%d" % i, [128, 512], BF16)) for i in range(2)]
                    st = [ps.enter_context(sbt("stB1_%d" % i, [128, 8], F32)) for i in range(2)]
                    yaB = [ps.enter_context(sbt("yaB%d" % i, [128, 4, 512], BF16)) for i in range(2)]
                    tu = [P.tb("u%d" % i) for i in range(2)]; tv = [P.tb("v%d" % i) for i in range(2)]
                    tvc = [P.tb("vc%d" % i) for i in range(2)]; tst = [P.tb("st%d" % i) for i in range(2)]
                    tya = [P.tb("yaB%d" % i) for i in range(2)]; tjunk = P.tb("junkB1")
                    P.dma(WA[:, :, 0:512], win_rm[l, :, 0:512].rearrange("(kc p) c -> p kc c", p=128), tWA, True)
                    P.dma(WA[:, :, 512:1024], win_rm[l, :, 512:1024].rearrange("(kc p) c -> p kc c", p=128), tWA, True)
                    P.dma(ws32[:], a_wsT[l, :, :, :], tws32, True)
                    P.op(act, lambda e: e.copy(wsT[:], ws32[:]), reads=[tws32], writes=[twsT])
                    P.dma(lng[:], a_ln_g[l, :].partition_broadcast(128), tln, True)
                    P.dma(lnb[:], a_ln_b[l, :].partition_broadcast(128), tln, True)
                    P.dma(bsT[:], a_bsT[l, :, :], tln, True)
                    ntile = nblk_lat * 4 if nblk_lat == 8 else 34
                    for tt in range(ntile):
                        i = tt % 2
                        b = min(tt // 4, 8)
                        bu, bv, bm, bt = (0, 1, 2, 3) if i == 0 else (4, 5, 6, 7)
                        tok = slice(tt * 128, (tt + 1) * 128)
                        for (bk, cs) in ((bu, 0), (bv, 512)):
                            for kc in range(8):
                                P.op(pe, lambda e, kc=kc, bk=bk, cs=cs: e.matmul(
                                    psb(bk), hT[:, kc, tok], WA[:, kc, cs:cs + 512], start=(kc == 0), stop=(kc == 7)),
                                    reads=[thT[b], tWA], writes=[bank[bk]], inc=(kc == 7))
                        P.op(dve, lambda e: e.memset(st[i][:, 0:2], 0.0), writes=[tst[i]])
                        P.op(act, lambda e: e.activation(out=u_sb[i][:], in_=psb(bu), func=AF.Gelu_apprx_tanh),
                             reads=[bank[bu]], writes=[tu[i]])
                        P.op(act, lambda e: e.activation(out=v_sb[i][:], in_=psb(bv), func=AF.Gelu_apprx_tanh, accum_out=st[i][:, 0:1]),
                             reads=[bank[bv]], writes=[tv[i], tst[i]])
                        P.op(act, lambda e: e.activation(out=junk[:], in_=v_sb[i][:], func=AF.Square, accum_out=st[i][:, 1:2]),
                             reads=[tv[i]], writes=[tjunk, tst[i]])
                        P.op(dve, lambda e: e.tensor_scalar(st[i][:, 2:4], st[i][:, 0:2], 1.0 / 512, None, ALU.mult), reads=[tst[i]], writes=[tst[i]])
                        P.op(dve, lambda e: e.tensor_tensor(st[i][:, 4:5], st[i][:, 2:3], st[i][:, 2:3], ALU.mult), reads=[tst[i]], writes=[tst[i]])
                        P.op(dve, lambda e: e.tensor_tensor(st[i][:, 5:6], st[i][:, 3:4], st[i][:, 4:5], ALU.subtract), reads=[tst[i]], writes=[tst[i]])
                        P.op(act, lambda e: e.activation(out=st[i][:, 6:7], in_=st[i][:, 5:6], func=AF.Sqrt, bias=epsT[:, 0:1]), reads=[tst[i], consts], writes=[tst[i]])
                        P.op(dve, lambda e: e.reciprocal(st[i][:, 7:8], st[i][:, 6:7]), reads=[tst[i]], writes=[tst[i]])
                        P.op(dve, lambda e: e.tensor_scalar(v_sb[i][:], v_sb[i][:], st[i][:, 2:3], st[i][:, 7:8], ALU.subtract, ALU.mult),
                             reads=[tst[i], tv[i]], writes=[tv[i]])
                        P.op(dve, lambda e: e.tensor_tensor(v_sb[i][:], v_sb[i][:], lng[:], ALU.mult), reads=[tv[i], tln], writes=[tv[i]])
                        P.op(dve, lambda e: e.tensor_tensor(vc[i][:], v_sb[i][:], lnb[:], ALU.add), reads=[tv[i], tln], writes=[tvc[i]])
                        for g in range(8):
                            P.op(pe, lambda e, g=g: e.matmul(psb(bm, 64, g * 64), wsT[:, g, :], vc[i][:, g * 64:(g + 1) * 64], start=True, stop=True),
                                 reads=[twsT, tvc[i]], writes=[bank[bm]], inc=(g == 7))
                        P.op(dve, lambda e: e.tensor_tensor(
                            v_sb[i][:].rearrange("p (g e) -> p g e", g=8), psb(bm).rearrange("p (g e) -> p g e", g=8),
                            bsT[:, :].unsqueeze(2).to_broadcast([128, 8, 64]), ALU.add),
                            reads=[bank[bm], tln], writes=[tv[i]])
                        P.op(dve, lambda e: e.tensor_tensor(u_sb[i][:], u_sb[i][:], v_sb[i][:], ALU.mult), reads=[tv[i], tu[i]], writes=[tu[i]])
                        for c in range(4):
                            P.op(pe, lambda e, c=c: e.transpose(psb(bt, 128, c * 128), u_sb[i][:, c * 128:(c + 1) * 128], ident[:]),
                                 reads=[tu[i], ident_t], writes=[bank[bt]], inc=(c == 3))
                        yb_i = (tt // 4) % 2
                        q = tt % 4
                        P.op(act, lambda e, yb_i=yb_i, q=q: e.copy(
                            yaB[yb_i][:, :, q * 128:(q + 1) * 128], psb(bt).rearrange("p (c t) -> p c t", c=4)),
                            reads=[bank[bt]], writes=[tya[yb_i]])
                        c0, n = BLKS[b]
                        if (tt + 1) * 128 == c0 + n:
                            P.dma(yaT_d[s, :, c0:c0 + n].rearrange("(c p) t -> p c t", p=128), yaB[yb_i][:, :, 0:n], tya[yb_i], False)
                    P.barrier()
                if stop == "B1":
                    return True

                with ExitStack() as ps:
                    WB = [ps.enter_context(sbt("WB%d" % i, [128, 3, 8, 128], BF16)) for i in range(2)]
                    tWB = [P.tb("WB%d" % i) for i in range(2)]
                    ZW = NT + 4
                    zf = ps.enter_context(sbt("zf", [128, ZW], F32)); tzf = P.tb("zf")
                    bgf = ps.enter_context(sbt("bgf", [128, NT], F32)); tbg = P.tb("bgf")
                    p4 = [ps.enter_context(sbt("p4_%d" % i, [128, 512], F32)) for i in range(2)]
                    tp4 = [P.tb("p4_%d" % i) for i in range(2)]
                    cv = [ps.enter_context(sbt("cv%d" % i, [128, 512], F32)) for i in range(2)]
                    tcv = [P.tb("cv%d" % i) for i in range(2)]
                    ybo = [ps.enter_context(sbt("ybo%d" % i, [128, 512], BF16)) for i in range(2)]
                    tybo = [P.tb("ybo%d" % i) for i in range(2)]
                    P.op(dve, lambda e: e.memset(zf[:], 0.0), writes=[tzf])

                    def zoff(b):
                        return 1 + BLKS[b][0] if b < 8 else NL + 2

                    def loadWB(fc):
                        i = fc % 2
                        for k3, off in enumerate((OFF_BB, OFF_BC, OFF_BH)):
                            P.dma(flat2(WB[i][:, k3, :, :]), wchunk(l, off + fc * 128), tWB[i], True)
                    loadWB(0)
                    cnt = 0
                    for fc in range(4):
                        i = fc % 2
                        if fc + 1 < 4:
                            loadWB(fc + 1)
                        for b in range(nblk_lat):
                            c0, n = BLKS[b]
                            pbs = (0, 1, 2) if cnt % 2 == 0 else (3, 4, 5)
                            k2 = cnt % 2
                            cnt += 1
                            for k3 in range(3):
                                for kc in range(8):
                                    P.op(pe, lambda e, k3=k3, kc=kc: e.matmul(
                                        psb(pbs[k3], n), WB[i][:, k3, kc, :], hT[:, kc, c0:c0 + n], start=(kc == 0), stop=(kc == 7)),
                                        reads=[tWB[i], thT[b]], writes=[bank[pbs[k3]]], inc=(kc == 7))
                            P.op(act, lambda e: e.copy(p4[k2][:, 0:n], psb(pbs[2], n)), reads=[bank[pbs[2]]], writes=[tp4[k2]])
                            P.op(dve, lambda e: e.tensor_tensor(zf[:, zoff(b):zoff(b) + n], psb(pbs[1], n), p4[k2][:, 0:n], ALU.mult),
                                 reads=[bank[pbs[1]], tp4[k2]], writes=[tzf])
                            P.op(act, lambda e: e.copy(bgf[:, c0:c0 + n], psb(pbs[0], n)), reads=[bank[pbs[0]]], writes=[tbg])
                        for b in range(nblk_lat):
                            c0, n = BLKS[b]
                            k2 = b % 2
                            z0 = zoff(b)
                            P.op(dve, lambda e: e.tensor_scalar(cv[k2][:, 0:n], zf[:, z0 - 1:z0 - 1 + n], convT[:, l, fc, 0:1], None, ALU.mult),
                                 reads=[tzf, cst2], writes=[tcv[k2]])
                            P.op(dve, lambda e: e.scalar_tensor_tensor(cv[k2][:, 0:n], zf[:, z0:z0 + n], convT[:, l, fc, 1:2], cv[k2][:, 0:n], ALU.mult, ALU.add),
                                 reads=[tzf, cst2, tcv[k2]], writes=[tcv[k2]])
                            P.op(dve, lambda e: e.scalar_tensor_tensor(cv[k2][:, 0:n], zf[:, z0 + 1:z0 + 1 + n], convT[:, l, fc, 2:3], cv[k2][:, 0:n], ALU.mult, ALU.add),
                                 reads=[tzf, cst2, tcv[k2]], writes=[tcv[k2]])
                            P.op(dve, lambda e: e.tensor_tensor(ybo[k2][:, 0:n], cv[k2][:, 0:n], bgf[:, c0:c0 + n], ALU.mult),
                                 reads=[tcv[k2], tbg], writes=[tybo[k2]])
                            P.dma(ybT_d[s, fc * 128:(fc + 1) * 128, c0:c0 + n], ybo[k2][:, 0:n], tybo[k2], False)
                    P.barrier()
                if stop == "B2":
                    return True

                with ExitStack() as ps:
                    cosS = ps.enter_context(sbt("cosS", [128, NT], F32))
                    sinS = ps.enter_context(sbt("sinS", [128, NT], F32)); ttab = P.tb("tab")
                    P.dma(cosS[:], cosT[:, :], ttab, True)
                    P.dma(sinS[:], sinT[:, :], ttab, True)
                    W5 = [ps.enter_context(sbt("W5_%d" % i, [128, 5, 8, 128], BF16)) for i in range(2)]
                    tW5 = [P.tb("W5_%d" % i) for i in range(2)]
                    QT = ps.enter_context(sbt("QT", [128, NT], BF16)); tQT = P.tb("QT")
                    KT = ps.enter_context(sbt("KT", [128, NT], BF16)); tKT = P.tb("KT")
                    VH = ps.enter_context(sbt("VH", [128, 34, 130], BF16)); tVH = P.tb("VH")
                    ycH = ps.enter_context(sbt("ycH", [128, NT], BF16)); tycH = P.tb("ycH")
                    r1 = [ps.enter_context(sbt("r1_%d" % i, [128, 512], F32)) for i in range(2)]
                    r2 = [ps.enter_context(sbt("r2_%d" % i, [128, 512], F32)) for i in range(2)]
                    tr1 = [P.tb("r1_%d" % i) for i in range(2)]; tr2 = [P.tb("r2_%d" % i) for i in range(2)]
                    PT = [ps.enter_context(sbt("PT%d" % i, [128, 1024], BF16)) for i in range(3)]
                    tPT = [P.tb("PT%d" % i) for i in range(3)]
                    o_sb = [ps.enter_context(sbt("o_sb%d" % i, [128, 128], F32)) for i in range(2)]
                    to = [P.tb("o_sb%d" % i) for i in range(2)]
                    sm = [ps.enter_context(sbt("sm%d" % i, [128, 8], F32)) for i in range(2)]
                    tsm = [P.tb("sm%d" % i) for i in range(2)]
                    junk = ps.enter_context(sbt("junkB3", [128, 128], F32)); tjunk = P.tb("junkB3")
                    osb = [ps.enter_context(sbt("osb%d" % i, [128, 3, 512], F32)) for i in range(2)]
                    tosb = [P.tb("osb%d" % i) for i in range(2)]
                    P.op(dve, lambda e: e.memset(VH[:, :, 128:130], 1.0), writes=[tVH])
                    P.op(dve, lambda e: e.memset(ycH[:], 0.0), writes=[tycH])

                    def loadW5(h):
                        i = h % 2
                        offs = (OFF_CQ + h * 128, OFF_QP + h * 128, OFF_CK + h * 128, OFF_KP + h * 128, OFF_CV + h * 128)
                        for k5, off in enumerate(offs):
                            P.dma(flat2(W5[i][:, k5, :, :]), wchunk(l, off), tW5[i], True)

                    def oacc(a, ncol=129):
                        return psb(4 + a // 3, ncol, (a % 3) * 160), bank[4 + a // 3]

                    loadW5(0)
                    pcount = [0]
                    for h in range(8):
                        wi = h % 2
                        if h + 1 < 8:
                            loadW5(h + 1)
                        for b in range(nblk_full):
                            c0, n = BLKS[b]
                            need_q = not (last and b == 8)
                            for (kk, dstT, tdst) in ((0, QT, tQT), (2, KT, tKT)):
                                if kk == 0 and not need_q:
                                    continue
                                i2 = pcount[0] % 2
                                pcount[0] += 1
                                b0, b1 = (0, 1) if i2 == 0 else (2, 3)
                                for (bk, k5) in ((b0, kk), (b1, kk + 1)):
                                    for kc in range(8):
                                        P.op(pe, lambda e, kc=kc, bk=bk, k5=k5: e.matmul(
                                            psb(bk, n), W5[wi][:, k5, kc, :], hT[:, kc, c0:c0 + n], start=(kc == 0), stop=(kc == 7)),
                                            reads=[tW5[wi], thT[b]], writes=[bank[bk]], inc=(kc == 7))
                                P.op(dve, lambda e: e.tensor_tensor(r1[i2][:, 0:n], psb(b0, n), cosS[:, c0:c0 + n], ALU.mult),
                                     reads=[bank[b0], ttab], writes=[tr1[i2]])
                                P.op(dve, lambda e: e.tensor_tensor(r2[i2][:, 0:n], psb(b1, n), sinS[:, c0:c0 + n], ALU.mult),
                                     reads=[bank[b1], ttab], writes=[tr2[i2]])
                                P.op(dve, lambda e, dstT=dstT: e.tensor_tensor(dstT[:, c0:c0 + n], r1[i2][:, 0:n], r2[i2][:, 0:n], ALU.add),
                                     reads=[tr1[i2], tr2[i2]], writes=[tdst])
                            nt_ = n // 128
                            for q in range(nt_):
                                tt = c0 // 128 + q
                                for kc in range(8):
                                    P.op(pe, lambda e, kc=kc, q=q, tt=tt: e.matmul(
                                        psb(7, 128, q * 128), hT[:, kc, tt * 128:(tt + 1) * 128], W5[wi][:, 4, kc, :], start=(kc == 0), stop=(kc == 7)),
                                        reads=[tW5[wi], thT[b]], writes=[bank[7]], inc=(kc == 7))
                            P.op(act, lambda e: e.copy(VH[:, c0 // 128:c0 // 128 + nt_, 0:128], psb(7, nt_ * 128).rearrange("p (q d) -> p q d", q=nt_)),
                                 reads=[bank[7]], writes=[tVH])
                        qblocks = [(qb * 512, 512, list(range(34))) for qb in range(8)]
                        if not last:
                            qblocks.append((NL, NCX, [32, 33]))
                        steps = []
                        for bi_, (q0, nq, kts) in enumerate(qblocks):
                            for ki, kt in enumerate(kts):
                                steps.append((bi_, q0, nq, kt, ki, len(kts)))
                        nst = len(steps)

                        def QK(i):
                            bi_, q0, nq, kt, ki, nk = steps[i]
                            sbuf_i = i % 2
                            S = psT[sbuf_i]
                            for m in range(2):
                                P.op(pe, lambda e, m=m: e.matmul(
                                    S[:, m * 512:m * 512 + nq], KT[m * 64:(m + 1) * 64, kt * 128:(kt + 1) * 128],
                                    QT[m * 64:(m + 1) * 64, q0:q0 + nq], start=True, stop=True),
                                    reads=[tKT, tQT], writes=[bank[2 * sbuf_i + m]], inc=(m == 1))

                        def EXP(i):
                            bi_, q0, nq, kt, ki, nk = steps[i]
                            sbuf_i = i % 2
                            pt_i = i % 3
                            S = psT[sbuf_i]
                            sb0 = 2 * sbuf_i
                            if nq == 512:
                                P.op(act, lambda e: e.activation(out=PT[pt_i][:], in_=S[:, :], func=AF.Exp, scale=0.125),
                                     reads=[bank[sb0], bank[sb0 + 1]], writes=[tPT[pt_i]])
                            else:
                                P.op(act, lambda e: e.activation(
                                    out=PT[pt_i][:, :].rearrange("p (m q) -> p m q", m=2)[:, :, 0:nq],
                                    in_=S[:, :].rearrange("p (m q) -> p m q", m=2)[:, :, 0:nq], func=AF.Exp, scale=0.125),
                                    reads=[bank[sb0], bank[sb0 + 1]], writes=[tPT[pt_i]])

                        def AV(i):
                            bi_, q0, nq, kt, ki, nk = steps[i]
                            pt_i = i % 3
                            nqi = nq // 128
                            for qi in range(nqi):
                                for m in range(2):
                                    a = qi * 2 + m
                                    ov, ob = oacc(a)
                                    P.op(pe, lambda e, m=m, qi=qi, ov=ov, a=a: e.matmul(
                                        ov, PT[pt_i][:, m * 512 + qi * 128:m * 512 + (qi + 1) * 128], VH[:, kt, 0:129],
                                        start=(ki == 0 and a % 3 == 0), stop=(ki == nk - 1)),
                                        reads=[tPT[pt_i], tVH], writes=[ob], inc=(qi == nqi - 1 and m == 1))

                        def EPI_evac(bi_):
                            q0, nq, kts = qblocks[bi_]
                            k3 = bi_ % 2
                            nb_ = 3 if nq == 512 else 2
                            for j3 in range(nb_):
                                P.op(dve, lambda e, j3=j3: e.tensor_copy(osb[k3][:, j3, 0:449], psb(4 + j3, 449)),
                                     reads=[bank[4 + j3]], writes=[tosb[k3]])

                        def EPI_rest(bi_):
                            q0, nq, kts = qblocks[bi_]
                            k3 = bi_ % 2
                            nqi = nq // 128
                            for qi in range(nqi):
                                k2 = qi % 2
                                a0, a1 = qi * 2, qi * 2 + 1
                                O0 = osb[k3][:, a0 // 3, (a0 % 3) * 160:(a0 % 3) * 160 + 129]
                                O1 = osb[k3][:, a1 // 3, (a1 % 3) * 160:(a1 % 3) * 160 + 129]
                                rd = [tosb[k3]]
                                P.op(dve, lambda e: e.reciprocal(sm[k2][:, 0:1], O0[:, 128:129]), reads=rd, writes=[tsm[k2]])
                                P.op(dve, lambda e: e.reciprocal(sm[k2][:, 1:2], O1[:, 128:129]), reads=rd, writes=[tsm[k2]])
                                P.op(dve, lambda e: e.tensor_tensor(sm[k2][:, 2:3], sm[k2][:, 1:2], neglam[:, l:l + 1], ALU.mult),
                                     reads=[tsm[k2], consts], writes=[tsm[k2]])
                                P.op(dve, lambda e: e.tensor_scalar(o_sb[k2][:], O0[:, 0:128], sm[k2][:, 0:1], None, ALU.mult),
                                     reads=rd + [tsm[k2]], writes=[to[k2]])
                                P.op(dve, lambda e: e.scalar_tensor_tensor(o_sb[k2][:], O1[:, 0:128], sm[k2][:, 2:3], o_sb[k2][:], ALU.mult, ALU.add),
                                     reads=rd + [tsm[k2], to[k2]], writes=[to[k2]])
                                P.op(dve, lambda e: e.memset(sm[k2][:, 3:4], 0.0), writes=[tsm[k2]])
                                P.op(dve, lambda e: e.tensor_tensor_scan if False else e.tensor_tensor(junk[:], o_sb[k2][:], o_sb[k2][:], ALU.mult),
                                     reads=[to[k2]], writes=[tjunk])
                                P.op(dve, lambda e: e.tensor_reduce(sm[k2][:, 3:4], junk[:], AX.X, ALU.add), reads=[tjunk], writes=[tsm[k2]])
                                P.op(act, lambda e: e.activation(out=sm[k2][:, 4:5], in_=sm[k2][:, 3:4], func=AF.Sqrt, bias=epsT[:, 0:1], scale=1.0 / 128),
                                     reads=[tsm[k2], consts], writes=[tsm[k2]])
                                P.op(dve, lambda e: e.reciprocal(sm[k2][:, 5:6], sm[k2][:, 4:5]), reads=[tsm[k2]], writes=[tsm[k2]])
                                P.op(dve, lambda e: e.scalar_tensor_tensor(o_sb[k2][:], o_sb[k2][:], sm[k2][:, 5:6], gsub[:, l, :], ALU.mult, ALU.mult),
                                     reads=[tsm[k2], to[k2], consts], writes=[to[k2]])
                                P.op(pe, lambda e, qi=qi: e.transpose(psb(7, 128, qi * 128), o_sb[k2][:], ident[:]),
                                     reads=[to[k2], ident_t], writes=[bank[7]])
                            P.op(dve, lambda e: e.tensor_copy(ycH[:, q0:q0 + nq], psb(7, nq)), reads=[bank[7]], writes=[tycH])

                        pending = []
                        QK(0)
                        for i in range(nst):
                            if i + 1 < nst:
                                QK(i + 1)
                            EXP(i)
                            AV(i)
                            bi_, q0, nq, kt, ki, nk = steps[i]
                            if ki == nk - 1:
                                EPI_evac(bi_)
                                pending.append((i + 6, bi_))
                            while pending and (pending[0][0] <= i or i == nst - 1):
                                EPI_rest(pending.pop(0)[1])
                        ncols = NT if not last else NL
                        P.dma(ycT_d[s, h * 128:(h + 1) * 128, 0:ncols], ycH[:, 0:ncols], tycH, False)
                    P.barrier()
                if stop == "B3":
                    return True

                with ExitStack() as ps:
                    WO = ps.enter_context(sbt("WO", [128, 8, D], BF16)); tWO = P.tb("WO")
                    P.dma(WO[:, :, :], wo_c[l, :, :, :].rearrange("g p x -> p g x"), tWO, True)
                    WC = [ps.enter_context(sbt("WC%d" % i, [128, 40, 128], BF16)) for i in range(2)]
                    tWC = [P.tb("WC%d" % i) for i in range(2)]
                    yin = [ps.enter_context(sbt("yin%d" % i, [128, 16, 512], BF16)) for i in range(2)]
                    tyin = [P.tb("yin%d" % i) for i in range(2)]
                    xb = [ps.enter_context(sbt("xbC%d" % i, [128, 8, 512], F32)) for i in range(2)]
                    txb = [P.tb("xbC%d" % i) for i in range(2)]
                    gs = [ps.enter_context(sbt("gs%d" % i, [128, 3, 512], F32)) for i in range(2)]
                    tgs = [P.tb("gs%d" % i) for i in range(2)]
                    t1 = [ps.enter_context(sbt("t1_%d" % i, [128, 512], F32)) for i in range(2)]
                    t2 = [ps.enter_context(sbt("t2_%d" % i, [128, 512], F32)) for i in range(2)]
                    tt1 = [P.tb("t1_%d" % i) for i in range(2)]; tt2 = [P.tb("t2_%d" % i) for i in range(2)]
                    yT = ps.enter_context(sbt("yT", [128, 8, 512], BF16)); tyT = P.tb("yT")

                    def loadWC(idx):
                        c = idx % 8
                        i = idx % 2
                        for br in range(3):
                            P.dma(flat2(WC[i][:, br * 8:(br + 1) * 8, :]), wchunk(l, OFF_GATE + br * D + c * 128), tWC[i], True)
                        P.dma(flat2(WC[i][:, 24:28, :]), wa_c[l, c, :, :], tWC[i], True)
                        P.dma(flat2(WC[i][:, 28:32, :]), wb_c[l, c, :, :], tWC[i], True)
                        P.dma(flat2(WC[i][:, 32:40, :]), wc_c[l, c, :, :], tWC[i], True)

                    def loadblk(b):
                        c0, n = BLKS[b]
                        i = b % 2
                        P.dma(yin[i][:, 0:4, 0:n], yaT_d[s, :, c0:c0 + n].rearrange("(c p) t -> p c t", p=128), tyin[i], True)
                        P.dma(yin[i][:, 4:8, 0:n], ybT_d[s, :, c0:c0 + n].rearrange("(c p) t -> p c t", p=128), tyin[i], True)
                        P.dma(yin[i][:, 8:16, 0:n], ycT_d[s, :, c0:c0 + n].rearrange("(c p) t -> p c t", p=128), tyin[i], True)
                        P.dma(xb[i][:, :, 0:n], xT_d[s, :, c0:c0 + n].rearrange("(kc p) t -> p kc t", p=128), txb[i], True)

                    loadblk(0)
                    loadWC(0)
                    idx = 0
                    nb = nblk_lat
                    for b in range(nb):
                        c0, n = BLKS[b]
                        bi = b % 2
                        j = s if b < 8 else 2
                        if b + 1 < nb:
                            loadblk(b + 1)
                        for c in range(8):
                            wi = idx % 2
                            k2 = idx % 2
                            if idx + 1 < nb * 8:
                                loadWC(idx + 1)
                            idx += 1
                            for br in range(3):
                                for kc in range(8):
                                    P.op(pe, lambda e, br=br, kc=kc: e.matmul(
                                        psb(br, n), WC[wi][:, br * 8 + kc, :], hT[:, kc, c0:c0 + n], start=(kc == 0), stop=(kc == 7)),
                                        reads=[tWC[wi], thT[b]], writes=[bank[br]], inc=(kc == 7))
                            for (br, k0, nk, y0) in ((0, 24, 4, 0), (1, 28, 4, 4), (2, 32, 8, 8)):
                                for kc in range(nk):
                                    P.op(pe, lambda e, br=br, kc=kc, k0=k0, y0=y0: e.matmul(
                                        psb(3 + br, n), WC[wi][:, k0 + kc, :], yin[bi][:, y0 + kc, 0:n], start=(kc == 0), stop=(kc == nk - 1)),
                                        reads=[tWC[wi], tyin[bi]], writes=[bank[3 + br]], inc=(kc == nk - 1))
                            for br in range(3):
                                P.op(act, lambda e, br=br: e.activation(out=gs[k2][:, br, 0:n], in_=psb(br, n), func=AF.Sigmoid,
                                                                        bias=bgT[:, l, br * 8 + c:br * 8 + c + 1]),
                                     reads=[bank[br], cst2], writes=[tgs[k2]])
                            P.op(dve, lambda e: e.tensor_tensor(t1[k2][:, 0:n], psb(3, n), gs[k2][:, 0, 0:n], ALU.mult), reads=[bank[3], tgs[k2]], writes=[tt1[k2]])
                            P.op(dve, lambda e: e.tensor_tensor(t2[k2][:, 0:n], psb(4, n), gs[k2][:, 1, 0:n], ALU.mult), reads=[bank[4], tgs[k2]], writes=[tt2[k2]])
                            P.op(dve, lambda e: e.tensor_tensor(t1[k2][:, 0:n], t1[k2][:, 0:n], t2[k2][:, 0:n], ALU.add), reads=[tt1[k2], tt2[k2]], writes=[tt1[k2]])
                            P.op(dve, lambda e: e.tensor_tensor(t2[k2][:, 0:n], psb(5, n), gs[k2][:, 2, 0:n], ALU.mult), reads=[bank[5], tgs[k2]], writes=[tt2[k2]])
                            P.op(dve, lambda e, c=c: e.tensor_tensor(yT[:, c, 0:n], t1[k2][:, 0:n], t2[k2][:, 0:n], ALU.add), reads=[tt1[k2], tt2[k2]], writes=[tyT])
                        for c in range(8):
                            pb_ = 6 + (c % 2)
                            for kc in range(8):
                                P.op(pe, lambda e, c=c, kc=kc, pb_=pb_: e.matmul(
                                    psb(pb_, n), WO[:, c, kc * 128:(kc + 1) * 128], yT[:, kc, 0:n], start=(kc == 0), stop=(kc == 7)),
                                    reads=[tWO, tyT], writes=[bank[pb_]], inc=(kc == 7))
                            P.op(dve, lambda e, c=c, pb_=pb_: e.scalar_tensor_tensor(
                                xb[bi][:, c, 0:n], psb(pb_, n), der[:, l, j, 2, c:c + 1], xb[bi][:, c, 0:n], ALU.mult, ALU.add),
                                reads=[bank[pb_], consts, txb[bi]], writes=[txb[bi]])
                        P.dma(xT_d[s, :, c0:c0 + n].rearrange("(kc p) t -> p kc t", p=128), xb[bi][:, :, 0:n], txb[bi], False)
                    P.barrier()
                if stop == "C1":
                    return True
            if l == 0:
                phaseC2_dense(s, l)
            else:
                phaseC2_moe(s, l)

        def phaseC2_dense(s, l):
            with ExitStack() as ps:
                xb = [ps.enter_context(sbt("xbD%d" % i, [128, 8, 512], F32)) for i in range(2)]
                txb = [P.tb("xbD%d" % i) for i in range(2)]
                sq = ps.enter_context(sbt("sqD", [128, 8, 512], BF16)); tsq = P.tb("sqD")
                rstd = ps.enter_context(sbt("rstdD", [128, 512], F32)); trs = P.tb("rstdD")
                tmpf = [ps.enter_context(sbt("tmpD%d" % i, [128, 512], F32)) for i in range(2)]
                ttmp = [P.tb("tmpD%d" % i) for i in range(2)]
                h2 = ps.enter_context(sbt("h2D", [128, 8, 512], BF16)); th2 = P.tb("h2D")
                hid = ps.enter_context(sbt("hidD", [128, NJ_FF, 512], BF16)); thid = P.tb("hidD")
                W1 = [ps.enter_context(sbt("W1D%d" % i, [128, 2, 8, 128], BF16)) for i in range(3)]
                tW1 = [P.tb("W1D%d" % i) for i in range(3)]
                W2 = [ps.enter_context(sbt("W2D%d" % i, [128, NJ_FF, 128], BF16)) for i in range(2)]
                tW2 = [P.tb("W2D%d" % i) for i in range(2)]
                ssb = [ps.enter_context(sbt("ssbD%d" % i, [128, 512], F32)) for i in range(2)]
                tss = [P.tb("ssbD%d" % i) for i in range(2)]

                def loadW1(idx):
                    jc = idx % NJ_FF
                    i = idx % 3
                    P.dma(flat2(W1[i][:, 0, :, :]), fg_c[jc, :, :], tW1[i], True)
                    P.dma(flat2(W1[i][:, 1, :, :]), fu_c[jc, :, :], tW1[i], True)

                def loadW2(idx):
                    c = idx % 8
                    i = idx % 2
                    P.dma(flat2(W2[i][:, :, :]), fd_c[c, :, :], tW2[i], True)

                def loadx(b):
                    c0, n = BLKS[b]
                    P.dma(xb[b % 2][:, :, 0:n], xT_d[s, :, c0:c0 + n].rearrange("(kc p) t -> p kc t", p=128), txb[b % 2], True)

                nb = 9
                loadx(0)
                loadW1(0); loadW1(1)
                loadW2(0)
                i1 = 0
                i2 = 0
                for b in range(nb):
                    c0, n = BLKS[b]
                    bi = b % 2
                    j = s if b < 8 else 2
                    if b + 1 < nb:
                        loadx(b + 1)
                    norm_mod(xb[bi], txb[bi], n, l, j, 3, lambda kc, n=n: h2[:, kc, 0:n], th2, sq, tsq, rstd, trs, tmpf, ttmp, 6)
                    for jc in range(NJ_FF):
                        wi = i1 % 3
                        k2 = i1 % 2
                        if i1 + 2 < nb * NJ_FF:
                            loadW1(i1 + 2)
                        i1 += 1
                        bg_, bu_ = (0, 1) if k2 == 0 else (2, 3)
                        for (bk, gi) in ((bg_, 0), (bu_, 1)):
                            for kc in range(8):
                                P.op(pe, lambda e, bk=bk, gi=gi, kc=kc: e.matmul(
                                    psb(bk, n), W1[wi][:, gi, kc, :], h2[:, kc, 0:n], start=(kc == 0), stop=(kc == 7)),
                                    reads=[tW1[wi], th2], writes=[bank[bk]], inc=(kc == 7))
                        P.op(act, lambda e: e.activation(out=ssb[k2][:, 0:n], in_=psb(bg_, n), func=AF.Silu), reads=[bank[bg_]], writes=[tss[k2]])
                        P.op(dve, lambda e, jc=jc: e.tensor_tensor(hid[:, jc, 0:n], psb(bu_, n), ssb[k2][:, 0:n], ALU.mult),
                             reads=[bank[bu_], tss[k2]], writes=[thid])
                    for c in range(8):
                        wi = i2 % 2
                        if i2 + 1 < nb * 8:
                            loadW2(i2 + 1)
                        i2 += 1
                        pb_ = 4 + (c % 2)
                        for jc in range(NJ_FF):
                            P.op(pe, lambda e, jc=jc, pb_=pb_: e.matmul(
                                psb(pb_, n), W2[wi][:, jc, :], hid[:, jc, 0:n], start=(jc == 0), stop=(jc == NJ_FF - 1)),
                                reads=[tW2[wi], thid], writes=[bank[pb_]], inc=(jc == NJ_FF - 1))
                        P.op(dve, lambda e, c=c, pb_=pb_: e.scalar_tensor_tensor(
                            xb[bi][:, c, 0:n], psb(pb_, n), der[:, l, j, 5, c:c + 1], xb[bi][:, c, 0:n], ALU.mult, ALU.add),
                            reads=[bank[pb_], consts, txb[bi]], writes=[txb[bi]])
                    P.dma(xT_d[s, :, c0:c0 + n].rearrange("(kc p) t -> p kc t", p=128), xb[bi][:, :, 0:n], txb[bi], False)
                P.barrier()

        def phaseC2_moe(s, l):
            T = 1024
            with ExitStack() as ps:
                xb = ps.enter_context(sbt("xbM", [128, 8, T], F32)); txb = P.tb("xbM")
                rstd = ps.enter_context(sbt("rstdM", [128, T], F32)); trs = P.tb("rstdM")
                tmpf = [ps.enter_context(sbt("tmpM%d" % i, [128, T], F32)) for i in range(2)]
                ttmp = [P.tb("tmpM%d" % i) for i in range(2)]
                h2f = [ps.enter_context(sbt("h2f%d" % i, [128, T], F32)) for i in range(2)]
                th2f = [P.tb("h2f%d" % i) for i in range(2)]
                h2 = ps.enter_context(sbt("h2M", [128, 8, T], BF16)); th2 = P.tb("h2M")
                hid = ps.enter_context(sbt("hidM", [128, NJ_E, T], BF16)); thid = P.tb("hidM")
                sq = hid[:, 0:8, 0:512]; tsq = thid
                acc = ps.enter_context(sbt("accM", [128, 8, T], F32)); tacc = P.tb("accM")
                cbc = [ps.enter_context(sbt("cbc%d" % i, [128, T], F32)) for i in range(2)]
                tcbc = [P.tb("cbc%d" % i) for i in range(2)]
                W1 = [ps.enter_context(sbt("W1M%d" % i, [128, 2, 8, 128], BF16)) for i in range(3)]
                tW1 = [P.tb("W1M%d" % i) for i in range(3)]
                W2 = [ps.enter_context(sbt("W2M%d" % i, [128, NJ_E, 128], BF16)) for i in range(2)]
                tW2 = [P.tb("W2M%d" % i) for i in range(2)]
                wr = ps.enter_context(sbt("wrM", [128, 8, NE], F32)); twr = P.tb("wrM")
                lg = ps.enter_context(sbt("lgM", [128, 8, NE], F32))
                lg2 = ps.enter_context(sbt("lg2M", [128, 8, NE], F32))
                mk1 = ps.enter_context(sbt("mk1M", [128, 8, NE], F32))
                mk2 = ps.enter_context(sbt("mk2M", [128, 8, NE], F32))
                comb = ps.enter_context(sbt("combM", [128, 8, NE], F32))
                rs = ps.enter_context(sbt("rsM", [128, 6, 8], F32)); trt = P.tb("routeM")
                combT = ps.enter_context(sbt("combTM", [8, T], F32)); tcT = P.tb("combTM")
                sel = ps.enter_context(sbt("selM", [8, NE, 128], F32)); tsel = P.tb("selM")
                P.dma(wr[:], moe_wr[:, :, :], twr, True)
                for e_ in range(NE):
                    P.op(dve, lambda e, e_=e_: e.tensor_copy(sel[:, e_, :], ident[0:8, e_:e_ + 1].to_broadcast([8, 128])),
                         reads=[ident_t], writes=[tsel])

                def loadW1(idx):
                    e_ = (idx // NJ_E) % NE
                    jc = idx % NJ_E
                    i = idx % 3
                    P.dma(flat2(W1[i][:, 0, :, :]), mg_c[e_, jc, :, :], tW1[i], True)
                    P.dma(flat2(W1[i][:, 1, :, :]), mu_c[e_, jc, :, :], tW1[i], True)

                def loadW2(idx):
                    e_ = (idx // 8) % NE
                    c = idx % 8
                    i = idx % 2
                    P.dma(flat2(W2[i][:, :, :]), md_c[e_, c, :, :], tW2[i], True)

                nb = NL // T
                loadW1(0); loadW1(1)
                loadW2(0)
                i1 = 0
                i2 = 0
                for b in range(nb):
                    c0 = b * T
                    P.dma(xb[:], xT_d[s, :, c0:c0 + T].rearrange("(kc p) t -> p kc t", p=128), txb, True)

                    def post(kc, i):
                        k2 = kc % 2
                        P.op(act, lambda e: e.activation(out=h2f[k2][:], in_=tmpf[i][:], func=AF.Identity, bias=der[:, l, s, 4, kc:kc + 1]),
                             reads=[ttmp[i], consts], writes=[th2f[k2]])
                        for tt in range(8):
                            P.op(pe, lambda e, tt=tt: e.matmul(psb(7, NE, tt * NE), h2f[k2][:, tt * 128:(tt + 1) * 128], wr[:, kc, :],
                                                               start=(kc == 0 and tt == 0), stop=(kc == 7)),
                                 reads=[th2f[k2], twr], writes=[bank[7]], inc=(tt == 7))
                        P.op(dve, lambda e: e.tensor_copy(h2[:, kc, :], h2f[k2][:]), reads=[th2f[k2]], writes=[th2])
                    norm_mod(xb, txb, T, l, s, 3, None, None, sq, tsq, rstd, trs, tmpf, ttmp, 6, post=post)
                    lgp = psb(7, 64).rearrange("p (t e) -> p t e", e=NE)
                    R = [trt]
                    P.op(dve, lambda e: e.tensor_copy(lg[:], lgp), reads=[bank[7]], writes=R)
                    P.op(dve, lambda e: e.tensor_reduce(rs[:, 0, :], lg[:], AX.X, ALU.max), reads=R, writes=R)
                    P.op(dve, lambda e: e.tensor_tensor(mk1[:], lg[:], rs[:, 0, :].unsqueeze(2).to_broadcast([128, 8, NE]), ALU.is_equal), reads=R, writes=R)
                    P.op(dve, lambda e: e.scalar_tensor_tensor(lg2[:], mk1[:], -1e30, lg[:], ALU.mult, ALU.add), reads=R, writes=R)
                    P.op(dve, lambda e: e.tensor_reduce(rs[:, 1, :], lg2[:], AX.X, ALU.max), reads=R, writes=R)
                    P.op(dve, lambda e: e.tensor_tensor(mk2[:], lg2[:], rs[:, 1, :].unsqueeze(2).to_broadcast([128, 8, NE]), ALU.is_equal), reads=R, writes=R)
                    P.op(dve, lambda e: e.tensor_tensor(rs[:, 2, :], rs[:, 1, :], rs[:, 0, :], ALU.subtract), reads=R, writes=R)
                    P.op(act, lambda e: e.activation(out=rs[:, 3, :], in_=rs[:, 2, :], func=AF.Exp), reads=R, writes=R)
                    P.op(dve, lambda e: e.tensor_scalar(rs[:, 4, :], rs[:, 3, :], 1.0, None, ALU.add), reads=R, writes=R)
                    P.op(dve, lambda e: e.reciprocal(rs[:, 4, :], rs[:, 4, :]), reads=R, writes=R)
                    P.op(dve, lambda e: e.tensor_tensor(rs[:, 5, :], rs[:, 3, :], rs[:, 4, :], ALU.mult), reads=R, writes=R)
                    P.op(dve, lambda e: e.tensor_tensor(mk1[:], mk1[:], rs[:, 4, :].unsqueeze(2).to_broadcast([128, 8, NE]), ALU.mult), reads=R, writes=R)
                    P.op(dve, lambda e: e.tensor_tensor(mk2[:], mk2[:], rs[:, 5, :].unsqueeze(2).to_broadcast([128, 8, NE]), ALU.mult), reads=R, writes=R)
                    P.op(dve, lambda e: e.tensor_tensor(comb[:], mk1[:], mk2[:], ALU.add), reads=R, writes=R)
                    for tt in range(8):
                        P.op(pe, lambda e, tt=tt: e.transpose(psT[3][0:8, tt * 128:(tt + 1) * 128], comb[:, tt, :], ident[:]),
                             reads=R + [ident_t], writes=[bank[6 + tt // 4]], inc=(tt % 4 == 3))
                    P.op(act, lambda e: e.copy(combT[:], psT[3][0:8, :]), reads=[bank[6], bank[7]], writes=[tcT])
                    for e_ in range(NE):
                        ci = e_ % 2
                        for sub in range(2):
                            P.op(pe, lambda e, sub=sub: e.matmul(psb(6 + sub), sel[:, e_, :], combT[:, sub * 512:(sub + 1) * 512], start=True, stop=True),
                                 reads=[tsel, tcT], writes=[bank[6 + sub]])
                        P.op(act, lambda e: e.copy(cbc[ci][:], psT[3][:, :]), reads=[bank[6], bank[7]], writes=[tcbc[ci]])
                        for jc in range(NJ_E):
                            wi = i1 % 3
                            k2 = i1 % 2
                            if i1 + 2 < nb * NE * NJ_E:
                                loadW1(i1 + 2)
                            i1 += 1
                            for gi in range(2):
                                for sub in range(2):
                                    for kc in range(8):
                                        P.op(pe, lambda e, gi=gi, sub=sub, kc=kc: e.matmul(
                                            psb(gi * 2 + sub), W1[wi][:, gi, kc, :], h2[:, kc, sub * 512:(sub + 1) * 512], start=(kc == 0), stop=(kc == 7)),
                                            reads=[tW1[wi], th2], writes=[bank[gi * 2 + sub]], inc=(kc == 7))
                            P.op(act, lambda e: e.activation(out=tmpf[k2][:], in_=psT[0][:, :], func=AF.Silu), reads=[bank[0], bank[1]], writes=[ttmp[k2]])
                            P.op(dve, lambda e, jc=jc: e.tensor_tensor(hid[:, jc, :], psT[1][:, :], tmpf[k2][:], ALU.mult),
                                 reads=[bank[2], bank[3], ttmp[k2]], writes=[thid])
                        for c in range(8):
                            wi = i2 % 2
                            if i2 + 1 < nb * NE * 8:
                                loadW2(i2 + 1)
                            i2 += 1
                            pq = 2 + (c % 2)
                            for sub in range(2):
                                for jc in range(NJ_E):
                                    P.op(pe, lambda e, jc=jc, sub=sub: e.matmul(
                                        psb(2 * pq + sub), W2[wi][:, jc, :], hid[:, jc, sub * 512:(sub + 1) * 512], start=(jc == 0), stop=(jc == NJ_E - 1)),
                                        reads=[tW2[wi], thid], writes=[bank[2 * pq + sub]], inc=(jc == NJ_E - 1))
                            if e_ == 0:
                                P.op(dve, lambda e, c=c: e.tensor_tensor(acc[:, c, :], psT[pq][:, :], cbc[ci][:], ALU.mult),
                                     reads=[bank[2 * pq], bank[2 * pq + 1], tcbc[ci]], writes=[tacc])
                            else:
                                k2 = c % 2
                                P.op(dve, lambda e: e.tensor_tensor(h2f[k2][:], psT[pq][:, :], cbc[ci][:], ALU.mult),
                                     reads=[bank[2 * pq], bank[2 * pq + 1], tcbc[ci]], writes=[th2f[k2]])
                                P.op(dve, lambda e, c=c: e.tensor_tensor(acc[:, c, :], acc[:, c, :], h2f[k2][:], ALU.add),
                                     reads=[th2f[k2], tacc], writes=[tacc])
                    for c in range(8):
                        P.op(dve, lambda e, c=c: e.scalar_tensor_tensor(xb[:, c, :], acc[:, c, :], der[:, l, s, 5, c:c + 1], xb[:, c, :], ALU.mult, ALU.add),
                             reads=[tacc, consts, txb], writes=[txb])
                    for sub in range(2):
                        P.op(act, lambda e, sub=sub: e.activation(out=sq[:, :, :], in_=xb[:, :, sub * 512:(sub + 1) * 512], func=AF.Square), reads=[txb], writes=[tsq])
                        for kc in range(8):
                            P.op(pe, lambda e, kc=kc: e.matmul(psb(6), ones_ms[:], sq[:, kc, :], start=(kc == 0), stop=(kc == 7)),
                                 reads=[tsq, consts], writes=[bank[6]], inc=(kc == 7))
                        P.op(act, lambda e, sub=sub: e.activation(out=rstd[:, sub * 512:(sub + 1) * 512], in_=psb(6), func=AF.Sqrt, bias=epsT[:, 0:1]),
                             reads=[bank[6], consts], writes=[trs])
                    P.op(dve, lambda e: e.reciprocal(rstd[:], rstd[:]), reads=[trs], writes=[trs])
                    for c in range(8):
                        P.op(dve, lambda e, c=c: e.scalar_tensor_tensor(xb[:, c, :], xb[:, c, :], fgs[:, c:c + 1], rstd[:], ALU.mult, ALU.mult),
                             reads=[txb, trs, cst2], writes=[txb])
                    for tt in range(8):
                        k2 = tt % 2
                        pst = psT[k2]
                        for c in range(8):
                            P.op(pe, lambda e, c=c, tt=tt, pst=pst: e.transpose(pst[:, c * 128:(c + 1) * 128], xb[:, c, tt * 128:(tt + 1) * 128], ident[:]),
                                 reads=[txb, ident_t], writes=[bank[2 * k2 + c // 4]], inc=(c % 4 == 3))
                        P.op(act, lambda e, pst=pst: e.copy(tmpf[k2][:], pst[:, :]), reads=[bank[2 * k2], bank[2 * k2 + 1]], writes=[ttmp[k2]])
                        P.dma(out2[s, c0 + tt * 128:c0 + (tt + 1) * 128, :], tmpf[k2][:], ttmp[k2], False)
                P.barrier()

        phase0()
        phaseM()
        stopped = False
        for s in seqs:
            for l in layers:
                if not stopped:
                    stopped = bool(seq_layer(s, l))
        P.barrier()
        print("instructions:", P.ninst, "sems:", P.nsem)
    return nc


def _rope_tables():
    t = np.arange(NL)
    row = (t // 64).astype(np.float64)
    col = (t % 64).astype(np.float64)
    inv = 10000.0 ** (-np.arange(16, dtype=np.float64) / 16)
    cosT = np.ones((128, NT), np.float32)
    sinT = np.zeros((128, NT), np.float32)
    for p in range(128):
        d = p % 64
        a, j, i = d // 32, (d // 16) % 2, d % 16
        pos = row if a == 0 else col
        ang = (pos.astype(np.float32) * np.float32(inv[i])).astype(np.float32)
        cosT[p, :NL] = np.cos(ang)
        sn = np.sin(ang)
        sinT[p, :NL] = -sn if j == 0 else sn
    return cosT, sinT


def _perm_cols():
    idx = np.arange(1024)
    d = idx % 64
    base = idx - d
    a, j, i = d // 32, (d // 16) % 2, d % 16
    return base + a * 32 + (1 - j) * 16 + i


def _prep_shared(inp):
    f = lambda a: np.ascontiguousarray(a, dtype=np.float32)
    T128 = lambda v: np.ascontiguousarray(v.reshape(-1, 128).T)
    perm = _perm_cols()
    w_in = inp["w_in"]
    w_perm = np.concatenate([w_in[:, :, OFF_CQ:OFF_CQ + 1024][:, :, perm], w_in[:, :, OFF_CK:OFF_CK + 1024][:, :, perm]], axis=2)
    cosT, sinT = _rope_tables()
    sh = {
        "w_ada": f(inp["w_ada"]),
        "b_adaT": f(np.stack([T128(inp["b_ada"][l]) for l in range(L)])),
        "n1gT": f(np.stack([T128(inp["norm1_g"][l]) for l in range(L)])),
        "n2gT": f(np.stack([T128(inp["norm2_g"][l]) for l in range(L)])),
        "fgT": f(T128(inp["final_norm_g"])),
        "w_in": f(w_in),
        "w_perm": f(w_perm),
        "b_gateT": f(np.stack([T128(inp["b_gate"][l]) for l in range(L)])),
        "a_ln_g": f(inp["a_ln_g"]),
        "a_ln_b": f(inp["a_ln_b"]),
        "a_wsT": f(np.transpose(inp["a_ws"], (0, 3, 1, 2))),
        "a_bsT": f(np.transpose(inp["a_bs"], (0, 2, 1))),
        "b_convT": f(np.stack([np.transpose(inp["b_conv"][l].reshape(3, 4, 128), (2, 1, 0)) for l in range(L)])),
        "c_lambda": f(inp["c_lambda"].reshape(L, 256)),
        "c_subln_g": f(inp["c_subln_g"]),
        "w_a_out": f(inp["w_a_out"]), "w_b_out": f(inp["w_b_out"]),
        "w_c_out": f(inp["w_c_out"]), "w_o": f(inp["w_o"]),
        "ff_wg": f(inp["ff_w_gate"][0]), "ff_wu": f(inp["ff_w_up"][0]), "ff_wd": f(inp["ff_w_down"][0]),
        "moe_wr": f(np.transpose(inp["moe_w_router"][0].reshape(8, 128, NE), (1, 0, 2))),
        "moe_wg": f(inp["moe_w_gate"][0]), "moe_wu": f(inp["moe_w_up"][0]), "moe_wd": f(inp["moe_w_down"][0]),
        "cosT": cosT, "sinT": sinT,
        "ident": np.eye(128, dtype=np.float32),
    }
    return sh


def _core_inputs(inp, shared, core):
    b0 = 2 * core
    c3 = np.stack([inp["c"][b0], inp["c"][b0 + 1], inp["c_ctx"]], axis=1)
    cT = np.ascontiguousarray(np.transpose(c3.reshape(8, 128, 3), (1, 0, 2)), dtype=np.float32)
    m = dict(shared)
    m["x2"] = np.ascontiguousarray(inp["x"][b0:b0 + 2], dtype=np.float32)
    m["ctx2"] = np.ascontiguousarray(inp["ctx"][b0:b0 + 2], dtype=np.float32)
    m["cT"] = cT
    return m


def kernel(**inputs):
    inp = {k: np.asarray(v) for k, v in inputs.items()}
    nc = build_nc()
    shared = _prep_shared(inp)
    in_maps = [_core_inputs(inp, shared, c) for c in range(8)]
    res = run_bass_kernel_spmd(nc, in_maps, core_ids=list(range(8)))
    out = np.concatenate([np.asarray(r["out2"]) for r in res.results], axis=0)
    return out.astype(np.float32)
```

```python
import math
from contextlib import ExitStack
import numpy as np
import concourse.bass as bass
import concourse.mybir as mybir
from concourse.bass_utils import run_bass_kernel_spmd

F32 = mybir.dt.float32
BF16 = mybir.dt.bfloat16
AF = mybir.ActivationFunctionType
ALU = mybir.AluOpType
AX = mybir.AxisListType

D = 1024
NL = 4096
NCX = 256
NT = NL + NCX
L = 2
EPS = 1e-6
OFF_AU, OFF_AV, OFF_BB, OFF_BC, OFF_BH = 0, 512, 1024, 1536, 2048
OFF_CQ, OFF_CK, OFF_CV, OFF_GATE = 2560, 3584, 4608, 5632
IN_COLS = 8704
WCOLS = IN_COLS + 2048
OFF_QP, OFF_KP = IN_COLS, IN_COLS + 1024
DFF = 2816
NE = 8
DFE = 3584
NJ_FF = DFF // 128
NJ_E = DFE // 128
BLKS = [(i * 512, 512) for i in range(8)] + [(NL, NCX)]


class Eng:
    def __init__(self, name, e, sem):
        self.name, self.e, self.sem, self.cnt, self.seen = name, e, sem, 0, {}


class TB:
    def __init__(self, name):
        self.name, self.w, self.r, self.dsem = name, None, {}, None


class DSem:
    def __init__(self, sem):
        self.sem, self.cnt = sem, 0


class Prog:
    def __init__(self, nc, es):
        self.nc, self.es = nc, es
        self.nsem = 0
        self.pe = Eng("pe", nc.tensor, self.mksem("s_pe"))
        self.act = Eng("act", nc.scalar, self.mksem("s_act"))
        self.dve = Eng("dve", nc.vector, self.mksem("s_dve"))
        self.pool = Eng("pool", nc.gpsimd, self.mksem("s_pool"))
        self.sp = Eng("sp", nc.sync, self.mksem("s_sp"))
        self.engs = [self.pe, self.act, self.dve, self.pool, self.sp]
        self.bar = self.mksem("s_bar")
        self.barcnt = 0
        self.tbs = []
        self.free_dsems = []
        self.dsems = []
        self.keep = set()
        self.ninst = 0

    def mksem(self, name):
        self.nsem += 1
        return self.es.enter_context(self.nc.semaphore(name))

    def tb(self, name, keep=False):
        t = TB(name)
        self.tbs.append(t)
        if keep:
            self.keep.add(name)
        return t

    def _wait(self, E, ev, raw=False):
        sem, val = ev
        if sem is E.sem and not (raw and E.name in ("act", "dve", "pool")):
            return
        k = id(sem)
        if E.seen.get(k, 0) >= val:
            return
        E.e.wait_ge(sem, val)
        self.ninst += 1
        E.seen[k] = val

    def _deps(self, E, reads, writes):
        for b in reads:
            if b.w is not None:
                self._wait(E, b.w, raw=True)
        for b in writes:
            if b.w is not None:
                self._wait(E, b.w, raw=True)
            for ev in b.r.values():
                self._wait(E, ev, raw=True)

    def _post(self, ev, reads, writes):
        for b in reads:
            b.r[id(ev[0])] = ev
        for b in writes:
            b.w = ev
            b.r = {}

    def op(self, E, fn, reads=(), writes=(), inc=True):
        self._deps(E, reads, writes)
        ins = fn(E.e)
        self.ninst += 1
        if inc:
            E.cnt += 1
            ins.then_inc(E.sem, 1)
            ev = (E.sem, E.cnt)
        else:
            ev = (E.sem, E.cnt + 1)
        self._post(ev, reads, writes)

    def dma(self, out, in_, sb, load, Q=None):
        Q = Q or self.sp
        if sb.dsem is None:
            if self.free_dsems:
                sb.dsem = self.free_dsems.pop()
            else:
                sb.dsem = DSem(self.mksem("dsem%d" % len(self.dsems)))
                self.dsems.append(sb.dsem)
        ds = sb.dsem
        reads = [] if load else [sb]
        writes = [sb] if load else []
        self._deps(Q, reads, writes)
        Q.e.dma_start(out=out, in_=in_).then_inc(ds.sem, 16)
        self.ninst += 1
        ds.cnt += 16
        self._post((ds.sem, ds.cnt), reads, writes)

    def barrier(self):
        sp = self.sp
        for E in self.engs:
            if E is not sp and E.cnt > 0:
                self._wait(sp, (E.sem, E.cnt))
        for ds in self.dsems:
            if ds.cnt:
                self._wait(sp, (ds.sem, ds.cnt))
        self.barcnt += 1
        sp.e.sem_inc(self.bar, 1)
        for E in self.engs:
            if E is sp:
                continue
            E.e.wait_ge(self.bar, self.barcnt)
            for E2 in self.engs:
                if E2.cnt:
                    E.seen[id(E2.sem)] = E2.cnt
            for ds in self.dsems:
                E.seen[id(ds.sem)] = ds.cnt
        for t in self.tbs:
            t.w = None
            t.r = {}
            if t.dsem is not None:
                self.free_dsems.append(t.dsem)
                t.dsem = None
        self.tbs = [t for t in self.tbs if t.name in self.keep]


class _Stop(Exception):
    pass


def build_nc(seqs=(0, 1), layers=(0, 1), debug_dump=False, stop=None):
    nc = bass.Bass("TRN2", target_bir_lowering=False)

    _uid = [0]

    def sbt(name, shape, dt):
        _uid[0] += 1
        return nc.sbuf_tensor("%s_u%d" % (name, _uid[0]), list(shape), dt)

    def din(name, shape, dt=F32):
        return nc.dram_tensor(name, list(shape), dt, kind="ExternalInput").ap()

    def dscr(name, shape, dt):
        return nc.dram_tensor(name, list(shape), dt).ap()

    x2 = din("x2", [2, NL, D])
    ctx2 = din("ctx2", [2, NCX, D])
    cT = din("cT", [128, 8, 3])
    w_ada = din("w_ada", [L, D, 6 * D])
    b_adaT = din("b_adaT", [L, 128, 48])
    n1gT = din("n1gT", [L, 128, 8])
    n2gT = din("n2gT", [L, 128, 8])
    fgT = din("fgT", [128, 8])
    w_in = din("w_in", [L, D, IN_COLS])
    w_perm = din("w_perm", [L, D, 2048])
    b_gateT = din("b_gateT", [L, 128, 24])
    a_ln_g = din("a_ln_g", [L, 512])
    a_ln_b = din("a_ln_b", [L, 512])
    a_wsT = din("a_wsT", [L, 128, 8, 128])
    a_bsT = din("a_bsT", [L, 128, 8])
    b_convT = din("b_convT", [L, 128, 4, 3])
    c_lambda = din("c_lambda", [L, 256])
    c_subln_g = din("c_subln_g", [L, 128])
    w_a_out = din("w_a_out", [L, 512, D])
    w_b_out = din("w_b_out", [L, 512, D])
    w_c_out = din("w_c_out", [L, D, D])
    w_o = din("w_o", [L, D, D])
    ff_wg = din("ff_wg", [D, DFF])
    ff_wu = din("ff_wu", [D, DFF])
    ff_wd = din("ff_wd", [DFF, D])
    moe_wr = din("moe_wr", [128, 8, NE])
    moe_wg = din("moe_wg", [NE, D, DFE])
    moe_wu = din("moe_wu", [NE, D, DFE])
    moe_wd = din("moe_wd", [NE, DFE, D])
    cosT = din("cosT", [128, NT])
    sinT = din("sinT", [128, NT])
    ident_d = din("ident", [128, 128])
    out2 = nc.dram_tensor("out2", [2, NL, D], F32, kind="ExternalOutput").ap()

    NCH = WCOLS // 128
    win_c = dscr("win_c", [L, NCH, 128, D], BF16)
    win_rm = dscr("win_rm", [L, D, 1024], BF16)
    wa_c = dscr("wa_c", [L, 8, 128, 512], BF16)
    wb_c = dscr("wb_c", [L, 8, 128, 512], BF16)
    wc_c = dscr("wc_c", [L, 8, 128, D], BF16)
    wo_c = dscr("wo_c", [L, 8, 128, D], BF16)
    fg_c = dscr("fg_c", [NJ_FF, 128, D], BF16)
    fu_c = dscr("fu_c", [NJ_FF, 128, D], BF16)
    fd_c = dscr("fd_c", [8, 128, DFF], BF16)
    mg_c = dscr("mg_c", [NE, NJ_E, 128, D], BF16)
    mu_c = dscr("mu_c", [NE, NJ_E, 128, D], BF16)
    md_c = dscr("md_c", [NE, 8, 128, DFE], BF16)
    kind = "ExternalOutput" if debug_dump else "Internal"
    xT_d = nc.dram_tensor("xT_d", [2, D, NT], F32, kind=kind).ap()
    yaT_d = nc.dram_tensor("yaT_d", [2, 512, NT], BF16, kind=kind).ap()
    ybT_d = nc.dram_tensor("ybT_d", [2, 512, NT], BF16, kind=kind).ap()
    ycT_d = nc.dram_tensor("ycT_d", [2, D, NT], BF16, kind=kind).ap()
    hT_dbg = nc.dram_tensor("hT_dbg", [128, 8, NT], BF16, kind=kind).ap()

    with ExitStack() as es:
        P = Prog(nc, es)
        pe, act, dve, sp = P.pe, P.act, P.dve, P.sp

        def sb(name, shape, dt):
            return es.enter_context(sbt(name, list(shape), dt))

        psT = [es.enter_context(nc.psum_tensor("ps%d" % i, [128, 1024], F32)) for i in range(4)]
        bank = [P.tb("bank%d" % i, True) for i in range(8)]

        def psb(i, n=512, off=0):
            t = psT[i // 2]
            o = (i % 2) * 512 + off
            return t[:, o:o + n]

        ident = sb("ident", [128, 128], F32); ident_t = P.tb("ident", True)
        ones_ms = sb("ones_ms", [128, 128], BF16)
        epsT = sb("epsT", [128, 1], F32)
        modT = sb("modT", [128, L, 48, 3], F32)
        der = sb("der", [128, L, 3, 6, 8], F32)
        neglam = sb("neglam", [128, L], F32)
        gsub = sb("gsub", [128, L, 128], F32)
        bgT = sb("bgT", [128, L, 24], F32)
        convT = sb("convT", [128, L, 4, 3], F32)
        fgs = sb("fgs", [128, 8], F32)
        consts = P.tb("consts", True)

        P.dma(ident[:], ident_d[:, :], ident_t, True)
        P.op(dve, lambda e: e.memset(ones_ms[:], 1.0 / D), writes=[consts])
        P.op(dve, lambda e: e.memset(epsT[:], EPS), writes=[consts])
        cst2 = P.tb("cst2", True)
        for l in range(L):
            P.dma(bgT[:, l, :], b_gateT[l, :, :], cst2, True)
            P.dma(convT[:, l, :, :], b_convT[l, :, :, :], cst2, True)
        P.dma(fgs[:], fgT[:, :], cst2, True)

        def cast_copy(i, out, in_, reads, writes):
            E = (act, dve)[i % 2]
            if E is act:
                P.op(act, lambda e: e.copy(out, in_), reads=reads, writes=writes)
            else:
                P.op(dve, lambda e: e.tensor_copy(out, in_), reads=reads, writes=writes)

        def phase0():
            with ExitStack() as ps:
                CH = 8192
                st32 = [ps.enter_context(sbt("st32_%d" % i, [128, CH], F32)) for i in range(2)]
                st16 = [ps.enter_context(sbt("st16_%d" % i, [128, CH], BF16)) for i in range(2)]
                t32 = [P.tb("st32_%d" % i) for i in range(2)]
                t16 = [P.tb("st16_%d" % i) for i in range(2)]
                k = [0]

                def conv_chunked(src, dst, ch0=0):
                    K_, N_ = src.shape
                    nk = K_ // 128
                    g = max(1, min(N_ // 128, CH // (nk * 128)))
                    for c0 in range(0, N_, g * 128):
                        gg = min(g, (N_ - c0) // 128)
                        gw = gg * 128
                        i = k[0] % 2
                        k[0] += 1
                        n = nk * gw
                        P.dma(st32[i][:, 0:n].rearrange("p (k c) -> p k c", k=nk),
                              src[:, c0:c0 + gw].rearrange("(k p) c -> p k c", p=128), t32[i], True)
                        cast_copy(k[0], st16[i][:, 0:n].rearrange("p (g k c) -> p g k c", g=gg, k=nk),
                                  st32[i][:, 0:n].rearrange("p (k g c) -> p g k c", k=nk, g=gg), [t32[i]], [t16[i]])
                        c_ = ch0 + c0 // 128
                        P.dma(dst[c_:c_ + gg, :, :].rearrange("g p x -> p g x"),
                              st16[i][:, 0:n].rearrange("p (g x) -> p g x", g=gg), t16[i], False)

                for l in layers:
                    conv_chunked(w_in[l], win_c[l], 0)
                    conv_chunked(w_perm[l], win_c[l], IN_COLS // 128)
                    for r0 in range(0, D, 128):
                        i = k[0] % 2
                        k[0] += 1
                        P.dma(st32[i][:, 0:1024], w_in[l, r0:r0 + 128, 0:1024], t32[i], True)
                        cast_copy(k[0], st16[i][:, 0:1024], st32[i][:, 0:1024], [t32[i]], [t16[i]])
                        P.dma(win_rm[l, r0:r0 + 128, :], st16[i][:, 0:1024], t16[i], False)
                    conv_chunked(w_a_out[l], wa_c[l])
                    conv_chunked(w_b_out[l], wb_c[l])
                    conv_chunked(w_c_out[l], wc_c[l])
                    conv_chunked(w_o[l], wo_c[l])
                if 0 in layers:
                    conv_chunked(ff_wg, fg_c)
                    conv_chunked(ff_wu, fu_c)
                    conv_chunked(ff_wd, fd_c)
                if 1 in layers:
                    for e_ in range(NE):
                        conv_chunked(moe_wg[e_], mg_c[e_])
                        conv_chunked(moe_wu[e_], mu_c[e_])
                        conv_chunked(moe_wd[e_], md_c[e_])
                P.barrier()

        def phaseM():
            with ExitStack() as ps:
                cTs = ps.enter_context(sbt("cTs", [128, 8, 3], F32))
                scT = ps.enter_context(sbt("scT", [128, 8, 3], F32))
                wad = [ps.enter_context(sbt("wad%d" % i, [128, 8, 512], F32)) for i in range(2)]
                twad = [P.tb("wad%d" % i) for i in range(2)]
                badT = ps.enter_context(sbt("badT", [128, L, 48], F32))
                ngT = ps.enter_context(sbt("ngT", [128, 2, L, 8], F32))
                lamt = ps.enter_context(sbt("lamt", [128, 256], F32))
                lprod = ps.enter_context(sbt("lprod", [128, 2, 64], F32))
                lsum = ps.enter_context(sbt("lsum", [128, 2], F32))
                lexp = ps.enter_context(sbt("lexp", [128, 2], F32))
                gsr = ps.enter_context(sbt("gsr", [128, 128], F32))
                tmp8 = ps.enter_context(sbt("tmp8", [128, 8], F32))
                tM = P.tb("tM"); tS = P.tb("tS")
                P.dma(cTs[:], cT[:, :, :], tM, True)
                P.op(act, lambda e: e.activation(out=scT[:], in_=cTs[:], func=AF.Silu), reads=[tM], writes=[tS])
                tb_bad = P.tb("badT")
                for l in range(L):
                    P.dma(badT[:, l, :], b_adaT[l, :, :], tb_bad, True)
                    P.dma(ngT[:, 0, l, :], n1gT[l, :, :], tb_bad, True)
                    P.dma(ngT[:, 1, l, :], n2gT[l, :, :], tb_bad, True)
                tmod = P.tb("modT")
                for l in range(L):
                    pb = bank[l]
                    for grp in range(12):
                        i = grp % 2
                        P.dma(wad[i][:], w_ada[l, :, grp * 512:(grp + 1) * 512].rearrange("(kc p) c -> p kc c", p=128), twad[i], True)
                        for cc in range(4):
                            col = (grp * 4 + cc) * 3
                            for kc in range(8):
                                P.op(pe, lambda e, i=i, cc=cc, kc=kc, col=col, l=l: e.matmul(
                                    psb(l, 3, col), wad[i][:, kc, cc * 128:(cc + 1) * 128], scT[:, kc, :],
                                    start=(kc == 0), stop=(kc == 7)),
                                    reads=[twad[i], tS], writes=[pb], inc=(kc == 7))
                    P.op(dve, lambda e, l=l: e.tensor_tensor(
                        modT[:, l, :, :], psb(l, 144).rearrange("p (c j) -> p c j", j=3),
                        badT[:, l, :].unsqueeze(2).to_broadcast([128, 48, 3]), ALU.add),
                        reads=[pb, tb_bad], writes=[tmod])
                    for j in range(3):
                        for half in range(2):
                            base = half * 24
                            P.op(dve, lambda e, l=l, j=j, base=base: e.tensor_scalar(
                                tmp8[:], modT[:, l, base + 8:base + 16, j], 1.0, None, ALU.add),
                                reads=[tmod], writes=[tM])
                            P.op(dve, lambda e, l=l, j=j, half=half: e.tensor_tensor(
                                der[:, l, j, half * 3 + 0, :], tmp8[:], ngT[:, half, l, :], ALU.mult),
                                reads=[tM, tb_bad], writes=[consts])
                            P.op(dve, lambda e, l=l, j=j, half=half, base=base: e.tensor_copy(
                                der[:, l, j, half * 3 + 1, :], modT[:, l, base:base + 8, j]),
                                reads=[tmod], writes=[consts])
                            P.op(dve, lambda e, l=l, j=j, half=half, base=base: e.tensor_copy(
                                der[:, l, j, half * 3 + 2, :], modT[:, l, base + 16:base + 24, j]),
                                reads=[tmod], writes=[consts])
                    lam_init = 0.8 - 0.6 * math.exp(-0.3 * l)
                    tl = P.tb("lam%d" % l)
                    P.dma(lamt[:], c_lambda[l, :].partition_broadcast(128), tl, True)
                    P.op(dve, lambda e: e.tensor_tensor(lprod[:, 0, :], lamt[:, 0:64], lamt[:, 64:128], ALU.mult), reads=[tl], writes=[tM])
                    P.op(dve, lambda e: e.tensor_tensor(lprod[:, 1, :], lamt[:, 128:192], lamt[:, 192:256], ALU.mult), reads=[tl], writes=[tM])
                    P.op(dve, lambda e: e.tensor_reduce(lsum[:], lprod[:], AX.X, ALU.add), reads=[tM], writes=[tM])
                    P.op(act, lambda e: e.activation(out=lexp[:], in_=lsum[:], func=AF.Exp), reads=[tM], writes=[tS])
                    P.op(dve, lambda e, l=l, li=lam_init: e.scalar_tensor_tensor(
                        neglam[:, l:l + 1], lexp[:, 1:2], -li, lexp[:, 0:1], ALU.add, ALU.subtract),
                        reads=[tS], writes=[consts])
                    tg = P.tb("gsr%d" % l)
                    P.dma(gsr[:], c_subln_g[l, :].partition_broadcast(128), tg, True)
                    P.op(act, lambda e, l=l, li=lam_init: e.mul(gsub[:, l, :], gsr[:], 1.0 - li), reads=[tg], writes=[consts])
                P.barrier()

        def norm_mod(xblk, txb, n, l, j, slot0, dst_fn, tdst, sq, tsq, rstd, trs, tmpf, ttmp, pbank, post=None):
            nsub = (n + 511) // 512
            for sub in range(nsub):
                c0 = sub * 512
                m = min(512, n - c0)
                P.op(act, lambda e: e.activation(out=sq[:, :, 0:m], in_=xblk[:, :, c0:c0 + m], func=AF.Square),
                     reads=[txb], writes=[tsq])
                for kc in range(8):
                    P.op(pe, lambda e, kc=kc: e.matmul(psb(pbank, m), ones_ms[:], sq[:, kc, 0:m], start=(kc == 0), stop=(kc == 7)),
                         reads=[tsq, consts], writes=[bank[pbank]], inc=(kc == 7))
                P.op(act, lambda e: e.activation(out=rstd[:, c0:c0 + m], in_=psb(pbank, m), func=AF.Sqrt, bias=epsT[:, 0:1]),
                     reads=[bank[pbank], consts], writes=[trs])
                P.op(dve, lambda e: e.reciprocal(rstd[:, c0:c0 + m], rstd[:, c0:c0 + m]), reads=[trs], writes=[trs])
            for kc in range(8):
                i = kc % 2
                P.op(dve, lambda e, kc=kc, i=i: e.scalar_tensor_tensor(
                    tmpf[i][:, 0:n], xblk[:, kc, 0:n], der[:, l, j, slot0, kc:kc + 1], rstd[:, 0:n], ALU.mult, ALU.mult),
                    reads=[txb, trs, consts], writes=[ttmp[i]])
                if post is not None:
                    post(kc, i)
                else:
                    P.op(act, lambda e, kc=kc, i=i: e.activation(
                        out=dst_fn(kc), in_=tmpf[i][:, 0:n], func=AF.Identity, bias=der[:, l, j, slot0 + 1, kc:kc + 1]),
                        reads=[ttmp[i], consts], writes=[tdst])

        def wchunk(l, c0):
            return win_c[l, c0 // 128, :, :]

        def flat2(ap):
            return ap.rearrange("p a b -> p (a b)")

        def seq_layer(s, l):
            last = (l == L - 1)
            nblk_full = 9
            nblk_lat = 8 if last else 9
            with ExitStack() as sl:
                hT = sl.enter_context(sbt("hT", [128, 8, NT], BF16))
                thT = [P.tb("hT%d" % b, True) for b in range(9)]

                with ExitStack() as ps:
                    xblk = [ps.enter_context(sbt("xblkA%d" % i, [128, 8, 512], F32)) for i in range(2)]
                    txb = [P.tb("xblkA%d" % i) for i in range(2)]
                    sq = ps.enter_context(sbt("sqA", [128, 8, 512], BF16)); tsq = P.tb("sqA")
                    rstd = ps.enter_context(sbt("rstdA", [128, 512], F32)); trs = P.tb("rstdA")
                    tmpf = [ps.enter_context(sbt("tmpA%d" % i, [128, 512], F32)) for i in range(2)]
                    ttmp = [P.tb("tmpA%d" % i) for i in range(2)]
                    if l == 0:
                        xtok = [ps.enter_context(sbt("xtok%d" % i, [128, D], F32)) for i in range(2)]
                        txt = [P.tb("xtok%d" % i) for i in range(2)]
                    tcount = 0
                    for b, (c0, n) in enumerate(BLKS):
                        i = b % 2
                        j = s if b < 8 else 2
                        if l == 0:
                            for tt in range(n // 128):
                                k = tcount % 2
                                tcount += 1
                                src = x2[s, c0 + tt * 128:c0 + (tt + 1) * 128, :] if b < 8 else ctx2[s, tt * 128:(tt + 1) * 128, :]
                                P.dma(xtok[k][:], src, txt[k], True)
                                pbk = 2 * (tcount % 2)
                                pst = psT[pbk // 2]
                                for kc in range(8):
                                    P.op(pe, lambda e, kc=kc, k=k, pst=pst: e.transpose(
                                        pst[:, kc * 128:(kc + 1) * 128], xtok[k][:, kc * 128:(kc + 1) * 128], ident[:]),
                                        reads=[txt[k], ident_t], writes=[bank[pbk + kc // 4]], inc=(kc % 4 == 3))
                                P.op(act, lambda e, pst=pst, i=i, tt=tt: e.copy(
                                    xblk[i][:, :, tt * 128:(tt + 1) * 128], pst[:, :].rearrange("p (k t) -> p k t", k=8)),
                                    reads=[bank[pbk], bank[pbk + 1]], writes=[txb[i]])
                            P.dma(xT_d[s, :, c0:c0 + n].rearrange("(kc p) t -> p kc t", p=128), xblk[i][:, :, 0:n], txb[i], False)
                        else:
                            P.dma(xblk[i][:, :, 0:n], xT_d[s, :, c0:c0 + n].rearrange("(kc p) t -> p kc t", p=128), txb[i], True)
                        norm_mod(xblk[i], txb[i], n, l, j, 0, lambda kc, c0=c0, n=n: hT[:, kc, c0:c0 + n], thT[b],
                                 sq, tsq, rstd, trs, tmpf, ttmp, 4 + (b % 2))
                    P.barrier()
                    if debug_dump:
                        tdbg = P.tb("hTdbg")
                        P.dma(hT_dbg[:, :, :], hT[:, :, :], tdbg, False)
                        P.barrier()
                if stop == "A":
                    return True

                with ExitStack() as ps:
                    WA = ps.enter_context(sbt("WA", [128, 8, 1024], BF16)); tWA = P.tb("WA")
                    ws32 = ps.enter_context(sbt("ws32", [128, 8, 128], F32)); tws32 = P.tb("ws32")
                    wsT = ps.enter_context(sbt("wsT", [128, 8, 128], BF16)); twsT = P.tb("wsT")
                    lng = ps.enter_context(sbt("lng", [128, 512], F32))
                    lnb = ps.enter_context(sbt("lnb", [128, 512], F32))
                    bsT = ps.enter_context(sbt("bsT", [128, 8], F32)); tln = P.tb("ln")
                    u_sb = [ps.enter_context(sbt("u_sb%d" % i, [128, 512], F32)) for i in range(2)]
                    v_sb = [ps.enter_context(sbt("v_sb%d" % i, [128, 512], F32)) for i in range(2)]
                    junk = ps.enter_context(sbt("junkB1", [128, 512], F32))
                    vc = [ps.enter_context(sbt("vc%d" % i, [128, 512], BF16)) for i in range(2)]
                    st = [ps.enter_context(sbt("stB1_%d" % i, [128, 8], F32)) for i in range(2)]
                    yaB = [ps.enter_context(sbt("yaB%d" % i, [128, 4, 512], BF16)) for i in range(2)]
                    tu = [P.tb("u%d" % i) for i in range(2)]; tv = [P.tb("v%d" % i) for i in range(2)]
                    tvc = [P.tb("vc%d" % i) for i in range(2)]; tst = [P.tb("st%d" % i) for i in range(2)]
                    tya = [P.tb("yaB%d" % i) for i in range(2)]; tjunk = P.tb("junkB1")
                    P.dma(WA[:, :, 0:512], win_rm[l, :, 0:512].rearrange("(kc p) c -> p kc c", p=128), tWA, True)
                    P.dma(WA[:, :, 512:1024], win_rm[l, :, 512:1024].rearrange("(kc p) c -> p kc c", p=128), tWA, True)
                    P.dma(ws32[:], a_wsT[l, :, :, :], tws32, True)
                    P.op(act, lambda e: e.copy(wsT[:], ws32[:]), reads=[tws32], writes=[twsT])
                    P.dma(lng[:], a_ln_g[l, :].partition_broadcast(128), tln, True)
                    P.dma(lnb[:], a_ln_b[l, :].partition_broadcast(128), tln, True)
                    P.dma(bsT[:], a_bsT[l, :, :], tln, True)
                    ntile = nblk_lat * 4 if nblk_lat == 8 else 34
                    def uvmm(tt):
                        i = tt % 2
                        b = min(tt // 4, 8)
                        bu, bv, bm, bt = (0, 1, 2, 3) if i == 0 else (4, 5, 6, 7)
                        tok = slice(tt * 128, (tt + 1) * 128)
                        for (bk, cs) in ((bu, 0), (bv, 512)):
                            for kc in range(8):
                                P.op(pe, lambda e, kc=kc, bk=bk, cs=cs: e.matmul(
                                    psb(bk), hT[:, kc, tok], WA[:, kc, cs:cs + 512], start=(kc == 0), stop=(kc == 7)),
                                    reads=[thT[b], tWA], writes=[bank[bk]], inc=(kc == 7))

                    uvmm(0)
                    for tt in range(ntile):
                        i = tt % 2
                        b = min(tt // 4, 8)
                        bu, bv, bm, bt = (0, 1, 2, 3) if i == 0 else (4, 5, 6, 7)
                        if tt + 1 < ntile:
                            uvmm(tt + 1)
                        P.op(dve, lambda e: e.memset(st[i][:, 0:2], 0.0), writes=[tst[i]])
                        P.op(act, lambda e: e.activation(out=u_sb[i][:], in_=psb(bu), func=AF.Gelu_apprx_tanh),
                             reads=[bank[bu]], writes=[tu[i]])
                        P.op(act, lambda e: e.activation(out=v_sb[i][:], in_=psb(bv), func=AF.Gelu_apprx_tanh, accum_out=st[i][:, 0:1]),
                             reads=[bank[bv]], writes=[tv[i], tst[i]])
                        P.op(act, lambda e: e.activation(out=junk[:], in_=v_sb[i][:], func=AF.Square, accum_out=st[i][:, 1:2]),
                             reads=[tv[i]], writes=[tjunk, tst[i]])
                        P.op(dve, lambda e: e.tensor_scalar(st[i][:, 2:4], st[i][:, 0:2], 1.0 / 512, None, ALU.mult), reads=[tst[i]], writes=[tst[i]])
                        P.op(dve, lambda e: e.tensor_tensor(st[i][:, 4:5], st[i][:, 2:3], st[i][:, 2:3], ALU.mult), reads=[tst[i]], writes=[tst[i]])
                        P.op(dve, lambda e: e.tensor_tensor(st[i][:, 5:6], st[i][:, 3:4], st[i][:, 4:5], ALU.subtract), reads=[tst[i]], writes=[tst[i]])
                        P.op(act, lambda e: e.activation(out=st[i][:, 6:7], in_=st[i][:, 5:6], func=AF.Sqrt, bias=epsT[:, 0:1]), reads=[tst[i], consts], writes=[tst[i]])
                        P.op(dve, lambda e: e.reciprocal(st[i][:, 7:8], st[i][:, 6:7]), reads=[tst[i]], writes=[tst[i]])
                        P.op(dve, lambda e: e.tensor_scalar(v_sb[i][:], v_sb[i][:], st[i][:, 2:3], st[i][:, 7:8], ALU.subtract, ALU.mult),
                             reads=[tst[i], tv[i]], writes=[tv[i]])
                        P.op(dve, lambda e: e.tensor_tensor(v_sb[i][:], v_sb[i][:], lng[:], ALU.mult), reads=[tv[i], tln], writes=[tv[i]])
                        P.op(dve, lambda e: e.tensor_tensor(vc[i][:], v_sb[i][:], lnb[:], ALU.add), reads=[tv[i], tln], writes=[tvc[i]])
                        for g in range(8):
                            P.op(pe, lambda e, g=g: e.matmul(psb(bm, 64, g * 64), wsT[:, g, :], vc[i][:, g * 64:(g + 1) * 64], start=True, stop=True),
                                 reads=[twsT, tvc[i]], writes=[bank[bm]], inc=(g == 7))
                        P.op(dve, lambda e: e.tensor_tensor(
                            v_sb[i][:].rearrange("p (g e) -> p g e", g=8), psb(bm).rearrange("p (g e) -> p g e", g=8),
                            bsT[:, :].unsqueeze(2).to_broadcast([128, 8, 64]), ALU.add),
                            reads=[bank[bm], tln], writes=[tv[i]])
                        P.op(dve, lambda e: e.tensor_tensor(u_sb[i][:], u_sb[i][:], v_sb[i][:], ALU.mult), reads=[tv[i], tu[i]], writes=[tu[i]])
                        for c in range(4):
                            P.op(pe, lambda e, c=c: e.transpose(psb(bt, 128, c * 128), u_sb[i][:, c * 128:(c + 1) * 128], ident[:]),
                                 reads=[tu[i], ident_t], writes=[bank[bt]], inc=(c == 3))
                        yb_i = (tt // 4) % 2
                        q = tt % 4
                        P.op(act, lambda e, yb_i=yb_i, q=q: e.copy(
                            yaB[yb_i][:, :, q * 128:(q + 1) * 128], psb(bt).rearrange("p (c t) -> p c t", c=4)),
                            reads=[bank[bt]], writes=[tya[yb_i]])
                        c0, n = BLKS[b]
                        if (tt + 1) * 128 == c0 + n:
                            P.dma(yaT_d[s, :, c0:c0 + n].rearrange("(c p) t -> p c t", p=128), yaB[yb_i][:, :, 0:n], tya[yb_i], False)
                    P.barrier()
                if stop == "B1":
                    return True

                with ExitStack() as ps:
                    WB = [ps.enter_context(sbt("WB%d" % i, [128, 3, 8, 128], BF16)) for i in range(2)]
                    tWB = [P.tb("WB%d" % i) for i in range(2)]
                    ZW = NT + 4
                    zf = ps.enter_context(sbt("zf", [128, ZW], F32)); tzf = P.tb("zf")
                    bgf = ps.enter_context(sbt("bgf", [128, NT], F32)); tbg = P.tb("bgf")
                    p4 = [ps.enter_context(sbt("p4_%d" % i, [128, 512], F32)) for i in range(2)]
                    tp4 = [P.tb("p4_%d" % i) for i in range(2)]
                    cv = [ps.enter_context(sbt("cv%d" % i, [128, 512], F32)) for i in range(2)]
                    tcv = [P.tb("cv%d" % i) for i in range(2)]
                    ybo = [ps.enter_context(sbt("ybo%d" % i, [128, 512], BF16)) for i in range(2)]
                    tybo = [P.tb("ybo%d" % i) for i in range(2)]
                    P.op(dve, lambda e: e.memset(zf[:], 0.0), writes=[tzf])

                    def zoff(b):
                        return 1 + BLKS[b][0] if b < 8 else NL + 2

                    def loadWB(fc):
                        i = fc % 2
                        for k3, off in enumerate((OFF_BB, OFF_BC, OFF_BH)):
                            P.dma(flat2(WB[i][:, k3, :, :]), wchunk(l, off + fc * 128), tWB[i], True)
                    loadWB(0)
                    cnt = 0
                    for fc in range(4):
                        i = fc % 2
                        if fc + 1 < 4:
                            loadWB(fc + 1)
                        for b in range(nblk_lat):
                            c0, n = BLKS[b]
                            pbs = (0, 1, 2) if cnt % 2 == 0 else (3, 4, 5)
                            k2 = cnt % 2
                            cnt += 1
                            for k3 in range(3):
                                for kc in range(8):
                                    P.op(pe, lambda e, k3=k3, kc=kc: e.matmul(
                                        psb(pbs[k3], n), WB[i][:, k3, kc, :], hT[:, kc, c0:c0 + n], start=(kc == 0), stop=(kc == 7)),
                                        reads=[tWB[i], thT[b]], writes=[bank[pbs[k3]]], inc=(kc == 7))
                            P.op(act, lambda e: e.copy(p4[k2][:, 0:n], psb(pbs[2], n)), reads=[bank[pbs[2]]], writes=[tp4[k2]])
                            P.op(dve, lambda e: e.tensor_tensor(zf[:, zoff(b):zoff(b) + n], psb(pbs[1], n), p4[k2][:, 0:n], ALU.mult),
                                 reads=[bank[pbs[1]], tp4[k2]], writes=[tzf])
                            P.op(act, lambda e: e.copy(bgf[:, c0:c0 + n], psb(pbs[0], n)), reads=[bank[pbs[0]]], writes=[tbg])
                        for b in range(nblk_lat):
                            c0, n = BLKS[b]
                            k2 = b % 2
                            z0 = zoff(b)
                            P.op(dve, lambda e: e.tensor_scalar(cv[k2][:, 0:n], zf[:, z0 - 1:z0 - 1 + n], convT[:, l, fc, 0:1], None, ALU.mult),
                                 reads=[tzf, cst2], writes=[tcv[k2]])
                            P.op(dve, lambda e: e.scalar_tensor_tensor(cv[k2][:, 0:n], zf[:, z0:z0 + n], convT[:, l, fc, 1:2], cv[k2][:, 0:n], ALU.mult, ALU.add),
                                 reads=[tzf, cst2, tcv[k2]], writes=[tcv[k2]])
                            P.op(dve, lambda e: e.scalar_tensor_tensor(cv[k2][:, 0:n], zf[:, z0 + 1:z0 + 1 + n], convT[:, l, fc, 2:3], cv[k2][:, 0:n], ALU.mult, ALU.add),
                                 reads=[tzf, cst2, tcv[k2]], writes=[tcv[k2]])
                            P.op(dve, lambda e: e.tensor_tensor(ybo[k2][:, 0:n], cv[k2][:, 0:n], bgf[:, c0:c0 + n], ALU.mult),
                                 reads=[tcv[k2], tbg], writes=[tybo[k2]])
                            P.dma(ybT_d[s, fc * 128:(fc + 1) * 128, c0:c0 + n], ybo[k2][:, 0:n], tybo[k2], False)
                    P.barrier()
                if stop == "B2":
                    return True

                with ExitStack() as ps:
                    cosS = ps.enter_context(sbt("cosS", [128, NT], F32))
                    sinS = ps.enter_context(sbt("sinS", [128, NT], F32)); ttab = P.tb("tab")
                    P.dma(cosS[:], cosT[:, :], ttab, True)
                    P.dma(sinS[:], sinT[:, :], ttab, True)
                    W5 = [ps.enter_context(sbt("W5_%d" % i, [128, 5, 8, 128], BF16)) for i in range(2)]
                    tW5 = [P.tb("W5_%d" % i) for i in range(2)]
                    QT = ps.enter_context(sbt("QT", [128, NT], BF16)); tQT = P.tb("QT")
                    KT = ps.enter_context(sbt("KT", [128, NT], BF16)); tKT = P.tb("KT")
                    VH = ps.enter_context(sbt("VH", [128, 34, 130], BF16)); tVH = P.tb("VH")
                    ycH = ps.enter_context(sbt("ycH", [128, NT], BF16)); tycH = P.tb("ycH")
                    r1 = [ps.enter_context(sbt("r1_%d" % i, [128, 512], F32)) for i in range(2)]
                    r2 = [ps.enter_context(sbt("r2_%d" % i, [128, 512], F32)) for i in range(2)]
                    tr1 = [P.tb("r1_%d" % i) for i in range(2)]; tr2 = [P.tb("r2_%d" % i) for i in range(2)]
                    PT = [ps.enter_context(sbt("PT%d" % i, [128, 1024], BF16)) for i in range(3)]
                    tPT = [P.tb("PT%d" % i) for i in range(3)]
                    o_sb = [ps.enter_context(sbt("o_sb%d" % i, [128, 128], F32)) for i in range(2)]
                    to = [P.tb("o_sb%d" % i) for i in range(2)]
                    sm = [ps.enter_context(sbt("sm%d" % i, [128, 8], F32)) for i in range(2)]
                    tsm = [P.tb("sm%d" % i) for i in range(2)]
                    junk = ps.enter_context(sbt("junkB3", [128, 128], F32)); tjunk = P.tb("junkB3")
                    osb = [ps.enter_context(sbt("osb%d" % i, [128, 3, 512], F32)) for i in range(2)]
                    tosb = [P.tb("osb%d" % i) for i in range(2)]
                    P.op(dve, lambda e: e.memset(VH[:, :, 128:130], 1.0), writes=[tVH])
                    P.op(dve, lambda e: e.memset(ycH[:], 0.0), writes=[tycH])

                    def loadW5(h):
                        i = h % 2
                        offs = (OFF_CQ + h * 128, OFF_QP + h * 128, OFF_CK + h * 128, OFF_KP + h * 128, OFF_CV + h * 128)
                        for k5, off in enumerate(offs):
                            P.dma(flat2(W5[i][:, k5, :, :]), wchunk(l, off), tW5[i], True)

                    def oacc(a, ncol=129):
                        return psb(4 + a // 3, ncol, (a % 3) * 160), bank[4 + a // 3]

                    loadW5(0)
                    pcount = [0]
                    for h in range(8):
                        wi = h % 2
                        if h + 1 < 8:
                            loadW5(h + 1)
                        for b in range(nblk_full):
                            c0, n = BLKS[b]
                            need_q = not (last and b == 8)
                            for (kk, dstT, tdst) in ((0, QT, tQT), (2, KT, tKT)):
                                if kk == 0 and not need_q:
                                    continue
                                i2 = pcount[0] % 2
                                pcount[0] += 1
                                b0, b1 = (0, 1) if i2 == 0 else (2, 3)
                                for (bk, k5) in ((b0, kk), (b1, kk + 1)):
                                    for kc in range(8):
                                        P.op(pe, lambda e, kc=kc, bk=bk, k5=k5: e.matmul(
                                            psb(bk, n), W5[wi][:, k5, kc, :], hT[:, kc, c0:c0 + n], start=(kc == 0), stop=(kc == 7)),
                                            reads=[tW5[wi], thT[b]], writes=[bank[bk]], inc=(kc == 7))
                                P.op(dve, lambda e: e.tensor_tensor(r1[i2][:, 0:n], psb(b0, n), cosS[:, c0:c0 + n], ALU.mult),
                                     reads=[bank[b0], ttab], writes=[tr1[i2]])
                                P.op(dve, lambda e: e.tensor_tensor(r2[i2][:, 0:n], psb(b1, n), sinS[:, c0:c0 + n], ALU.mult),
                                     reads=[bank[b1], ttab], writes=[tr2[i2]])
                                P.op(dve, lambda e, dstT=dstT: e.tensor_tensor(dstT[:, c0:c0 + n], r1[i2][:, 0:n], r2[i2][:, 0:n], ALU.add),
                                     reads=[tr1[i2], tr2[i2]], writes=[tdst])
                            nt_ = n // 128
                            for q in range(nt_):
                                tt = c0 // 128 + q
                                for kc in range(8):
                                    P.op(pe, lambda e, kc=kc, q=q, tt=tt: e.matmul(
                                        psb(7, 128, q * 128), hT[:, kc, tt * 128:(tt + 1) * 128], W5[wi][:, 4, kc, :], start=(kc == 0), stop=(kc == 7)),
                                        reads=[tW5[wi], thT[b]], writes=[bank[7]], inc=(kc == 7))
                            P.op(act, lambda e: e.copy(VH[:, c0 // 128:c0 // 128 + nt_, 0:128], psb(7, nt_ * 128).rearrange("p (q d) -> p q d", q=nt_)),
                                 reads=[bank[7]], writes=[tVH])
                        qblocks = [(qb * 512, 512, list(range(34))) for qb in range(8)]
                        if not last:
                            qblocks.append((NL, NCX, [32, 33]))
                        steps = []
                        for bi_, (q0, nq, kts) in enumerate(qblocks):
                            for ki, kt in enumerate(kts):
                                steps.append((bi_, q0, nq, kt, ki, len(kts)))
                        nst = len(steps)

                        def QK(i):
                            bi_, q0, nq, kt, ki, nk = steps[i]
                            sbuf_i = i % 2
                            S = psT[sbuf_i]
                            for m in range(2):
                                P.op(pe, lambda e, m=m: e.matmul(
                                    S[:, m * 512:m * 512 + nq], KT[m * 64:(m + 1) * 64, kt * 128:(kt + 1) * 128],
                                    QT[m * 64:(m + 1) * 64, q0:q0 + nq], start=True, stop=True),
                                    reads=[tKT, tQT], writes=[bank[2 * sbuf_i + m]], inc=(m == 1))

                        def EXP(i):
                            bi_, q0, nq, kt, ki, nk = steps[i]
                            sbuf_i = i % 2
                            pt_i = i % 3
                            S = psT[sbuf_i]
                            sb0 = 2 * sbuf_i
                            if nq == 512:
                                P.op(act, lambda e: e.activation(out=PT[pt_i][:], in_=S[:, :], func=AF.Exp, scale=0.125),
                                     reads=[bank[sb0], bank[sb0 + 1]], writes=[tPT[pt_i]])
                            else:
                                P.op(act, lambda e: e.activation(
                                    out=PT[pt_i][:, :].rearrange("p (m q) -> p m q", m=2)[:, :, 0:nq],
                                    in_=S[:, :].rearrange("p (m q) -> p m q", m=2)[:, :, 0:nq], func=AF.Exp, scale=0.125),
                                    reads=[bank[sb0], bank[sb0 + 1]], writes=[tPT[pt_i]])

                        def AV(i):
                            bi_, q0, nq, kt, ki, nk = steps[i]
                            pt_i = i % 3
                            nqi = nq // 128
                            for qi in range(nqi):
                                for m in range(2):
                                    a = qi * 2 + m
                                    ov, ob = oacc(a)
                                    P.op(pe, lambda e, m=m, qi=qi, ov=ov, a=a: e.matmul(
                                        ov, PT[pt_i][:, m * 512 + qi * 128:m * 512 + (qi + 1) * 128], VH[:, kt, 0:129],
                                        start=(ki == 0 and a % 3 == 0), stop=(ki == nk - 1)),
                                        reads=[tPT[pt_i], tVH], writes=[ob], inc=(qi == nqi - 1 and m == 1))

                        def EPI_evac(bi_):
                            q0, nq, kts = qblocks[bi_]
                            k3 = bi_ % 2
                            nb_ = 3 if nq == 512 else 2
                            for j3 in range(nb_):
                                P.op(dve, lambda e, j3=j3: e.tensor_copy(osb[k3][:, j3, 0:449], psb(4 + j3, 449)),
                                     reads=[bank[4 + j3]], writes=[tosb[k3]])

                        def EPI_rest(bi_):
                            q0, nq, kts = qblocks[bi_]
                            k3 = bi_ % 2
                            nqi = nq // 128
                            for qi in range(nqi):
                                k2 = qi % 2
                                a0, a1 = qi * 2, qi * 2 + 1
                                O0 = osb[k3][:, a0 // 3, (a0 % 3) * 160:(a0 % 3) * 160 + 129]
                                O1 = osb[k3][:, a1 // 3, (a1 % 3) * 160:(a1 % 3) * 160 + 129]
                                rd = [tosb[k3]]
                                P.op(dve, lambda e: e.reciprocal(sm[k2][:, 0:1], O0[:, 128:129]), reads=rd, writes=[tsm[k2]])
                                P.op(dve, lambda e: e.reciprocal(sm[k2][:, 1:2], O1[:, 128:129]), reads=rd, writes=[tsm[k2]])
                                P.op(dve, lambda e: e.tensor_tensor(sm[k2][:, 2:3], sm[k2][:, 1:2], neglam[:, l:l + 1], ALU.mult),
                                     reads=[tsm[k2], consts], writes=[tsm[k2]])
                                P.op(dve, lambda e: e.tensor_scalar(o_sb[k2][:], O0[:, 0:128], sm[k2][:, 0:1], None, ALU.mult),
                                     reads=rd + [tsm[k2]], writes=[to[k2]])
                                P.op(dve, lambda e: e.scalar_tensor_tensor(o_sb[k2][:], O1[:, 0:128], sm[k2][:, 2:3], o_sb[k2][:], ALU.mult, ALU.add),
                                     reads=rd + [tsm[k2], to[k2]], writes=[to[k2]])
                                P.op(dve, lambda e: e.memset(sm[k2][:, 3:4], 0.0), writes=[tsm[k2]])
                                P.op(dve, lambda e: e.tensor_tensor_scan if False else e.tensor_tensor(junk[:], o_sb[k2][:], o_sb[k2][:], ALU.mult),
                                     reads=[to[k2]], writes=[tjunk])
                                P.op(dve, lambda e: e.tensor_reduce(sm[k2][:, 3:4], junk[:], AX.X, ALU.add), reads=[tjunk], writes=[tsm[k2]])
                                P.op(act, lambda e: e.activation(out=sm[k2][:, 4:5], in_=sm[k2][:, 3:4], func=AF.Sqrt, bias=epsT[:, 0:1], scale=1.0 / 128),
                                     reads=[tsm[k2], consts], writes=[tsm[k2]])
                                P.op(dve, lambda e: e.reciprocal(sm[k2][:, 5:6], sm[k2][:, 4:5]), reads=[tsm[k2]], writes=[tsm[k2]])
                                P.op(dve, lambda e: e.scalar_tensor_tensor(o_sb[k2][:], o_sb[k2][:], sm[k2][:, 5:6], gsub[:, l, :], ALU.mult, ALU.mult),
                                     reads=[tsm[k2], to[k2], consts], writes=[to[k2]])
                                P.op(pe, lambda e, qi=qi: e.transpose(psb(7, 128, qi * 128), o_sb[k2][:], ident[:]),
                                     reads=[to[k2], ident_t], writes=[bank[7]])
                            P.op(dve, lambda e: e.tensor_copy(ycH[:, q0:q0 + nq], psb(7, nq)), reads=[bank[7]], writes=[tycH])

                        pending = []
                        QK(0)
                        for i in range(nst):
                            if i + 1 < nst:
                                QK(i + 1)
                            EXP(i)
                            AV(i)
                            bi_, q0, nq, kt, ki, nk = steps[i]
                            if ki == nk - 1:
                                EPI_evac(bi_)
                                pending.append((i + 6, bi_))
                            while pending and (pending[0][0] <= i or i == nst - 1):
                                EPI_rest(pending.pop(0)[1])
                        ncols = NT if not last else NL
                        P.dma(ycT_d[s, h * 128:(h + 1) * 128, 0:ncols], ycH[:, 0:ncols], tycH, False)
                    P.barrier()
                if stop == "B3":
                    return True

                with ExitStack() as ps:
                    WO = ps.enter_context(sbt("WO", [128, 8, D], BF16)); tWO = P.tb("WO")
                    P.dma(WO[:, :, :], wo_c[l, :, :, :].rearrange("g p x -> p g x"), tWO, True)
                    WC = [ps.enter_context(sbt("WC%d" % i, [128, 40, 128], BF16)) for i in range(2)]
                    tWC = [P.tb("WC%d" % i) for i in range(2)]
                    yin = [ps.enter_context(sbt("yin%d" % i, [128, 16, 512], BF16)) for i in range(2)]
                    tyin = [P.tb("yin%d" % i) for i in range(2)]
                    xb = [ps.enter_context(sbt("xbC%d" % i, [128, 8, 512], F32)) for i in range(2)]
                    txb = [P.tb("xbC%d" % i) for i in range(2)]
                    gs = [ps.enter_context(sbt("gs%d" % i, [128, 3, 512], F32)) for i in range(2)]
                    tgs = [P.tb("gs%d" % i) for i in range(2)]
                    t1 = [ps.enter_context(sbt("t1_%d" % i, [128, 512], F32)) for i in range(2)]
                    t2 = [ps.enter_context(sbt("t2_%d" % i, [128, 512], F32)) for i in range(2)]
                    tt1 = [P.tb("t1_%d" % i) for i in range(2)]; tt2 = [P.tb("t2_%d" % i) for i in range(2)]
                    yT = ps.enter_context(sbt("yT", [128, 8, 512], BF16)); tyT = P.tb("yT")

                    def loadWC(idx):
                        c = idx % 8
                        i = idx % 2
                        for br in range(3):
                            P.dma(flat2(WC[i][:, br * 8:(br + 1) * 8, :]), wchunk(l, OFF_GATE + br * D + c * 128), tWC[i], True)
                        P.dma(flat2(WC[i][:, 24:28, :]), wa_c[l, c, :, :], tWC[i], True)
                        P.dma(flat2(WC[i][:, 28:32, :]), wb_c[l, c, :, :], tWC[i], True)
                        P.dma(flat2(WC[i][:, 32:40, :]), wc_c[l, c, :, :], tWC[i], True)

                    def loadblk(b):
                        c0, n = BLKS[b]
                        i = b % 2
                        P.dma(yin[i][:, 0:4, 0:n], yaT_d[s, :, c0:c0 + n].rearrange("(c p) t -> p c t", p=128), tyin[i], True)
                        P.dma(yin[i][:, 4:8, 0:n], ybT_d[s, :, c0:c0 + n].rearrange("(c p) t -> p c t", p=128), tyin[i], True)
                        P.dma(yin[i][:, 8:16, 0:n], ycT_d[s, :, c0:c0 + n].rearrange("(c p) t -> p c t", p=128), tyin[i], True)
                        P.dma(xb[i][:, :, 0:n], xT_d[s, :, c0:c0 + n].rearrange("(kc p) t -> p kc t", p=128), txb[i], True)

                    loadblk(0)
                    loadWC(0)
                    idx = 0
                    nb = nblk_lat
                    for b in range(nb):
                        c0, n = BLKS[b]
                        bi = b % 2
                        j = s if b < 8 else 2
                        if b + 1 < nb:
                            loadblk(b + 1)
                        for c in range(8):
                            wi = idx % 2
                            k2 = idx % 2
                            if idx + 1 < nb * 8:
                                loadWC(idx + 1)
                            idx += 1
                            for br in range(3):
                                for kc in range(8):
                                    P.op(pe, lambda e, br=br, kc=kc: e.matmul(
                                        psb(br, n), WC[wi][:, br * 8 + kc, :], hT[:, kc, c0:c0 + n], start=(kc == 0), stop=(kc == 7)),
                                        reads=[tWC[wi], thT[b]], writes=[bank[br]], inc=(kc == 7))
                            for (br, k0, nk, y0) in ((0, 24, 4, 0), (1, 28, 4, 4), (2, 32, 8, 8)):
                                for kc in range(nk):
                                    P.op(pe, lambda e, br=br, kc=kc, k0=k0, y0=y0: e.matmul(
                                        psb(3 + br, n), WC[wi][:, k0 + kc, :], yin[bi][:, y0 + kc, 0:n], start=(kc == 0), stop=(kc == nk - 1)),
                                        reads=[tWC[wi], tyin[bi]], writes=[bank[3 + br]], inc=(kc == nk - 1))
                            for br in range(3):
                                P.op(act, lambda e, br=br: e.activation(out=gs[k2][:, br, 0:n], in_=psb(br, n), func=AF.Sigmoid,
                                                                        bias=bgT[:, l, br * 8 + c:br * 8 + c + 1]),
                                     reads=[bank[br], cst2], writes=[tgs[k2]])
                            P.op(dve, lambda e: e.tensor_tensor(t1[k2][:, 0:n], psb(3, n), gs[k2][:, 0, 0:n], ALU.mult), reads=[bank[3], tgs[k2]], writes=[tt1[k2]])
                            P.op(dve, lambda e: e.tensor_tensor(t2[k2][:, 0:n], psb(4, n), gs[k2][:, 1, 0:n], ALU.mult), reads=[bank[4], tgs[k2]], writes=[tt2[k2]])
                            P.op(dve, lambda e: e.tensor_tensor(t1[k2][:, 0:n], t1[k2][:, 0:n], t2[k2][:, 0:n], ALU.add), reads=[tt1[k2], tt2[k2]], writes=[tt1[k2]])
                            P.op(dve, lambda e: e.tensor_tensor(t2[k2][:, 0:n], psb(5, n), gs[k2][:, 2, 0:n], ALU.mult), reads=[bank[5], tgs[k2]], writes=[tt2[k2]])
                            P.op(dve, lambda e, c=c: e.tensor_tensor(yT[:, c, 0:n], t1[k2][:, 0:n], t2[k2][:, 0:n], ALU.add), reads=[tt1[k2], tt2[k2]], writes=[tyT])
                        for c in range(8):
                            pb_ = 6 + (c % 2)
                            for kc in range(8):
                                P.op(pe, lambda e, c=c, kc=kc, pb_=pb_: e.matmul(
                                    psb(pb_, n), WO[:, c, kc * 128:(kc + 1) * 128], yT[:, kc, 0:n], start=(kc == 0), stop=(kc == 7)),
                                    reads=[tWO, tyT], writes=[bank[pb_]], inc=(kc == 7))
                            P.op(dve, lambda e, c=c, pb_=pb_: e.scalar_tensor_tensor(
                                xb[bi][:, c, 0:n], psb(pb_, n), der[:, l, j, 2, c:c + 1], xb[bi][:, c, 0:n], ALU.mult, ALU.add),
                                reads=[bank[pb_], consts, txb[bi]], writes=[txb[bi]])
                        P.dma(xT_d[s, :, c0:c0 + n].rearrange("(kc p) t -> p kc t", p=128), xb[bi][:, :, 0:n], txb[bi], False)
                    P.barrier()
                if stop == "C1":
                    return True
            if l == 0:
                phaseC2_dense(s, l)
            else:
                phaseC2_moe(s, l)

        def phaseC2_dense(s, l):
            with ExitStack() as ps:
                xb = [ps.enter_context(sbt("xbD%d" % i, [128, 8, 512], F32)) for i in range(2)]
                txb = [P.tb("xbD%d" % i) for i in range(2)]
                sq = ps.enter_context(sbt("sqD", [128, 8, 512], BF16)); tsq = P.tb("sqD")
                rstd = ps.enter_context(sbt("rstdD", [128, 512], F32)); trs = P.tb("rstdD")
                tmpf = [ps.enter_context(sbt("tmpD%d" % i, [128, 512], F32)) for i in range(2)]
                ttmp = [P.tb("tmpD%d" % i) for i in range(2)]
                h2 = ps.enter_context(sbt("h2D", [128, 8, 512], BF16)); th2 = P.tb("h2D")
                hid = ps.enter_context(sbt("hidD", [128, NJ_FF, 512], BF16)); thid = P.tb("hidD")
                W1 = [ps.enter_context(sbt("W1D%d" % i, [128, 2, 8, 128], BF16)) for i in range(3)]
                tW1 = [P.tb("W1D%d" % i) for i in range(3)]
                W2 = [ps.enter_context(sbt("W2D%d" % i, [128, NJ_FF, 128], BF16)) for i in range(2)]
                tW2 = [P.tb("W2D%d" % i) for i in range(2)]
                ssb = [ps.enter_context(sbt("ssbD%d" % i, [128, 512], F32)) for i in range(2)]
                tss = [P.tb("ssbD%d" % i) for i in range(2)]

                def loadW1(idx):
                    jc = idx % NJ_FF
                    i = idx % 3
                    P.dma(flat2(W1[i][:, 0, :, :]), fg_c[jc, :, :], tW1[i], True)
                    P.dma(flat2(W1[i][:, 1, :, :]), fu_c[jc, :, :], tW1[i], True)

                def loadW2(idx):
                    c = idx % 8
                    i = idx % 2
                    P.dma(flat2(W2[i][:, :, :]), fd_c[c, :, :], tW2[i], True)

                def loadx(b):
                    c0, n = BLKS[b]
                    P.dma(xb[b % 2][:, :, 0:n], xT_d[s, :, c0:c0 + n].rearrange("(kc p) t -> p kc t", p=128), txb[b % 2], True)

                nb = 9
                loadx(0)
                loadW1(0); loadW1(1)
                loadW2(0)
                i1 = 0
                i2 = 0
                for b in range(nb):
                    c0, n = BLKS[b]
                    bi = b % 2
                    j = s if b < 8 else 2
                    if b + 1 < nb:
                        loadx(b + 1)
                    norm_mod(xb[bi], txb[bi], n, l, j, 3, lambda kc, n=n: h2[:, kc, 0:n], th2, sq, tsq, rstd, trs, tmpf, ttmp, 6)
                    for jc in range(NJ_FF):
                        wi = i1 % 3
                        k2 = i1 % 2
                        if i1 + 2 < nb * NJ_FF:
                            loadW1(i1 + 2)
                        i1 += 1
                        bg_, bu_ = (0, 1) if k2 == 0 else (2, 3)
                        for (bk, gi) in ((bg_, 0), (bu_, 1)):
                            for kc in range(8):
                                P.op(pe, lambda e, bk=bk, gi=gi, kc=kc: e.matmul(
                                    psb(bk, n), W1[wi][:, gi, kc, :], h2[:, kc, 0:n], start=(kc == 0), stop=(kc == 7)),
                                    reads=[tW1[wi], th2], writes=[bank[bk]], inc=(kc == 7))
                        P.op(act, lambda e: e.activation(out=ssb[k2][:, 0:n], in_=psb(bg_, n), func=AF.Silu), reads=[bank[bg_]], writes=[tss[k2]])
                        P.op(dve, lambda e, jc=jc: e.tensor_tensor(hid[:, jc, 0:n], psb(bu_, n), ssb[k2][:, 0:n], ALU.mult),
                             reads=[bank[bu_], tss[k2]], writes=[thid])
                    for c in range(8):
                        wi = i2 % 2
                        if i2 + 1 < nb * 8:
                            loadW2(i2 + 1)
                        i2 += 1
                        pb_ = 4 + (c % 2)
                        for jc in range(NJ_FF):
                            P.op(pe, lambda e, jc=jc, pb_=pb_: e.matmul(
                                psb(pb_, n), W2[wi][:, jc, :], hid[:, jc, 0:n], start=(jc == 0), stop=(jc == NJ_FF - 1)),
                                reads=[tW2[wi], thid], writes=[bank[pb_]], inc=(jc == NJ_FF - 1))
                        P.op(dve, lambda e, c=c, pb_=pb_: e.scalar_tensor_tensor(
                            xb[bi][:, c, 0:n], psb(pb_, n), der[:, l, j, 5, c:c + 1], xb[bi][:, c, 0:n], ALU.mult, ALU.add),
                            reads=[bank[pb_], consts, txb[bi]], writes=[txb[bi]])
                    P.dma(xT_d[s, :, c0:c0 + n].rearrange("(kc p) t -> p kc t", p=128), xb[bi][:, :, 0:n], txb[bi], False)
                P.barrier()

        def phaseC2_moe(s, l):
            T = 1024
            with ExitStack() as ps:
                xb = ps.enter_context(sbt("xbM", [128, 8, T], F32)); txb = P.tb("xbM")
                rstd = ps.enter_context(sbt("rstdM", [128, T], F32)); trs = P.tb("rstdM")
                tmpf = [ps.enter_context(sbt("tmpM%d" % i, [128, T], F32)) for i in range(2)]
                ttmp = [P.tb("tmpM%d" % i) for i in range(2)]
                h2f = [ps.enter_context(sbt("h2f%d" % i, [128, T], F32)) for i in range(2)]
                th2f = [P.tb("h2f%d" % i) for i in range(2)]
                h2 = ps.enter_context(sbt("h2M", [128, 8, T], BF16)); th2 = P.tb("h2M")
                hid = ps.enter_context(sbt("hidM", [128, NJ_E, T], BF16)); thid = P.tb("hidM")
                sq = hid[:, 0:8, 0:512]; tsq = thid
                acc = ps.enter_context(sbt("accM", [128, 8, T], F32)); tacc = P.tb("accM")
                cbc = [ps.enter_context(sbt("cbc%d" % i, [128, T], F32)) for i in range(2)]
                tcbc = [P.tb("cbc%d" % i) for i in range(2)]
                W1 = [ps.enter_context(sbt("W1M%d" % i, [128, 2, 8, 128], BF16)) for i in range(3)]
                tW1 = [P.tb("W1M%d" % i) for i in range(3)]
                W2 = [ps.enter_context(sbt("W2M%d" % i, [128, NJ_E, 128], BF16)) for i in range(2)]
                tW2 = [P.tb("W2M%d" % i) for i in range(2)]
                wr = ps.enter_context(sbt("wrM", [128, 8, NE], F32)); twr = P.tb("wrM")
                lg = ps.enter_context(sbt("lgM", [128, 8, NE], F32))
                lg2 = ps.enter_context(sbt("lg2M", [128, 8, NE], F32))
                mk1 = ps.enter_context(sbt("mk1M", [128, 8, NE], F32))
                mk2 = ps.enter_context(sbt("mk2M", [128, 8, NE], F32))
                comb = ps.enter_context(sbt("combM", [128, 8, NE], F32))
                rs = ps.enter_context(sbt("rsM", [128, 6, 8], F32)); trt = P.tb("routeM")
                combT = ps.enter_context(sbt("combTM", [8, T], F32)); tcT = P.tb("combTM")
                sel = ps.enter_context(sbt("selM", [8, NE, 128], F32)); tsel = P.tb("selM")
                P.dma(wr[:], moe_wr[:, :, :], twr, True)
                for e_ in range(NE):
                    P.op(dve, lambda e, e_=e_: e.tensor_copy(sel[:, e_, :], ident[0:8, e_:e_ + 1].to_broadcast([8, 128])),
                         reads=[ident_t], writes=[tsel])

                def loadW1(idx):
                    e_ = (idx // NJ_E) % NE
                    jc = idx % NJ_E
                    i = idx % 3
                    P.dma(flat2(W1[i][:, 0, :, :]), mg_c[e_, jc, :, :], tW1[i], True)
                    P.dma(flat2(W1[i][:, 1, :, :]), mu_c[e_, jc, :, :], tW1[i], True)

                def loadW2(idx):
                    e_ = (idx // 8) % NE
                    c = idx % 8
                    i = idx % 2
                    P.dma(flat2(W2[i][:, :, :]), md_c[e_, c, :, :], tW2[i], True)

                nb = NL // T
                loadW1(0); loadW1(1)
                loadW2(0)
                i1 = 0
                i2 = 0
                for b in range(nb):
                    c0 = b * T
                    P.dma(xb[:], xT_d[s, :, c0:c0 + T].rearrange("(kc p) t -> p kc t", p=128), txb, True)

                    def post(kc, i):
                        k2 = kc % 2
                        P.op(act, lambda e: e.activation(out=h2f[k2][:], in_=tmpf[i][:], func=AF.Identity, bias=der[:, l, s, 4, kc:kc + 1]),
                             reads=[ttmp[i], consts], writes=[th2f[k2]])
                        for tt in range(8):
                            P.op(pe, lambda e, tt=tt: e.matmul(psb(7, NE, tt * NE), h2f[k2][:, tt * 128:(tt + 1) * 128], wr[:, kc, :],
                                                               start=(kc == 0 and tt == 0), stop=(kc == 7)),
                                 reads=[th2f[k2], twr], writes=[bank[7]], inc=(tt == 7))
                        P.op(dve, lambda e: e.tensor_copy(h2[:, kc, :], h2f[k2][:]), reads=[th2f[k2]], writes=[th2])
                    norm_mod(xb, txb, T, l, s, 3, None, None, sq, tsq, rstd, trs, tmpf, ttmp, 6, post=post)
                    lgp = psb(7, 64).rearrange("p (t e) -> p t e", e=NE)
                    R = [trt]
                    P.op(dve, lambda e: e.tensor_copy(lg[:], lgp), reads=[bank[7]], writes=R)
                    P.op(dve, lambda e: e.tensor_reduce(rs[:, 0, :], lg[:], AX.X, ALU.max), reads=R, writes=R)
                    P.op(dve, lambda e: e.tensor_tensor(mk1[:], lg[:], rs[:, 0, :].unsqueeze(2).to_broadcast([128, 8, NE]), ALU.is_equal), reads=R, writes=R)
                    P.op(dve, lambda e: e.scalar_tensor_tensor(lg2[:], mk1[:], -1e30, lg[:], ALU.mult, ALU.add), reads=R, writes=R)
                    P.op(dve, lambda e: e.tensor_reduce(rs[:, 1, :], lg2[:], AX.X, ALU.max), reads=R, writes=R)
                    P.op(dve, lambda e: e.tensor_tensor(mk2[:], lg2[:], rs[:, 1, :].unsqueeze(2).to_broadcast([128, 8, NE]), ALU.is_equal), reads=R, writes=R)
                    P.op(dve, lambda e: e.tensor_tensor(rs[:, 2, :], rs[:, 1, :], rs[:, 0, :], ALU.subtract), reads=R, writes=R)
                    P.op(act, lambda e: e.activation(out=rs[:, 3, :], in_=rs[:, 2, :], func=AF.Exp), reads=R, writes=R)
                    P.op(dve, lambda e: e.tensor_scalar(rs[:, 4, :], rs[:, 3, :], 1.0, None, ALU.add), reads=R, writes=R)
                    P.op(dve, lambda e: e.reciprocal(rs[:, 4, :], rs[:, 4, :]), reads=R, writes=R)
                    P.op(dve, lambda e: e.tensor_tensor(rs[:, 5, :], rs[:, 3, :], rs[:, 4, :], ALU.mult), reads=R, writes=R)
                    P.op(dve, lambda e: e.tensor_tensor(mk1[:], mk1[:], rs[:, 4, :].unsqueeze(2).to_broadcast([128, 8, NE]), ALU.mult), reads=R, writes=R)
                    P.op(dve, lambda e: e.tensor_tensor(mk2[:], mk2[:], rs[:, 5, :].unsqueeze(2).to_broadcast([128, 8, NE]), ALU.mult), reads=R, writes=R)
                    P.op(dve, lambda e: e.tensor_tensor(comb[:], mk1[:], mk2[:], ALU.add), reads=R, writes=R)
                    for tt in range(8):
                        P.op(pe, lambda e, tt=tt: e.transpose(psT[3][0:8, tt * 128:(tt + 1) * 128], comb[:, tt, :], ident[:]),
                             reads=R + [ident_t], writes=[bank[6 + tt // 4]], inc=(tt % 4 == 3))
                    P.op(act, lambda e: e.copy(combT[:], psT[3][0:8, :]), reads=[bank[6], bank[7]], writes=[tcT])
                    for e_ in range(NE):
                        ci = e_ % 2
                        for sub in range(2):
                            P.op(pe, lambda e, sub=sub: e.matmul(psb(6 + sub), sel[:, e_, :], combT[:, sub * 512:(sub + 1) * 512], start=True, stop=True),
                                 reads=[tsel, tcT], writes=[bank[6 + sub]])
                        P.op(act, lambda e: e.copy(cbc[ci][:], psT[3][:, :]), reads=[bank[6], bank[7]], writes=[tcbc[ci]])
                        for jc in range(NJ_E):
                            wi = i1 % 3
                            k2 = i1 % 2
                            if i1 + 2 < nb * NE * NJ_E:
                                loadW1(i1 + 2)
                            i1 += 1
                            for gi in range(2):
                                for sub in range(2):
                                    for kc in range(8):
                                        P.op(pe, lambda e, gi=gi, sub=sub, kc=kc: e.matmul(
                                            psb(gi * 2 + sub), W1[wi][:, gi, kc, :], h2[:, kc, sub * 512:(sub + 1) * 512], start=(kc == 0), stop=(kc == 7)),
                                            reads=[tW1[wi], th2], writes=[bank[gi * 2 + sub]], inc=(kc == 7))
                            P.op(act, lambda e: e.activation(out=tmpf[k2][:], in_=psT[0][:, :], func=AF.Silu), reads=[bank[0], bank[1]], writes=[ttmp[k2]])
                            P.op(dve, lambda e, jc=jc: e.tensor_tensor(hid[:, jc, :], psT[1][:, :], tmpf[k2][:], ALU.mult),
                                 reads=[bank[2], bank[3], ttmp[k2]], writes=[thid])
                        for c in range(8):
                            wi = i2 % 2
                            if i2 + 1 < nb * NE * 8:
                                loadW2(i2 + 1)
                            i2 += 1
                            pq = 2 + (c % 2)
                            for sub in range(2):
                                for jc in range(NJ_E):
                                    P.op(pe, lambda e, jc=jc, sub=sub: e.matmul(
                                        psb(2 * pq + sub), W2[wi][:, jc, :], hid[:, jc, sub * 512:(sub + 1) * 512], start=(jc == 0), stop=(jc == NJ_E - 1)),
                                        reads=[tW2[wi], thid], writes=[bank[2 * pq + sub]], inc=(jc == NJ_E - 1))
                            if e_ == 0:
                                P.op(dve, lambda e, c=c: e.tensor_tensor(acc[:, c, :], psT[pq][:, :], cbc[ci][:], ALU.mult),
                                     reads=[bank[2 * pq], bank[2 * pq + 1], tcbc[ci]], writes=[tacc])
                            else:
                                k2 = c % 2
                                P.op(dve, lambda e: e.tensor_tensor(h2f[k2][:], psT[pq][:, :], cbc[ci][:], ALU.mult),
                                     reads=[bank[2 * pq], bank[2 * pq + 1], tcbc[ci]], writes=[th2f[k2]])
                                P.op(dve, lambda e, c=c: e.tensor_tensor(acc[:, c, :], acc[:, c, :], h2f[k2][:], ALU.add),
                                     reads=[th2f[k2], tacc], writes=[tacc])
                    for c in range(8):
                        P.op(dve, lambda e, c=c: e.scalar_tensor_tensor(xb[:, c, :], acc[:, c, :], der[:, l, s, 5, c:c + 1], xb[:, c, :], ALU.mult, ALU.add),
                             reads=[tacc, consts, txb], writes=[txb])
                    for sub in range(2):
                        P.op(act, lambda e, sub=sub: e.activation(out=sq[:, :, :], in_=xb[:, :, sub * 512:(sub + 1) * 512], func=AF.Square), reads=[txb], writes=[tsq])
                        for kc in range(8):
                            P.op(pe, lambda e, kc=kc: e.matmul(psb(6), ones_ms[:], sq[:, kc, :], start=(kc == 0), stop=(kc == 7)),
                                 reads=[tsq, consts], writes=[bank[6]], inc=(kc == 7))
                        P.op(act, lambda e, sub=sub: e.activation(out=rstd[:, sub * 512:(sub + 1) * 512], in_=psb(6), func=AF.Sqrt, bias=epsT[:, 0:1]),
                             reads=[bank[6], consts], writes=[trs])
                    P.op(dve, lambda e: e.reciprocal(rstd[:], rstd[:]), reads=[trs], writes=[trs])
                    for c in range(8):
                        P.op(dve, lambda e, c=c: e.scalar_tensor_tensor(xb[:, c, :], xb[:, c, :], fgs[:, c:c + 1], rstd[:], ALU.mult, ALU.mult),
                             reads=[txb, trs, cst2], writes=[txb])
                    for tt in range(8):
                        k2 = tt % 2
                        pst = psT[k2]
                        for c in range(8):
                            P.op(pe, lambda e, c=c, tt=tt, pst=pst: e.transpose(pst[:, c * 128:(c + 1) * 128], xb[:, c, tt * 128:(tt + 1) * 128], ident[:]),
                                 reads=[txb, ident_t], writes=[bank[2 * k2 + c // 4]], inc=(c % 4 == 3))
                        P.op(act, lambda e, pst=pst: e.copy(tmpf[k2][:], pst[:, :]), reads=[bank[2 * k2], bank[2 * k2 + 1]], writes=[ttmp[k2]])
                        P.dma(out2[s, c0 + tt * 128:c0 + (tt + 1) * 128, :], tmpf[k2][:], ttmp[k2], False)
                P.barrier()

        phase0()
        phaseM()
        stopped = False
        for s in seqs:
            for l in layers:
                if not stopped:
                    stopped = bool(seq_layer(s, l))
        P.barrier()
        print("instructions:", P.ninst, "sems:", P.nsem)
    return nc


def _rope_tables():
    t = np.arange(NL)
    row = (t // 64).astype(np.float64)
    col = (t % 64).astype(np.float64)
    inv = 10000.0 ** (-np.arange(16, dtype=np.float64) / 16)
    cosT = np.ones((128, NT), np.float32)
    sinT = np.zeros((128, NT), np.float32)
    for p in range(128):
        d = p % 64
        a, j, i = d // 32, (d // 16) % 2, d % 16
        pos = row if a == 0 else col
        ang = (pos.astype(np.float32) * np.float32(inv[i])).astype(np.float32)
        cosT[p, :NL] = np.cos(ang)
        sn = np.sin(ang)
        sinT[p, :NL] = -sn if j == 0 else sn
    return cosT, sinT


def _perm_cols():
    idx = np.arange(1024)
    d = idx % 64
    base = idx - d
    a, j, i = d // 32, (d // 16) % 2, d % 16
    return base + a * 32 + (1 - j) * 16 + i


def _prep_shared(inp):
    f = lambda a: np.ascontiguousarray(a, dtype=np.float32)
    T128 = lambda v: np.ascontiguousarray(v.reshape(-1, 128).T)
    perm = _perm_cols()
    w_in = inp["w_in"]
    w_perm = np.concatenate([w_in[:, :, OFF_CQ:OFF_CQ + 1024][:, :, perm], w_in[:, :, OFF_CK:OFF_CK + 1024][:, :, perm]], axis=2)
    cosT, sinT = _rope_tables()
    sh = {
        "w_ada": f(inp["w_ada"]),
        "b_adaT": f(np.stack([T128(inp["b_ada"][l]) for l in range(L)])),
        "n1gT": f(np.stack([T128(inp["norm1_g"][l]) for l in range(L)])),
        "n2gT": f(np.stack([T128(inp["norm2_g"][l]) for l in range(L)])),
        "fgT": f(T128(inp["final_norm_g"])),
        "w_in": f(w_in),
        "w_perm": f(w_perm),
        "b_gateT": f(np.stack([T128(inp["b_gate"][l]) for l in range(L)])),
        "a_ln_g": f(inp["a_ln_g"]),
        "a_ln_b": f(inp["a_ln_b"]),
        "a_wsT": f(np.transpose(inp["a_ws"], (0, 3, 1, 2))),
        "a_bsT": f(np.transpose(inp["a_bs"], (0, 2, 1))),
        "b_convT": f(np.stack([np.transpose(inp["b_conv"][l].reshape(3, 4, 128), (2, 1, 0)) for l in range(L)])),
        "c_lambda": f(inp["c_lambda"].reshape(L, 256)),
        "c_subln_g": f(inp["c_subln_g"]),
        "w_a_out": f(inp["w_a_out"]), "w_b_out": f(inp["w_b_out"]),
        "w_c_out": f(inp["w_c_out"]), "w_o": f(inp["w_o"]),
        "ff_wg": f(inp["ff_w_gate"][0]), "ff_wu": f(inp["ff_w_up"][0]), "ff_wd": f(inp["ff_w_down"][0]),
        "moe_wr": f(np.transpose(inp["moe_w_router"][0].reshape(8, 128, NE), (1, 0, 2))),
        "moe_wg": f(inp["moe_w_gate"][0]), "moe_wu": f(inp["moe_w_up"][0]), "moe_wd": f(inp["moe_w_down"][0]),
        "cosT": cosT, "sinT": sinT,
        "ident": np.eye(128, dtype=np.float32),
    }
    return sh


def _core_inputs(inp, shared, core):
    b0 = 2 * core
    c3 = np.stack([inp["c"][b0], inp["c"][b0 + 1], inp["c_ctx"]], axis=1)
    cT = np.ascontiguousarray(np.transpose(c3.reshape(8, 128, 3), (1, 0, 2)), dtype=np.float32)
    m = dict(shared)
    m["x2"] = np.ascontiguousarray(inp["x"][b0:b0 + 2], dtype=np.float32)
    m["ctx2"] = np.ascontiguousarray(inp["ctx"][b0:b0 + 2], dtype=np.float32)
    m["cT"] = cT
    return m


def kernel(**inputs):
    inp = {k: np.asarray(v) for k, v in inputs.items()}
    nc = build_nc()
    shared = _prep_shared(inp)
    in_maps = [_core_inputs(inp, shared, c) for c in range(8)]
    res = run_bass_kernel_spmd(nc, in_maps, core_ids=list(range(8)))
    out = np.concatenate([np.asarray(r["out2"]) for r in res.results], axis=0)
    return out.astype(np.float32)
```
